# Optimizing a Trainium2 kernel written in Bass

```python
import jax, jax.numpy as jnp
from jax import lax
import numpy as np

D_MODEL = 1024
BATCH = 8
SEQ = 2048
DEPTH = 2

ATT_HEADS = 8
ATT_HEAD_DIM = 64
ATT_WIDTH = ATT_HEADS * ATT_HEAD_DIM
MOBA_BLOCK = 256
MOBA_TOPK = 3
MOBA_Q_CHUNK = 32
RET_HEADS = 4
RET_HEAD_DIM = 128
RET_WIDTH = RET_HEADS * RET_HEAD_DIM
RET_CHUNK = 128
ROPE_BASE = 10000.0
IN_PROJ_WIDTH = 3 * ATT_WIDTH + 4 * RET_WIDTH
D_FF = 2816
N_EXPERTS = 8
TOP_K = 2
EXPERT_D_FF = 2816
MOE_GROUP = 128
EPS = 1e-6

kernel_name = "hymba_moba_retnet_moe_adaln"


def rms_norm(x, g):
    xf = x.astype(jnp.float32)
    y = xf * lax.rsqrt(jnp.mean(xf * xf, axis=-1, keepdims=True) + EPS)
    return (y * g.astype(jnp.float32)).astype(x.dtype)


def rotary(x):
    s, d = x.shape[1], x.shape[-1]
    half = d // 2
    inv_freq = ROPE_BASE ** (-jnp.arange(half, dtype=jnp.float32) / half)
    ang = jnp.arange(s, dtype=jnp.float32)[:, None] * inv_freq[None, :]
    cos = jnp.cos(ang)[None, :, None, :]
    sin = jnp.sin(ang)[None, :, None, :]
    xf = x.astype(jnp.float32)
    x1, x2 = xf[..., :half], xf[..., half:]
    return jnp.concatenate([x1 * cos - x2 * sin, x1 * sin + x2 * cos], axis=-1).astype(x.dtype)


def moba_attention(q, k, v):
    b, h, s, dh = q.shape
    nb = -(-s // MOBA_BLOCK)
    pad = nb * MOBA_BLOCK - s
    kp = jnp.pad(k, ((0, 0), (0, 0), (0, pad), (0, 0)))
    vp = jnp.pad(v, ((0, 0), (0, 0), (0, pad), (0, 0)))
    kb = kp.reshape(b, h, nb, MOBA_BLOCK, dh)
    vb = vp.reshape(b, h, nb, MOBA_BLOCK, dh)
    scale = dh ** -0.5
    n_sel = min(MOBA_TOPK, nb - 1)
    q_block = jnp.arange(s) // MOBA_BLOCK
    if n_sel > 0:
        k_mean = jnp.mean(kb.astype(jnp.float32), axis=3)
        gate = jnp.einsum('bhsd,bhnd->bhsn', q.astype(jnp.float32), k_mean)
        past = jnp.arange(nb)[None, :] < q_block[:, None]
        gate = jnp.where(past[None, None], gate, -1e30)
        _, sel = lax.top_k(gate, n_sel)
        sel_valid = sel < q_block[None, None, :, None]
    bi = jnp.arange(b)[:, None, None, None]
    hi = jnp.arange(h)[None, :, None, None]

    def chunk_fn(ci):
        start = ci * MOBA_Q_CHUNK
        qc = lax.dynamic_slice_in_dim(q, start, MOBA_Q_CHUNK, axis=2)
        pos = start + jnp.arange(MOBA_Q_CHUNK)
        own = start // MOBA_BLOCK
        k_own = lax.dynamic_index_in_dim(kb, own, axis=2, keepdims=False)
        v_own = lax.dynamic_index_in_dim(vb, own, axis=2, keepdims=False)
        key_pos = own * MOBA_BLOCK + jnp.arange(MOBA_BLOCK)
        s_own = jnp.einsum('bhqd,bhkd->bhqk', qc, k_own).astype(jnp.float32) * scale
        s_own = jnp.where(key_pos[None, :] <= pos[:, None], s_own, -jnp.inf)
        if n_sel > 0:
            sel_c = lax.dynamic_slice_in_dim(sel, start, MOBA_Q_CHUNK, axis=2)
            val_c = lax.dynamic_slice_in_dim(sel_valid, start, MOBA_Q_CHUNK, axis=2)
            k_sel = kb[bi, hi, sel_c]
            v_sel = vb[bi, hi, sel_c]
            s_sel = jnp.einsum('bhqd,bhqnkd->bhqnk', qc, k_sel).astype(jnp.float32) * scale
            s_sel = jnp.where(val_c[..., None], s_sel, -jnp.inf)
            s_sel = s_sel.reshape(b, h, MOBA_Q_CHUNK, n_sel * MOBA_BLOCK)
            p = jax.nn.softmax(jnp.concatenate([s_own, s_sel], axis=-1), axis=-1)
            p_own = p[..., :MOBA_BLOCK].astype(v.dtype)
            p_sel = p[..., MOBA_BLOCK:].reshape(b, h, MOBA_Q_CHUNK, n_sel, MOBA_BLOCK).astype(v.dtype)
            o = (jnp.einsum('bhqk,bhkd->bhqd', p_own, v_own)
                 + jnp.einsum('bhqnk,bhqnkd->bhqd', p_sel, v_sel))
        else:
            p_own = jax.nn.softmax(s_own, axis=-1).astype(v.dtype)
            o = jnp.einsum('bhqk,bhkd->bhqd', p_own, v_own)
        return o

    out = lax.map(chunk_fn, jnp.arange(s // MOBA_Q_CHUNK))
    return jnp.transpose(out, (1, 2, 0, 3, 4)).reshape(b, h, s, dh)


def retention(q, k, v):
    b, h, s, d = q.shape
    c = RET_CHUNK
    nc = s // c
    dt = v.dtype
    lg = jnp.log(1.0 - 2.0 ** (-5.0 - jnp.arange(h, dtype=jnp.float32)))
    idx = jnp.arange(c, dtype=jnp.float32)
    qf = q.astype(jnp.float32).reshape(b, h, nc, c, d)
    kf = (k.astype(jnp.float32) * d ** -0.5).reshape(b, h, nc, c, d)
    vf = v.astype(jnp.float32).reshape(b, h, nc, c, d)
    diff = idx[:, None] - idx[None, :]
    dmask = jnp.where(diff >= 0, jnp.exp(jnp.maximum(diff, 0.0)[None] * lg[:, None, None]), 0.0)
    scores = jnp.einsum('bhncd,bhnmd->bhncm', qf, kf) * dmask[None, :, None]
    inner = jnp.einsum('bhncm,bhnme->bhnce', scores, vf)
    k_dec = kf * jnp.exp((c - 1 - idx)[None, :] * lg[:, None])[None, :, None, :, None]
    kv = jnp.einsum('bhnmd,bhnme->bhnde', k_dec, vf)
    chunk_decay = jnp.exp(c * lg)[None, :, None, None]

    def step(state, kv_n):
        return state * chunk_decay + kv_n, state

    _, r_prev = lax.scan(step, jnp.zeros((b, h, d, d), jnp.float32), jnp.moveaxis(kv, 2, 0))
    r_prev = jnp.moveaxis(r_prev, 0, 2)
    q_dec = qf * jnp.exp((idx + 1.0)[None, :] * lg[:, None])[None, :, None, :, None]
    cross = jnp.einsum('bhncd,bhnde->bhnce', q_dec, r_prev)
    return (inner + cross).reshape(b, h, s, d).astype(dt)


def hybrid_mixer(h, w_in, w_out, att_out_g, ret_out_g):
    b, s, _ = h.shape
    proj = h @ w_in
    A, R = ATT_WIDTH, RET_WIDTH
    q_a, k_a, v_a, q_r, k_r, v_r, g_r = jnp.split(
        proj, [A, 2 * A, 3 * A, 3 * A + R, 3 * A + 2 * R, 3 * A + 3 * R], axis=-1)

    def att_heads(t):
        return t.reshape(b, s, ATT_HEADS, ATT_HEAD_DIM).transpose(0, 2, 1, 3)

    o_a = moba_attention(att_heads(q_a), att_heads(k_a), att_heads(v_a))
    o_a = o_a.transpose(0, 2, 1, 3).reshape(b, s, A)
    o_a = rms_norm(o_a, att_out_g)

    def ret_heads(t):
        return t.reshape(b, s, RET_HEADS, RET_HEAD_DIM)

    qr = rotary(ret_heads(q_r)).transpose(0, 2, 1, 3)
    kr = rotary(ret_heads(k_r)).transpose(0, 2, 1, 3)
    vr = ret_heads(v_r).transpose(0, 2, 1, 3)
    o_r = retention(qr, kr, vr).transpose(0, 2, 1, 3)
    o_r = rms_norm(o_r, ret_out_g.reshape(RET_HEADS, RET_HEAD_DIM)).reshape(b, s, R)
    o_r = jax.nn.silu(g_r) * o_r
    return jnp.concatenate([o_a, o_r], axis=-1) @ w_out


def swiglu(x, w_gate, w_up, w_down):
    return (jax.nn.silu(x @ w_gate) * (x @ w_up)) @ w_down


def moe_ffn(h, w_router, w_gate, w_up, w_down):
    b, s, d = h.shape
    xf = h.reshape(-1, d)
    n = xf.shape[0]
    e_count = w_gate.shape[0]
    g = MOE_GROUP
    logits = (xf @ w_router).astype(jnp.float32)
    top_logits, top_idx = lax.top_k(logits, TOP_K)
    top_w = jax.nn.softmax(top_logits, axis=-1)
    a = n * TOP_K
    e_flat = top_idx.reshape(-1)
    t_flat = jnp.repeat(jnp.arange(n, dtype=jnp.int32), TOP_K)
    w_flat = top_w.reshape(-1)
    order = jnp.argsort(e_flat, stable=True)
    e_sorted = e_flat[order]
    counts = jnp.bincount(e_flat, length=e_count)
    starts = jnp.cumsum(counts) - counts
    padded = (counts + g - 1) // g * g
    pad_ends = jnp.cumsum(padded)
    pad_starts = pad_ends - padded
    dest = pad_starts[e_sorted] + (jnp.arange(a) - starts[e_sorted])
    p_total = (-(-a // g) + e_count) * g
    tok_buf = jnp.full((p_total,), n, jnp.int32).at[dest].set(t_flat[order])
    w_buf = jnp.zeros((p_total,), jnp.float32).at[dest].set(w_flat[order])
    nblk = p_total // g
    blk_e = jnp.minimum(jnp.searchsorted(pad_ends, jnp.arange(nblk) * g, side='right'), e_count - 1)
    x_pad = jnp.concatenate([xf, jnp.zeros((1, d), xf.dtype)], axis=0)
    xb = x_pad[tok_buf].reshape(nblk, g, d)

    def expert_block(args):
        xg, e = args
        return swiglu(xg, w_gate[e], w_up[e], w_down[e])

    yb = lax.map(expert_block, (xb, blk_e)).reshape(p_total, d)
    yb = yb * w_buf[:, None].astype(yb.dtype)
    y = jax.ops.segment_sum(yb, tok_buf, num_segments=n + 1)[:n]
    return y.reshape(b, s, d)


def setup_inputs(seed: int = 0) -> dict:
    key = jax.random.key(seed)
    ks = jax.random.split(key, 20)
    n_dense = (DEPTH + 1) // 2
    n_moe = DEPTH // 2
    D = D_MODEL

    def nrm(k, shape, scale):
        return jax.random.normal(k, shape, jnp.float32) * scale

    return {
        "x": nrm(ks[0], (BATCH, SEQ, D), 1.0),
        "c": nrm(ks[1], (BATCH, D), 1.0),
        "norm_mix_g": 1.0 + nrm(ks[2], (DEPTH, D), 0.05),
        "norm_ffn_g": 1.0 + nrm(ks[3], (DEPTH, D), 0.05),
        "ada_w": nrm(ks[4], (DEPTH, D, 6 * D), 0.5 * D ** -0.5),
        "ada_b": nrm(ks[5], (DEPTH, 6 * D), 0.02),
        "w_in": nrm(ks[6], (DEPTH, D, IN_PROJ_WIDTH), D ** -0.5),
        "w_out": nrm(ks[7], (DEPTH, ATT_WIDTH + RET_WIDTH, D), (ATT_WIDTH + RET_WIDTH) ** -0.5),
        "att_out_g": 1.0 + nrm(ks[8], (DEPTH, ATT_WIDTH), 0.05),
        "ret_out_g": 1.0 + nrm(ks[9], (DEPTH, RET_WIDTH), 0.05),
        "ffn_w_gate": nrm(ks[10], (n_dense, D, D_FF), D ** -0.5),
        "ffn_w_up": nrm(ks[11], (n_dense, D, D_FF), D ** -0.5),
        "ffn_w_down": nrm(ks[12], (n_dense, D_FF, D), D_FF ** -0.5),
        "router_w": nrm(ks[13], (n_moe, D, N_EXPERTS), D ** -0.5),
        "moe_w_gate": nrm(ks[14], (n_moe, N_EXPERTS, D, EXPERT_D_FF), D ** -0.5),
        "moe_w_up": nrm(ks[15], (n_moe, N_EXPERTS, D, EXPERT_D_FF), D ** -0.5),
        "moe_w_down": nrm(ks[16], (n_moe, N_EXPERTS, EXPERT_D_FF, D), EXPERT_D_FF ** -0.5),
        "final_norm_g": 1.0 + nrm(ks[17], (D,), 0.05),
    }


def reference(x, c, norm_mix_g, norm_ffn_g, ada_w, ada_b, w_in, w_out, att_out_g, ret_out_g,
              ffn_w_gate, ffn_w_up, ffn_w_down, router_w, moe_w_gate, moe_w_up, moe_w_down,
              final_norm_g):
    cs = jax.nn.silu(c)
    for l in range(DEPTH):
        mod = cs @ ada_w[l] + ada_b[l]
        sh1, sc1, g1, sh2, sc2, g2 = jnp.split(mod, 6, axis=-1)
        h = rms_norm(x, norm_mix_g[l]) * (1.0 + sc1[:, None]) + sh1[:, None]
        x = x + g1[:, None] * hybrid_mixer(h, w_in[l], w_out[l], att_out_g[l], ret_out_g[l])
        h = rms_norm(x, norm_ffn_g[l]) * (1.0 + sc2[:, None]) + sh2[:, None]
        if l % 2 == 0:
            f = swiglu(h, ffn_w_gate[l // 2], ffn_w_up[l // 2], ffn_w_down[l // 2])
        else:
            f = moe_ffn(h, router_w[l // 2], moe_w_gate[l // 2], moe_w_up[l // 2], moe_w_down[l // 2])
        x = x + g2[:, None] * f
    return rms_norm(x, final_norm_g)
```

```python
import numpy as np
from contextlib import ExitStack
import concourse.bass as bass
import concourse.mybir as mybir
from concourse.bass_utils import run_bass_kernel_spmd
from concourse.alu_op_type import AluOpType as ALU

F32 = mybir.dt.float32
BF16 = mybir.dt.bfloat16
AF = mybir.ActivationFunctionType
AX = mybir.AxisListType

DEPTH = 2
S = 2048
D = 1024
NT = 16
KC = 8
DFF = 2816
NFC = 22
NE = 8
EPS = 1e-6
NEG = -240000.0
SAME_ENGINE_SYNC = True


class Prog:
    ENG = ["pe", "act", "dve", "pool", "sp"]

    def __init__(self, nc, sems, dma_sems):
        self.nc = nc
        self.items = {e: [] for e in self.ENG}
        self.sem = {e: sems[i] for i, e in enumerate(self.ENG)}
        self.cnt = {e: 0 for e in self.ENG}
        self.waited = {e: {} for e in self.ENG}
        self.lastw = {}
        self.readers = {}
        self.dma_sems = dma_sems
        self.dma_cnt = [0] * len(dma_sems)
        self.lock_last = {}

    @staticmethod
    def _locks(reads, writes):
        return {k[0] for k in list(reads) + list(writes)
                if isinstance(k, tuple) and len(k) == 2 and isinstance(k[0], int)}

    def _deps(self, eng, reads, writes):
        toks = {}

        def add(t):
            if t is None:
                return
            s, v = t
            k = id(s)
            if k not in toks or toks[k][1] < v:
                toks[k] = (s, v)
        for r in reads:
            add(self.lastw.get(r))
        for w in writes:
            add(self.lastw.get(w))
            for t in self.readers.get(w, {}).values():
                add(t)
        for b in self._locks(reads, writes):
            for e2, t in self.lock_last.get(b, {}).items():
                if e2 != eng:
                    add(t)
        out = []
        for k, (s, v) in toks.items():
            if s is self.sem[eng] and (eng == "pe" or not SAME_ENGINE_SYNC):
                continue
            if self.waited[eng].get(k, 0) < v:
                self.waited[eng][k] = v
                out.append((s, v))
        return out

    def _commit(self, tok, reads, writes):
        k = id(tok[0])
        for r in reads:
            d = self.readers.setdefault(r, {})
            if k not in d or d[k][1] < tok[1]:
                d[k] = tok
        for w in writes:
            self.lastw[w] = tok
            self.readers[w] = {}

    def op(self, eng, fn, reads=(), writes=()):
        for s, v in self._deps(eng, reads, writes):
            self.items[eng].append(("wait", s, v))
        self.cnt[eng] += 1
        self.items[eng].append(("op", fn, self.sem[eng], 1))
        tok = (self.sem[eng], self.cnt[eng])
        self._commit(tok, reads, writes)
        for b in self._locks(reads, writes):
            self.lock_last.setdefault(b, {})[eng] = tok

    def dma(self, eng, chan, fn, reads=(), writes=()):
        s = self.dma_sems[chan]
        deps = self._deps(eng, reads, writes)
        prev = self.dma_cnt[chan] * 16
        if prev and self.waited[eng].get(id(s), 0) < prev:
            self.waited[eng][id(s)] = prev
            deps.append((s, prev))
        for ss, v in deps:
            self.items[eng].append(("wait", ss, v))
        self.dma_cnt[chan] += 1
        self.items[eng].append(("op", fn, s, 16))
        self._commit((s, self.dma_cnt[chan] * 16), reads, writes)

    def final_wait(self, eng, keys):
        for s, v in self._deps(eng, keys, ()):
            self.items[eng].append(("wait", s, v))

    def replay(self, eng, e):
        for it in self.items[eng]:
            if it[0] == "wait":
                e.wait_ge(it[1], it[2])
            else:
                it[1](e).then_inc(it[2], it[3])


class StopBuild(Exception):
    pass


def build_nc(debug=(), stop=None, taps=()):
    nc = bass.Bass("TRN2", target_bir_lowering=False)

    def din(name, shape):
        return nc.dram_tensor(name, list(shape), F32, kind="ExternalInput").ap()
    x_d = din("x", [S, D])
    cT_d = din("cT", [128, KC])
    nmg_d = din("nmg", [128, DEPTH * KC])
    nfg_d = din("nfg", [128, DEPTH * KC])
    aog_d = din("aog", [128, DEPTH * 4])
    rog_d = din("rog", [128, DEPTH * 4])
    fng_d = din("fng", [128, KC])
    adaw_d = din("adaw", [DEPTH, KC, 128, 6 * D])
    adab_d = din("adab", [128, DEPTH * 48])
    adabr_d = din("adabr", [DEPTH, 6 * D])
    win_d = din("win", [DEPTH, 128, KC, 3584])
    wout_d = din("wout", [DEPTH, 128, KC, D])
    fg_d = din("fg", [128, KC, DFF])
    fu_d = din("fu", [128, KC, DFF])
    fd_d = din("fd", [128, NFC, D])
    rw_d = din("rw", [128, KC * NE])
    mg_d = din("mg", [NE, 128, KC, DFF])
    mu_d = din("mu", [NE, 128, KC, DFF])
    md_d = din("md", [NE, 128, NFC, D])
    idn_d = din("idn", [128, 128])
    cos_d = din("cos", [128, NT * 64])
    sin_d = din("sin", [128, NT * 64])
    dec_d = din("dec", [128, 12])
    m01_d = din("m01", [128, 128])
    mneg_d = din("mneg", [128, 128])
    en_d = din("en", [128, 8 * 128])
    past_d = din("past", [128, 256])
    nb_d = din("nb", [128, 256])
    y_d = nc.dram_tensor("y", [S, D], F32, kind="ExternalOutput").ap()
    tap_d = {}
    for name, (shp, dt_) in dict(taps).items():
        tap_d[name] = nc.dram_tensor("tap_" + name, list(shp), dt_, kind="ExternalOutput").ap()
    dbg_d = {}
    for name in debug:
        dbg_d[name] = nc.dram_tensor("dbg_" + name, [S, D], F32, kind="ExternalOutput").ap()

    with ExitStack() as st:
        TOT = 52800
        A = st.enter_context(nc.sbuf_tensor("arena", [128, TOT], F32))
        banks = [st.enter_context(nc.psum_tensor(f"bank{i}", [128, 512], F32)) for i in range(8)]
        sems = [st.enter_context(nc.semaphore(f"es{i}")) for i in range(5)]
        NCH = 24
        dsems = [st.enter_context(nc.semaphore(f"ds{i}")) for i in range(NCH)]
        P = Prog(nc, sems, dsems)

        cur = [0]

        def carve(n, dt=F32, at=None):
            if at is None:
                off = cur[0]
                cur[0] += n
            else:
                off = at
            assert off + n <= TOT, (off, n)
            ap = A[:, off:off + n]
            if dt != F32:
                ap = ap.bitcast(dt)
            return ap

        X = carve(NT * D).rearrange("p (t d) -> p t d", d=D)
        HT = carve(KC * S // 2, BF16).rearrange("p (k s) -> p k s", s=S)
        identb = carve(64, BF16)
        identf = carve(128)
        onesf = carve(128)
        cosT = carve(NT * 64).rearrange("p (t c) -> p t c", c=64)
        sinT = carve(NT * 64).rearrange("p (t c) -> p t c", c=64)
        dec = carve(12)
        m01 = carve(64, BF16)
        mneg = carve(64, BF16)
        EN = carve(512, BF16).rearrange("p (n k) -> p n k", k=128)
        pastm = carve(256)
        NB = carve(256)
        modT = carve(48)
        adab = carve(DEPTH * 48)
        nmg = carve(DEPTH * KC)
        nfg = carve(DEPTH * KC)
        aog = carve(DEPTH * 4)
        rog = carve(DEPTH * 4)
        fng = carve(KC)
        s1 = carve(KC)
        s2 = carve(KC)
        GBC = carve(D)
        cTt = carve(KC)
        csb = carve(KC // 2, BF16)
        csf = carve(KC)
        ss = carve(NT)
        rstd = carve(NT)
        tmp16 = carve(NT)
        ssa = carve(4 * NT)
        rstda = carve(NT)
        rwb = carve(KC * NE // 2, BF16).rearrange("p (k e) -> p k e", e=NE)
        lg = carve(NT * NE)
        wgt = carve(NT * NE)
        r1 = carve(NT * NE)
        r2 = carve(NT * NE)
        r3 = carve(NT)
        r4 = carve(NT)
        PH = cur[0]
        PHN = TOT - PH

        def bankv(b, n, dt=F32, off=0):
            ap = banks[b][:, off:off + n]
            if dt != F32:
                ap = ap.bitcast(dt)
            return ap

        def bk(b):
            return [(b, 0), (b, 1)]

        def mm(out, lhsT, rhs, start, stop, reads, writes, sgc=False):
            P.op("pe", lambda e: e.matmul(out, lhsT=lhsT, rhs=rhs, start=start, stop=stop, skip_group_check=sgc),
                 reads, writes)

        def tr(out, in_, reads, writes):
            P.op("pe", lambda e: e.transpose(out, in_, identb), list(reads) + ["identb"], writes)

        def act(out, in_, func, reads, writes, **kw):
            P.op("act", lambda e: e.activation(out=out, in_=in_, func=func, **kw), reads, writes)

        def tt(eng, out, in0, in1, op, reads, writes):
            P.op(eng, lambda e: e.tensor_tensor(out=out, in0=in0, in1=in1, op=op), reads, writes)

        def ts(eng, out, in0, s1_, s2_, op0, op1, reads, writes):
            if op1 is None:
                P.op(eng, lambda e: e.tensor_scalar(out=out, in0=in0, scalar1=s1_, scalar2=None, op0=op0), reads, writes)
            else:
                P.op(eng, lambda e: e.tensor_scalar(out=out, in0=in0, scalar1=s1_, scalar2=s2_, op0=op0, op1=op1), reads, writes)

        def stt(out, in0, scalar, in1, op0, op1, reads, writes):
            P.op("dve", lambda e: e.scalar_tensor_tensor(out=out, in0=in0, scalar=scalar, in1=in1, op0=op0, op1=op1),
                 reads, writes)

        def cp(eng, out, in_, reads, writes):
            if eng == "act":
                act(out, in_, AF.Copy, reads, writes)
            else:
                P.op(eng, lambda e: e.tensor_copy(out=out, in_=in_), reads, writes)

        cch = [0]

        def cload(out, in_, key, eng="sp"):
            ch = 16 + (cch[0] % 4)
            cch[0] += 1
            P.dma(eng, ch, lambda e: e.dma_start(out=out, in_=in_), (), [key])

        def castload(chan, out, in_, reads, writes):
            P.dma("pool", chan, lambda e: e.dma_start(out=out, in_=in_, max_dma_last_dim=4096), reads, writes)

        cload(identf, idn_d[:, :], "identf")
        cload(cosT, cos_d[:, :].rearrange("p (t c) -> p t c", c=64), "cos")
        cload(sinT, sin_d[:, :].rearrange("p (t c) -> p t c", c=64), "sin")
        cload(dec, dec_d[:, :], "dec")
        cload(pastm, past_d[:, :], "past")
        cload(NB, nb_d[:, :], "NB")
        cload(adab, adab_d[:, :], "adab")
        cload(nmg, nmg_d[:, :], "nmg")
        cload(nfg, nfg_d[:, :], "nfg")
        cload(aog, aog_d[:, :], "aog")
        cload(rog, rog_d[:, :], "rog")
        cload(fng, fng_d[:, :], "fng")
        cload(cTt, cT_d[:, :], "cT")
        castload(20, identb, idn_d[:, :], (), ["identb"])
        castload(21, m01, m01_d[:, :], (), ["m01"])
        castload(22, mneg, mneg_d[:, :], (), ["mneg"])
        castload(23, EN.rearrange("p n k -> p (n k)"), en_d[:, :], (), ["EN"])
        castload(20, rwb.rearrange("p k e -> p (k e)"), rw_d[:, :], (), ["rwb"])
        P.op("dve", lambda e: e.memset(onesf, 1.0), (), ["onesf"])
        for t4 in range(4):
            P.dma("sp", t4, lambda e, t4=t4: e.dma_start(
                out=X[:, 4 * t4:4 * t4 + 4, :],
                in_=x_d[512 * t4:512 * (t4 + 1), :].rearrange("(t p) d -> p t d", p=128)),
                (), [("X", 4 * t4 + i) for i in range(4)])
        act(csb, cTt, AF.Silu, ["cT"], ["csb"])
        act(csf, cTt, AF.Silu, ["cT"], ["csf"])

        def barrier():
            alle = ["pe", "act", "dve", "pool", "sp"]
            for e in alle:
                for o in alle:
                    if o == e or P.cnt[o] == 0:
                        continue
                    k = id(P.sem[o])
                    if P.waited[e].get(k, 0) < P.cnt[o]:
                        P.waited[e][k] = P.cnt[o]
                        P.items[e].append(("wait", P.sem[o], P.cnt[o]))
                for ch in range(NCH):
                    v = P.dma_cnt[ch] * 16
                    k = id(P.dma_sems[ch])
                    if v and P.waited[e].get(k, 0) < v:
                        P.waited[e][k] = v
                        P.items[e].append(("wait", P.dma_sems[ch], v))

        def dump(name):
            if name in dbg_d:
                for t4 in range(4):
                    P.dma("sp", 2 + (t4 % 2), lambda e, t4=t4: e.dma_start(
                        out=dbg_d[name][512 * t4:512 * (t4 + 1), :].rearrange("(t p) d -> p t d", p=128),
                        in_=X[:, 4 * t4:4 * t4 + 4, :]),
                        [("X", 4 * t4 + i) for i in range(4)], [("dbg", name, t4)])

        def compute_mod(l):
            ADA = [carve(3072, at=PH + 3000 + i * 3072) for i in range(2)]
            dtmp = carve(2048, at=PH + 9200)
            csrep = carve(KC * 128, at=PH + 11300).rearrange("p (k m) -> p k m", m=128)
            for kc in range(KC):
                ts("dve", csrep[:, kc, :], onesf, csf[:, kc:kc + 1], None, ALU.mult, None, ["onesf", "csf"], ["csrep"])
            it = 0
            for hf in range(2):
                for kc in range(KC):
                    sl = it % 2
                    it += 1
                    P.dma("sp", 4 + sl, lambda e, sl=sl, kc=kc, hf=hf: e.dma_start(
                        out=ADA[sl], in_=adaw_d[l, kc, :, hf * 3072:(hf + 1) * 3072]), (), [("ADA", sl)])
                    for j in range(6):
                        mm(bankv(j, 512), csrep[:, kc, :], ADA[sl][:, j * 512:(j + 1) * 512], kc == 0, kc == KC - 1,
                           [("ADA", sl), "csrep"], bk(j))
                for j in range(6):
                    d3 = dtmp[:, (j % 4) * 512:(j % 4 + 1) * 512].rearrange("p (a b) -> p a b", b=128)
                    tt("dve", d3, bankv(j, 512).rearrange("p (a b) -> p a b", b=128),
                       identf.unsqueeze(1).broadcast_to([128, 4, 128]), ALU.mult, bk(j) + ["identf"], [("dtmp", j % 4)])
                    c0 = hf * 24 + j * 4
                    P.op("dve", lambda e, d3=d3, c0=c0: e.tensor_reduce(out=modT[:, c0:c0 + 4], in_=d3, axis=AX.X, op=ALU.add),
                         [("dtmp", j % 4)], ["modT"])
            tt("dve", modT, modT, adab[:, l * 48:(l + 1) * 48], ALU.add, ["modT", "adab"], ["modT"])
            stt(s1, modT[:, 8:16], 1.0, nmg[:, l * KC:(l + 1) * KC], ALU.add, ALU.mult, ["modT", "nmg"], ["s1"])
            stt(s2, modT[:, 32:40], 1.0, nfg[:, l * KC:(l + 1) * KC], ALU.add, ALU.mult, ["modT", "nfg"], ["s2"])

        def make_gbc(gT, gkey):
            diag = carve(2 * 128, at=PH + 2600).rearrange("p (a b) -> p a b", b=128)
            for kc in range(KC):
                sl = kc % 2
                ts("dve", diag[:, sl, :], identf, gT[:, kc:kc + 1], None, ALU.mult, None,
                   ["identf", gkey], [("diag", sl)])
                b = 5 + kc // 4
                mm(bankv(b, 128, off=(kc % 4) * 128), onesf, diag[:, sl, :], True, True,
                   ["onesf", ("diag", sl)], bk(b))
            for h in range(2):
                cp("dve", GBC[:, h * 512:(h + 1) * 512], bankv(5 + h, 512), bk(5 + h), ["GBC"])

        def make_hT(sT, shT, skeys):
            junk = carve(512, BF16, at=PH)
            xn = carve(2048, BF16, at=PH + 512).rearrange("p (i d) -> p i d", d=D)
            for tq in range(4):
                for i in range(4):
                    t = tq * 4 + i
                    act(junk, X[:, t, :], AF.Square, [("X", t)], ["junk", ("ss", tq)], accum_out=ss[:, t:t + 1])
                q = slice(tq * 4, tq * 4 + 4)
                ts("dve", tmp16[:, q], ss[:, q], 1.0 / D, EPS, ALU.mult, ALU.add, [("ss", tq)], [("tmp16", tq)])
                act(tmp16[:, q], tmp16[:, q], AF.Sqrt, [("tmp16", tq)], [("tmp16", tq)])
                P.op("dve", lambda e, q=q: e.reciprocal(out=rstd[:, q], in_=tmp16[:, q]), [("tmp16", tq)], [("rstd", tq)])
                for i in range(4):
                    t = tq * 4 + i
                    if i % 2 == 0:
                        act(xn[:, i, :], X[:, t, :], AF.Identity, [("X", t), ("rstd", tq)], [("xn", i)], scale=rstd[:, t:t + 1])
                    else:
                        ts("pool", xn[:, i, :], X[:, t, :], rstd[:, t:t + 1], 0.0, ALU.mult, ALU.add,
                           [("X", t), ("rstd", tq)], [("xn", i)])
                for kc in range(KC):
                    b = kc % 4
                    pT = bankv(b, 256, BF16)
                    for i in range(4):
                        tr(pT[:, i * 128:(i + 1) * 128], xn[:, i, kc * 128:(kc + 1) * 128], [("xn", i)], bk(b))
                    o = HT[:, kc, tq * 512:(tq + 1) * 512]
                    if kc % 2 == 0:
                        act(o, pT, AF.Identity, bk(b) + skeys, [("HT", kc, tq)],
                            scale=sT[:, kc:kc + 1], bias=shT[:, kc:kc + 1])
                    else:
                        ts("dve", o, pT, sT[:, kc:kc + 1], shT[:, kc:kc + 1], ALU.mult, ALU.add,
                           bk(b) + skeys, [("HT", kc, tq)])

        HT_ALL = [("HT", kc, tq) for kc in range(KC) for tq in range(4)]

        def HTk(tq):
            return [("HT", kc, tq) for kc in range(KC)]

        def mixer(l):
            o = PH
            CT = carve(8192, BF16, at=o).rearrange("p (k s) -> p k s", s=S); o += 8192
            WIN = [carve(2048, BF16, at=o + i * 2048).rearrange("p (k c) -> p k c", c=512) for i in range(2)]
            WO = carve(4096, BF16, at=o).rearrange("p (k c) -> p k c", c=D); o += 4096
            U = o

            def load_unit(u):
                sl = u % 2
                if u < 4:
                    castload(6 + sl, WIN[sl][:, :, 0:384], win_d[l, :, :, u * 384:(u + 1) * 384], (), [("WIN", sl)])
                else:
                    r = u - 4
                    castload(6 + sl, WIN[sl][:, :, :], win_d[l, :, :, 1536 + r * 512:1536 + (r + 1) * 512], (), [("WIN", sl)])

            def att_unit(j):
                sl = j % 2
                W = WIN[sl]
                o = U
                QZ = [carve(1024, BF16, at=o + h * 1024) for h in range(2)]; o += 2048
                KZ = [carve(1024, BF16, at=o + h * 1024) for h in range(2)]; o += 2048
                VA = carve(1040, BF16, at=o).rearrange("p (t h c) -> p t h c", h=2, c=65); o += 1040
                bias2 = carve(1024, BF16, at=o).rearrange("p (t c) -> p t c", c=128); o += 1024
                PT = [carve(128, BF16, at=o + i * 128) for i in range(8)]; o += 1024
                otok = carve(1024, BF16, at=o).rearrange("p (t c) -> p t c", c=128); o += 1024
                gm = carve(256, at=o); o += 256
                cmp = carve(1024, at=o); o += 1024
                cnt = carve(256, at=o); o += 256
                kms = carve(8, at=o); o += 8
                kmbz = [carve(4, BF16, at=o + h * 4) for h in range(2)]; o += 8
                rec = carve(2, at=o); o += 2
                recb = carve(2, at=o); o += 2
                sq = carve(64, BF16, at=o); o += 64
                assert o <= TOT, o
                wk = ("WIN", sl)
                own = [slice(0, 64), slice(64, 128)]
                oth = [slice(64, 128), slice(0, 64)]
                aug = [slice(64, 72), slice(0, 8)]
                for h in range(2):
                    P.op("pool", lambda e, h=h: e.memset(QZ[h][oth[h], :], 0.0), (), [("QZz", h, 0), ("QZz", h, 1)])
                    P.op("pool", lambda e, h=h: e.memset(KZ[h][oth[h], :], 0.0), (), [("KZz", h)])
                    P.op("pool", lambda e, h=h: e.tensor_copy(
                        out=KZ[h][aug[h], :].rearrange("p (n r k) -> p n r k", r=2, k=128),
                        in_=EN[aug[h], :, :].unsqueeze(2).broadcast_to([8, 8, 2, 128])), ["EN", ("KZz", h)], [("KZz", h)])
                    P.op("pool", lambda e, h=h: e.memset(kmbz[h], 0.0), (), [("kmbz", h)])
                for which, dst, nm in ((0, QZ, "QZ"), (1, KZ, "KZ")):
                    for tc in range(4):
                        b = tc % 2
                        for kc in range(KC):
                            mm(bankv(b, 512), W[:, kc, which * 128:(which + 1) * 128], HT[:, kc, tc * 512:(tc + 1) * 512],
                               kc == 0, kc == KC - 1, [wk, ("HT", kc, tc)], bk(b))
                        for h in range(2):
                            cp("act" if h == 0 else "dve", dst[h][own[h], tc * 512:(tc + 1) * 512], bankv(b, 512)[own[h], :],
                               bk(b), [(nm, h, tc)])
                P.op("pool", lambda e: e.memset(VA[:, :, :, 64:65], 1.0), (), ["VAone"])
                for tq in range(4):
                    b = 2 + tq % 2
                    for i in range(4):
                        t = tq * 4 + i
                        for kc in range(KC):
                            mm(bankv(b, 128, off=i * 128), HT[:, kc, t * 128:(t + 1) * 128], W[:, kc, 256:384],
                               kc == 0, kc == KC - 1, [wk, ("HT", kc, tq)], bk(b))
                    cp("act" if tq % 2 == 0 else "dve", VA[:, tq * 4:tq * 4 + 4, :, 0:64],
                       bankv(b, 512).rearrange("p (t h c) -> p t h c", h=2, c=64), bk(b), [("VA", tq)])
                P.op("pool", lambda e: e.memset(bias2, 0.0), (), ["bias2"])
                for h in range(2):
                    P.op("dve", lambda e, h=h: e.tensor_reduce(out=kms[own[h], :], in_=KZ[h][own[h], :].rearrange("p (n k) -> p n k", k=256),
                                                              axis=AX.X, op=ALU.add),
                         [("KZ", h, tc) for tc in range(4)], [("kms", h)])
                    cp("dve", kmbz[h][own[h], :], kms[own[h], :], [("kms", h), ("kmbz", h)], [("kmbz", h)])
                    gp = bankv(h, 128)
                    for t in range(NT):
                        mm(gp[:, t * 8:t * 8 + 8], QZ[h][:, t * 128:(t + 1) * 128], kmbz[h],
                           True, True, [("QZ", h, t // 4), ("QZz", h, t // 8), ("kmbz", h)], bk(h))
                    tt("dve", gm[:, h * 128:(h + 1) * 128], gp, pastm[:, h * 128:(h + 1) * 128], ALU.add,
                       bk(h) + ["past"], [("gm", h)])
                    g3 = gm[:, h * 128:(h + 1) * 128].rearrange("p (g n) -> p g n", n=8)
                    cmp4 = cmp.rearrange("p (g n m) -> p g n m", n=8, m=8)
                    tt("dve", cmp4, g3.unsqueeze(2).broadcast_to([128, 16, 8, 8]), g3.unsqueeze(3).broadcast_to([128, 16, 8, 8]),
                       ALU.is_gt, [("gm", h)], ["cmp"])
                    P.op("dve", lambda e, h=h, cmp4=cmp4: e.tensor_reduce(
                        out=cnt[:, h * 128:(h + 1) * 128].rearrange("p (g n) -> p g n", n=8), in_=cmp4, axis=AX.X, op=ALU.add),
                        ["cmp"], [("cnt", h)])
                    c0 = 64 if h == 0 else 0
                    stt(bias2[:, :, c0:c0 + 8], cnt[:, h * 128:(h + 1) * 128].rearrange("p (t n) -> p t n", n=8), 2.5,
                        NB[:, h * 128:(h + 1) * 128].rearrange("p (t n) -> p t n", n=8), ALU.is_gt, ALU.mult,
                        [("cnt", h), "NB", "bias2"], ["bias2"])

                def emit_bias_T():
                    for tq in range(2, 4):
                        b = tq % 2
                        pT = bankv(b, 256, BF16)
                        for i in range(4):
                            t = tq * 4 + i
                            tr(pT[:, i * 128:(i + 1) * 128], bias2[:, t, :], ["bias2"], bk(b))
                        for h in range(2):
                            cp("act", QZ[h][aug[h], tq * 512:(tq + 1) * 512], pT[aug[h], :], bk(b), [("QZz", h, 1)])

                def emit_tail(qb):
                    for t in (2 * qb, 2 * qb + 1):
                        act(sq, otok[:, t, :], AF.Square, [("otok", t, 0), ("otok", t, 1)], ["sq", ("ssa", j)],
                            accum_out=ssa[:, j * NT + t:j * NT + t + 1])
                    if qb % 2 == 1:
                        tq = qb // 2
                        b = tq % 2
                        pT = bankv(b, 256, BF16)
                        for i in range(4):
                            t = tq * 4 + i
                            tr(pT[:, i * 128:(i + 1) * 128], otok[:, t, :], [("otok", t, 0), ("otok", t, 1)], bk(b))
                        ts("dve", CT[:, j, tq * 512:(tq + 1) * 512], pT, aog[:, l * 4 + j:l * 4 + j + 1], None, ALU.mult, None,
                           bk(b) + ["aog"], [("CT", j, tq)])

                early = [(h, qb, kt) for h in range(2) for qb in range(4) for kt in range(2 * qb + 2)]
                late = [(h, qb, kt) for h in range(2) for qb in range(4, 8) for kt in range(2 * qb + 2)]
                tiles = early + late
                rec2 = [rec, recb]

                def emit_st(i):
                    h, qb, kt = tiles[i]
                    hr = slice(h * 64, (h + 1) * 64)
                    slot = i % 8
                    sb_ = 2 + (i % 4)
                    hb_ = (i // 4) % 2
                    ST = bankv(sb_, 256, off=hb_ * 256)
                    skey = (sb_, hb_)
                    qlo = 128 if kt == 2 * qb + 1 else 0
                    qs = slice(qb * 256 + qlo, qb * 256 + 256)
                    diag = kt >= 2 * qb
                    mm(ST[:, qlo:256], KZ[h][:, kt * 128:(kt + 1) * 128], QZ[h][:, qs], True, not diag,
                       [("KZ", h, kt // 4), ("KZz", h), ("QZ", h, qb // 2), ("QZz", h, qb // 4)], [skey])
                    if diag:
                        dq0 = 0 if kt == 2 * qb else 128
                        mm(ST[:, dq0:dq0 + 128], identb, mneg, False, True, ["identb", "mneg"], [skey])
                    act(PT[slot][:, qlo:256], ST[:, qlo:256], AF.Exp, [skey], [("PT", slot)], scale=0.125)

                def emit_pv(i):
                    h, qb, kt = tiles[i]
                    slot = i % 8
                    pt = PT[slot]
                    ob = 6 + (h * 8 + qb) % 2
                    O = bankv(ob, 130).rearrange("p (q c) -> p q c", c=65)
                    nkt = 2 * qb + 2
                    qlo = 128 if kt == 2 * qb + 1 else 0
                    for qi in range(2):
                        if qi * 128 < qlo:
                            continue
                        mm(O[:, qi, :], pt[:, qi * 128:(qi + 1) * 128], VA[:, kt, h, :], (kt == 0 and qi == 0),
                           (kt == 2 * qb + qi), [("PT", slot), ("VA", kt // 4), "VAone"], bk(ob), sgc=True)
                    if kt == nkt - 1:
                        rc = rec2[ob % 2]
                        rk = ("rec", ob % 2)
                        P.op("dve", lambda e, O=O, rc=rc: e.reciprocal(out=rc.rearrange("p (q c) -> p q c", c=1), in_=O[:, :, 64:65]),
                             bk(ob), [rk])
                        for qi in range(2):
                            t = qb * 2 + qi
                            ts("dve", otok[:, t, h * 64:(h + 1) * 64], O[:, qi, 0:64], rc[:, qi:qi + 1], None,
                               ALU.mult, None, bk(ob) + [rk], [("otok", t, h)])
                        if h == 1:
                            emit_tail(qb)

                LAG = 4
                for i in range(len(tiles) + LAG):
                    if i == len(early):
                        emit_bias_T()
                    if i < len(tiles):
                        emit_st(i)
                    if i >= LAG:
                        emit_pv(i - LAG)

            def ret_unit(r):
                u = 4 + r
                sl = u % 2
                W = WIN[sl]
                wk = ("WIN", sl)
                o = U
                raS = [carve(256, at=o + i * 256) for i in range(2)]; o += 512
                rbS = [carve(256, at=o + i * 256) for i in range(2)]; o += 512
                rrS = [carve(256, at=o + i * 256) for i in range(2)]; o += 512
                qd = carve(256, BF16, at=o).rearrange("p (i c) -> p i c", c=128); o += 256
                ki = carve(256, BF16, at=o).rearrange("p (i c) -> p i c", c=128); o += 256
                kd = carve(1024, BF16, at=o).rearrange("p (t c) -> p t c", c=128); o += 1024
                vr = carve(1024, BF16, at=o).rearrange("p (t c) -> p t c", c=128); o += 1024
                sg = carve(1024, BF16, at=o).rearrange("p (t c) -> p t c", c=128); o += 1024
                qdT = carve(1024, BF16, at=o); o += 1024
                kiT = carve(1024, BF16, at=o); o += 1024
                PTr = [carve(64, BF16, at=o + i * 64) for i in range(2)]; o += 128
                stf = [carve(128, at=o + i * 128) for i in range(2)]; o += 256
                stb = [carve(64, BF16, at=o + i * 64) for i in range(2)]; o += 128
                Of = carve(2048, at=o).rearrange("p (t c) -> p t c", c=128); o += 2048
                on = [carve(64, BF16, at=o + i * 64) for i in range(2)]; o += 128
                sqr = carve(64, BF16, at=o); o += 64
                ssr = carve(NT, at=o); o += NT
                rsr = carve(NT, at=o); o += NT
                assert o <= TOT
                dq = dec[:, r * 3 + 0:r * 3 + 1]
                dki = dec[:, r * 3 + 1:r * 3 + 2]
                dkd = dec[:, r * 3 + 2:r * 3 + 3]
                gamma_c = float((1.0 - 2.0 ** (-5.0 - r)) ** 128)
                def emit_inproj_tile(t):
                    tq, i = t // 4, t % 4
                    b = t % 2
                    pp = bankv(b, 512)
                    for kc in range(KC):
                        mm(pp, HT[:, kc, t * 128:(t + 1) * 128], W[:, kc, :], kc == 0, kc == KC - 1,
                           [wk, ("HT", kc, tq)], bk(b))
                    bkk = bk(b)
                    ra, rb, rr = raS[t % 2], rbS[t % 2], rrS[t % 2]
                    p2 = t % 2
                    QK = pp[:, 0:256].rearrange("p (a h c) -> p a h c", a=2, h=2)
                    ra4 = ra.rearrange("p (a h c) -> p a h c", a=2, h=2)
                    rb3 = rb.rearrange("p (a h c) -> p a h c", a=2, h=2)
                    rr4 = rr.rearrange("p (a h c) -> p a h c", a=2, h=2)
                    cb4 = cosT[:, t, :].unsqueeze(1).unsqueeze(1).broadcast_to([128, 2, 2, 64])
                    sb3 = sinT[:, t, :].unsqueeze(1).broadcast_to([128, 2, 64])
                    tt("dve", ra4, QK, cb4, ALU.mult, bkk + ["cos"], [("ra", p2)])
                    tt("dve", rb3[:, :, 0, :], QK[:, :, 1, :], sb3, ALU.mult, bkk + ["sin"], [("rb0", p2)])
                    tt("dve", rb3[:, :, 1, :], QK[:, :, 0, :], sb3, ALU.mult, bkk + ["sin"], [("rb1", p2)])
                    tt("dve", rr4[:, :, 0, :], ra4[:, :, 0, :], rb3[:, :, 0, :], ALU.subtract, [("ra", p2), ("rb0", p2)], [("rr0", p2)])
                    tt("dve", rr4[:, :, 1, :], ra4[:, :, 1, :], rb3[:, :, 1, :], ALU.add, [("ra", p2), ("rb1", p2)], [("rr1", p2)])
                    rk = [("rr0", p2), ("rr1", p2)]
                    act(qd[:, i, :], rr[:, 0:128], AF.Copy, rk + ["dec"], [("qd", i)], scale=dq)
                    ts("pool", ki[:, i, :], rr[:, 128:256], dki, 0.0, ALU.mult, ALU.add, rk + ["dec"], [("ki", i)])
                    act(kd[:, t, :], rr[:, 128:256], AF.Copy, rk + ["dec"], [("kd", t)], scale=dkd)
                    cp("act", vr[:, t, :], pp[:, 256:384], bkk, [("vr", t)])
                    act(sg[:, t, :], pp[:, 384:512], AF.Silu, bkk, [("sg", t)])
                    if i == 3:
                        for src, dst, nm in ((qd, qdT, "qdT"), (ki, kiT, "kiT")):
                            bb = 0 if nm == "qdT" else 1
                            pT = bankv(bb, 256, BF16)
                            for ii in range(4):
                                tr(pT[:, ii * 128:(ii + 1) * 128], src[:, ii, :], [(nm[:2], ii)], bk(bb))
                            cp("act" if nm == "qdT" else "dve", dst[:, tq * 512:(tq + 1) * 512], pT, bk(bb), [(nm, tq)])

                for t in range(4):
                    emit_inproj_tile(t)
                def emit_sc(n):
                    cs_ = slice(n * 128, (n + 1) * 128)
                    sl2 = n % 2
                    SC = bankv(2 + sl2, 128)
                    mm(SC, kiT[:, cs_], qdT[:, cs_], True, True, [("kiT", n // 4), ("qdT", n // 4)], [(2 + sl2, 0)])
                    tt("dve", PTr[sl2], SC, m01, ALU.mult, [(2 + sl2, 0), "m01"], [("PTr", sl2)])
                    if n < NT - 1:
                        KV = bankv(6 + sl2, 128)
                        mm(KV, kd[:, n, :], vr[:, n, :], True, True, [("kd", n), ("vr", n)], [(6 + sl2, 0)])

                emit_sc(0)
                for n in range(NT):
                    cs_ = slice(n * 128, (n + 1) * 128)
                    sl2 = n % 2
                    if n + 4 < NT:
                        emit_inproj_tile(n + 4)
                    if n + 1 < NT:
                        emit_sc(n + 1)
                    OB = bankv(4 + sl2, 128)
                    mm(OB, PTr[sl2], vr[:, n, :], True, n == 0, [("PTr", sl2), ("vr", n)], [(4 + sl2, 0)])
                    if n > 0:
                        mm(OB, qdT[:, cs_], stb[(n - 1) % 2], False, True, [("qdT", n // 4), ("stb", (n - 1) % 2)],
                           [(4 + sl2, 0)])
                    if n < NT - 1:
                        KV = bankv(6 + sl2, 128)
                        if n == 0:
                            cp("dve", stf[0], KV, [(6 + sl2, 0)], [("stf", 0)])
                        else:
                            stt(stf[n % 2], stf[(n - 1) % 2], gamma_c, KV, ALU.mult, ALU.add,
                                [("stf", (n - 1) % 2), (6 + sl2, 0)], [("stf", n % 2)])
                        cp("act", stb[n % 2], stf[n % 2], [("stf", n % 2)], [("stb", n % 2)])
                    cp("act", Of[:, n, :], OB, [(4 + sl2, 0)], [("Of", n)])
                    act(sqr, OB, AF.Square, [(4 + sl2, 0)], ["sqr", "ssr"], accum_out=ssr[:, n:n + 1])
                ts("dve", rsr, ssr, 1.0 / 128, EPS, ALU.mult, ALU.add, ["ssr"], ["rsr"])
                act(rsr, rsr, AF.Sqrt, ["rsr"], ["rsr"])
                P.op("dve", lambda e: e.reciprocal(out=rsr, in_=rsr), ["rsr"], ["rsr"])
                for tq in range(4):
                    b = 6 + tq % 2
                    pT = bankv(b, 256, BF16)
                    for i in range(4):
                        t = tq * 4 + i
                        stt(on[t % 2], Of[:, t, :], rsr[:, t:t + 1], sg[:, t, :], ALU.mult, ALU.mult,
                            [("Of", t), "rsr", ("sg", t)], [("on", t % 2)])
                        tr(pT[:, i * 128:(i + 1) * 128], on[t % 2], [("on", t % 2)], bk(b))
                    ts("dve", CT[:, 4 + r, tq * 512:(tq + 1) * 512], pT, rog[:, l * 4 + r:l * 4 + r + 1], None, ALU.mult, None,
                       bk(b) + ["rog"], [("CT", 4 + r, tq)])

            load_unit(0)
            for u in range(8):
                if u + 1 < 8:
                    load_unit(u + 1)
                if u < 4:
                    att_unit(u)
                else:
                    ret_unit(u - 4)
                tap(f"CT{u}_{l}", CT[:, u, :], [("CT", u, tq) for tq in range(4)])
                ck(f"u{u}_{l}")
            castload(6, WO[:, 0:4, :], wout_d[l, :, 0:4, :], (), [("WIN", 0)])
            castload(7, WO[:, 4:8, :], wout_d[l, :, 4:8, :], (), [("WIN", 1)])
            for h in range(2):
                tt("pool", WO[:, 4 * h:4 * h + 4, :], WO[:, 4 * h:4 * h + 4, :], GBC.unsqueeze(1).broadcast_to([128, 4, D]), ALU.mult,
                   [("WIN", h), "GBC"], [("WIN", h)])
            P.op("dve", lambda e: e.tensor_reduce(out=rstda, in_=ssa.rearrange("p (u t) -> p t u", t=NT), axis=AX.X, op=ALU.add),
                 [("ssa", j) for j in range(4)], ["rstda"])
            ts("dve", rstda, rstda, 1.0 / 512, EPS, ALU.mult, ALU.add, ["rstda"], ["rstda"])
            act(rstda, rstda, AF.Sqrt, ["rstda"], ["rstda"])
            P.op("dve", lambda e: e.reciprocal(out=rstda, in_=rstda), ["rstda"], ["rstda"])
            bi = 0
            for t in range(NT):
                for hf in range(2):
                    xs = X[:, t, hf * 512:(hf + 1) * 512]
                    b = bi % 4; bi += 1
                    for kc in range(4):
                        mm(bankv(b, 512), CT[:, kc, t * 128:(t + 1) * 128], WO[:, kc, hf * 512:(hf + 1) * 512],
                           kc == 0, kc == 3, [("CT", kc, t // 4), ("WIN", 0)], bk(b))
                    stt(xs, bankv(b, 512), rstda[:, t:t + 1], xs, ALU.mult, ALU.add, bk(b) + ["rstda", ("X", t)], [("X", t)])
                    b = bi % 4; bi += 1
                    for kc in range(4, 8):
                        mm(bankv(b, 512), CT[:, kc, t * 128:(t + 1) * 128], WO[:, kc, hf * 512:(hf + 1) * 512],
                           kc == 4, kc == 7, [("CT", kc, t // 4), ("WIN", 1)], bk(b))
                    tt("dve", xs, bankv(b, 512), xs, ALU.add, bk(b) + [("X", t)], [("X", t)])

        GROUPS = [(0, 4), (4, 4), (8, 4), (12, 4), (16, 4), (20, 2)]

        def ffn(experts, stage):
            o = PH
            AT = carve(4096, BF16, at=o).rearrange("p (j s) -> p j s", s=S); o += 4096
            WG = [carve(2048, BF16, at=o + i * 6144).rearrange("p (k c) -> p k c", c=512) for i in range(2)]
            WU = [carve(2048, BF16, at=o + 2048 + i * 6144).rearrange("p (k c) -> p k c", c=512) for i in range(2)]
            WD = [carve(2048, BF16, at=o + 4096 + i * 6144).rearrange("p (j c) -> p j c", c=D) for i in range(2)]
            o += 12288
            SG = [carve(512, at=o + i * 512) for i in range(2)]; o += 1024
            assert o <= TOT
            work = [(e, g) for e in range(len(experts)) for g in range(len(GROUPS))]

            def load(idx):
                e, g = work[idx]
                gd, ud, dd, _ = experts[e]
                j0, nj = GROUPS[g]
                sl = idx % 2
                castload(8 + sl, WG[sl][:, :, 0:nj * 128], gd[:, :, j0 * 128:(j0 + nj) * 128], (), [("WG", sl)])
                castload(10 + sl, WU[sl][:, :, 0:nj * 128], ud[:, :, j0 * 128:(j0 + nj) * 128], (), [("WU", sl)])
                castload(12 + sl, WD[sl][:, 0:nj, :], dd[:, j0:j0 + nj, :], (), [("WD", sl)])
                tt("pool", WD[sl][:, 0:nj, :], WD[sl][:, 0:nj, :], GBC.unsqueeze(1).broadcast_to([128, nj, D]), ALU.mult,
                   [("WD", sl), "GBC"], [("WD", sl)])

            if stage == "pre":
                load(0)
                return
            gi = 0
            di = 0
            for idx, (e, g) in enumerate(work):
                if idx + 1 < len(work):
                    load(idx + 1)
                sl = idx % 2
                j0, nj = GROUPS[g]
                wcol = experts[e][3]
                for tc in range(4):
                    for j in range(nj):
                        bg = gi % 2
                        bu = 2 + gi % 2
                        gi += 1
                        for kc in range(KC):
                            mm(bankv(bg, 512), WG[sl][:, kc, j * 128:(j + 1) * 128], HT[:, kc, tc * 512:(tc + 1) * 512],
                               kc == 0, kc == KC - 1, [("WG", sl), ("HT", kc, tc)], bk(bg))
                        for kc in range(KC):
                            mm(bankv(bu, 512), WU[sl][:, kc, j * 128:(j + 1) * 128], HT[:, kc, tc * 512:(tc + 1) * 512],
                               kc == 0, kc == KC - 1, [("WU", sl), ("HT", kc, tc)], bk(bu))
                        sgt = SG[bg]
                        act(sgt, bankv(bg, 512), AF.Silu, bk(bg), [("SG", bg)])
                        tt("dve", AT[:, j, tc * 512:(tc + 1) * 512], sgt, bankv(bu, 512), ALU.mult,
                           [("SG", bg)] + bk(bu), [("AT", j, tc)])
                for t in range(NT):
                    for hf in range(2):
                        b = 4 + di % 4
                        di += 1
                        for j in range(nj):
                            mm(bankv(b, 512), AT[:, j, t * 128:(t + 1) * 128], WD[sl][:, j, hf * 512:(hf + 1) * 512],
                               j == 0, j == nj - 1, [("AT", j, t // 4), ("WD", sl)], bk(b))
                        xs = X[:, t, hf * 512:(hf + 1) * 512]
                        if wcol is None:
                            tt("dve", xs, bankv(b, 512), xs, ALU.add, bk(b) + [("X", t)], [("X", t)])
                        else:
                            stt(xs, bankv(b, 512), wgt[:, t * NE + wcol:t * NE + wcol + 1], xs, ALU.mult, ALU.add,
                                bk(b) + [("X", t), "wgt"], [("X", t)])

        def router():
            lp = bankv(7, 128)
            for t in range(NT):
                for kc in range(KC):
                    mm(lp[:, t * 8:(t + 1) * 8], HT[:, kc, t * 128:(t + 1) * 128], rwb[:, kc, :], kc == 0, kc == KC - 1,
                       [("HT", kc, t // 4), "rwb"], bk(7))
            cp("dve", lg, lp, bk(7), ["lg"])
            l3 = lg.rearrange("p (t e) -> p t e", e=NE)
            a3 = r1.rearrange("p (t e) -> p t e", e=NE)
            b3 = r2.rearrange("p (t e) -> p t e", e=NE)
            w3 = wgt.rearrange("p (t e) -> p t e", e=NE)
            m1b = r3.unsqueeze(2).broadcast_to([128, NT, NE])
            m2b = r4.unsqueeze(2).broadcast_to([128, NT, NE])
            P.op("dve", lambda e: e.tensor_reduce(out=r3, in_=l3, axis=AX.X, op=ALU.max), ["lg"], ["r3"])
            tt("dve", a3, l3, m1b, ALU.is_equal, ["lg", "r3"], ["r1"])
            stt(b3, a3, -1e30, l3, ALU.mult, ALU.add, ["r1", "lg"], ["r2"])
            P.op("dve", lambda e: e.tensor_reduce(out=r4, in_=b3, axis=AX.X, op=ALU.max), ["r2"], ["r4"])
            tt("dve", a3, l3, m2b, ALU.is_ge, ["lg", "r4", "r1"], ["r1"])
            tt("dve", b3, l3, m1b, ALU.subtract, ["lg", "r3", "r2"], ["r2"])
            act(r2, r2, AF.Exp, ["r2"], ["r2"])
            tt("dve", b3, b3, a3, ALU.mult, ["r2", "r1"], ["r2"])
            P.op("dve", lambda e: e.tensor_reduce(out=r3, in_=b3, axis=AX.X, op=ALU.add), ["r2", "r3"], ["r3"])
            P.op("dve", lambda e: e.reciprocal(out=r3, in_=r3), ["r3"], ["r3"])
            tt("dve", w3, b3, m1b, ALU.mult, ["r2", "r3"], ["wgt"])

        def ck(name):
            if stop == name:
                raise StopBuild()

        def tap(name, ap, keys):
            if name not in tap_d:
                return
            dt_ = tap_d[name]
            P.dma("sp", 3, lambda e: e.dma_start(out=dt_, in_=ap), list(keys), [("dbg", "tap", name)])

        try:
            ck("consts")
            for l in range(DEPTH):
                compute_mod(l)
                tap(f"modT{l}", modT, ["modT"])
                ck(f"mod{l}")
                make_gbc(modT[:, 16:24], "modT")
                tap(f"gbc{l}", GBC, ["GBC"])
                ck(f"gbc{l}")
                make_hT(s1, modT[:, 0:8], ["s1", "modT"])
                tap(f"hT{l}", HT.rearrange("p k s -> p (k s)"), HT_ALL)
                ck(f"hT{l}")
                barrier()
                mixer(l)
                dump(f"mix{l}")
                ck(f"mix{l}")
                barrier()
                make_gbc(modT[:, 40:48], "modT")
                if l % 2 == 0:
                    experts = [(fg_d, fu_d, fd_d, None)]
                else:
                    experts = [(mg_d[e], mu_d[e], md_d[e], e) for e in range(NE)]
                ffn(experts, "pre")
                make_hT(s2, modT[:, 24:32], ["s2", "modT"])
                barrier()
                if l % 2 == 1:
                    router()
                ffn(experts, "main")
                dump(f"ffn{l}")
                ck(f"ffn{l}")
                barrier()
        except StopBuild:
            barrier()
        make_gbc(fng, "fng")
        yt = [carve(D, at=PH + 8000 + i * D) for i in range(2)]
        junk = carve(512, BF16, at=PH)
        for t in range(NT):
            act(junk, X[:, t, :], AF.Square, [("X", t)], ["junk", "ssf"], accum_out=ss[:, t:t + 1])
        ts("dve", rstd, ss, 1.0 / D, EPS, ALU.mult, ALU.add, ["ssf"], ["rstdf"])
        act(rstd, rstd, AF.Sqrt, ["rstdf"], ["rstdf"])
        P.op("dve", lambda e: e.reciprocal(out=rstd, in_=rstd), ["rstdf"], ["rstdf"])
        for t in range(NT):
            stt(yt[t % 2], X[:, t, :], rstd[:, t:t + 1], GBC, ALU.mult, ALU.mult, [("X", t), "rstdf", "GBC"], [("yt", t % 2)])
            P.dma("sp", 14 + t % 2, lambda e, t=t: e.dma_start(out=y_d[t * 128:(t + 1) * 128, :], in_=yt[t % 2]),
                  [("yt", t % 2)], [("y", t)])
        P.final_wait("sp", [("y", t) for t in range(NT)] + [k for k in P.lastw if isinstance(k, tuple) and k[0] == "dbg"])

        with nc.Block() as block:
            @block.sync
            def _(e):
                P.replay("sp", e)

            @block.tensor
            def _(e):
                P.replay("pe", e)

            @block.vector
            def _(e):
                P.replay("dve", e)

            @block.scalar
            def _(e):
                P.replay("act", e)

            @block.gpsimd
            def _(e):
                P.replay("pool", e)
    return nc


def host_consts():
    f = np.float32
    c = {}
    c["idn"] = np.eye(128, dtype=f)
    half = 64
    inv_freq = (10000.0 ** (-np.arange(half, dtype=np.float32) / half)).astype(f)
    pos = np.arange(S, dtype=np.float32)
    ang = (pos[:, None] * inv_freq[None, :]).astype(f)
    cos = np.cos(ang).astype(f).reshape(NT, 128, half).transpose(1, 0, 2).reshape(128, NT * half)
    sin = np.sin(ang).astype(f).reshape(NT, 128, half).transpose(1, 0, 2).reshape(128, NT * half)
    c["cos"] = np.ascontiguousarray(cos)
    c["sin"] = np.ascontiguousarray(sin)
    dec = np.zeros((128, 12), f)
    idx = np.arange(128, dtype=np.float64)
    for r in range(4):
        lgm = np.log(1.0 - 2.0 ** (-5.0 - r))
        dec[:, r * 3 + 0] = np.exp((idx + 1.0) * lgm)
        dec[:, r * 3 + 1] = np.exp(-(idx + 1.0) * lgm) * (128.0 ** -0.5)
        dec[:, r * 3 + 2] = np.exp((127.0 - idx) * lgm) * (128.0 ** -0.5)
    c["dec"] = dec
    kk = np.arange(128)
    c["m01"] = (kk[None, :] >= kk[:, None]).astype(f)
    c["mneg"] = np.where(kk[:, None] > kk[None, :], NEG, 0.0).astype(f)
    en = np.zeros((128, 8, 128), f)
    for n in range(8):
        en[n, n, :] = 1.0
        en[64 + n, n, :] = 1.0
    c["en"] = en.reshape(128, 8 * 128)
    past = np.zeros((2, NT, 8), f)
    nb = np.zeros((2, NT, 8), f)
    for t in range(NT):
        qb = t // 2
        for n in range(8):
            past[:, t, n] = 0.0 if n < qb else -1e30
            nb[:, t, n] = 0.0 if n == qb else NEG
    c["past"] = np.ascontiguousarray(np.broadcast_to(past.reshape(1, 256), (128, 256)))
    c["nb"] = np.ascontiguousarray(np.broadcast_to(nb.reshape(1, 256), (128, 256)))
    return c


def fm(v, n):
    return np.ascontiguousarray(np.asarray(v, np.float32).reshape(n, 128).T)


def prep_shared(inp):
    f = np.float32
    sh = dict(host_consts())
    sh["nmg"] = np.concatenate([fm(inp["norm_mix_g"][l], KC) for l in range(DEPTH)], axis=1)
    sh["nfg"] = np.concatenate([fm(inp["norm_ffn_g"][l], KC) for l in range(DEPTH)], axis=1)
    sh["aog"] = np.concatenate([fm(inp["att_out_g"][l], 4) for l in range(DEPTH)], axis=1)
    sh["rog"] = np.concatenate([fm(inp["ret_out_g"][l], 4) for l in range(DEPTH)], axis=1)
    sh["fng"] = fm(inp["final_norm_g"], KC)
    sh["adaw"] = np.ascontiguousarray(np.asarray(inp["ada_w"], f).reshape(DEPTH, KC, 128, 6 * D))
    sh["adabr"] = np.ascontiguousarray(np.asarray(inp["ada_b"], f))
    sh["adab"] = np.concatenate([fm(inp["ada_b"][l], 48) for l in range(DEPTH)], axis=1)
    cols = []
    for j in range(4):
        for w in range(3):
            cols += list(range(w * 512 + j * 128, w * 512 + (j + 1) * 128))
    for r in range(4):
        for w in range(4):
            cols += list(range(1536 + w * 512 + r * 128, 1536 + w * 512 + (r + 1) * 128))
    cols = np.array(cols)

    def pk(w):
        w = np.asarray(w, f)
        k = w.shape[0] // 128
        return np.ascontiguousarray(w.reshape(k, 128, w.shape[1]).transpose(1, 0, 2))
    sh["win"] = np.stack([pk(np.asarray(inp["w_in"][l], f)[:, cols]) for l in range(DEPTH)])
    sh["wout"] = np.stack([pk(inp["w_out"][l]) for l in range(DEPTH)])
    sh["fg"] = pk(inp["ffn_w_gate"][0])
    sh["fu"] = pk(inp["ffn_w_up"][0])
    sh["fd"] = pk(inp["ffn_w_down"][0])
    sh["rw"] = pk(inp["router_w"][0]).reshape(128, KC * NE)
    sh["mg"] = np.stack([pk(inp["moe_w_gate"][0][e]) for e in range(NE)])
    sh["mu"] = np.stack([pk(inp["moe_w_up"][0][e]) for e in range(NE)])
    sh["md"] = np.stack([pk(inp["moe_w_down"][0][e]) for e in range(NE)])
    return sh


def kernel(**inputs):
    inp = {k: np.asarray(v) for k, v in inputs.items()}
    sh = prep_shared(inp)
    nc = build_nc()
    in_maps = []
    for b in range(8):
        m = dict(sh)
        m["x"] = np.ascontiguousarray(inp["x"][b], dtype=np.float32)
        m["cT"] = fm(inp["c"][b], KC)
        in_maps.append(m)
    res = run_bass_kernel_spmd(nc, in_maps, core_ids=list(range(8)))
    return np.stack([np.asarray(r["y"], dtype=np.float32) for r in res.results], axis=0)
```

```python
import numpy as np
from contextlib import ExitStack
import concourse.bass as bass
import concourse.mybir as mybir
from concourse.bass_utils import run_bass_kernel_spmd
from concourse.alu_op_type import AluOpType as ALU

F32 = mybir.dt.float32
BF16 = mybir.dt.bfloat16
AF = mybir.ActivationFunctionType
AX = mybir.AxisListType

DEPTH = 2
S = 2048
D = 1024
NT = 16
KC = 8
DFF = 2816
NFC = 22
NE = 8
EPS = 1e-6
NEG = -240000.0
SAME_ENGINE_SYNC = True


class Prog:
    ENG = ["pe", "act", "dve", "pool", "sp"]

    def __init__(self, nc, sems, dma_sems):
        self.nc = nc
        self.items = {e: [] for e in self.ENG}
        self.sem = {e: sems[i] for i, e in enumerate(self.ENG)}
        self.cnt = {e: 0 for e in self.ENG}
        self.waited = {e: {} for e in self.ENG}
        self.lastw = {}
        self.readers = {}
        self.dma_sems = dma_sems
        self.dma_cnt = [0] * len(dma_sems)
        self.lock_last = {}

    @staticmethod
    def _locks(reads, writes):
        return {k[0] for k in list(reads) + list(writes)
                if isinstance(k, tuple) and len(k) == 2 and isinstance(k[0], int)}

    def _deps(self, eng, reads, writes):
        toks = {}

        def add(t):
            if t is None:
                return
            s, v = t
            k = id(s)
            if k not in toks or toks[k][1] < v:
                toks[k] = (s, v)
        for r in reads:
            add(self.lastw.get(r))
        for w in writes:
            add(self.lastw.get(w))
            for t in self.readers.get(w, {}).values():
                add(t)
        for b in self._locks(reads, writes):
            for e2, t in self.lock_last.get(b, {}).items():
                if e2 != eng:
                    add(t)
        out = []
        for k, (s, v) in toks.items():
            if s is self.sem[eng] and (eng == "pe" or not SAME_ENGINE_SYNC):
                continue
            if self.waited[eng].get(k, 0) < v:
                self.waited[eng][k] = v
                out.append((s, v))
        return out

    def _commit(self, tok, reads, writes):
        k = id(tok[0])
        for r in reads:
            d = self.readers.setdefault(r, {})
            if k not in d or d[k][1] < tok[1]:
                d[k] = tok
        for w in writes:
            self.lastw[w] = tok
            self.readers[w] = {}

    def op(self, eng, fn, reads=(), writes=()):
        for s, v in self._deps(eng, reads, writes):
            self.items[eng].append(("wait", s, v))
        self.cnt[eng] += 1
        self.items[eng].append(("op", fn, self.sem[eng], 1))
        tok = (self.sem[eng], self.cnt[eng])
        self._commit(tok, reads, writes)
        for b in self._locks(reads, writes):
            self.lock_last.setdefault(b, {})[eng] = tok

    def dma(self, eng, chan, fn, reads=(), writes=()):
        s = self.dma_sems[chan]
        deps = self._deps(eng, reads, writes)
        prev = self.dma_cnt[chan] * 16
        if prev and self.waited[eng].get(id(s), 0) < prev:
            self.waited[eng][id(s)] = prev
            deps.append((s, prev))
        for ss, v in deps:
            self.items[eng].append(("wait", ss, v))
        self.dma_cnt[chan] += 1
        self.items[eng].append(("op", fn, s, 16))
        self._commit((s, self.dma_cnt[chan] * 16), reads, writes)

    def final_wait(self, eng, keys):
        for s, v in self._deps(eng, keys, ()):
            self.items[eng].append(("wait", s, v))

    def replay(self, eng, e):
        for it in self.items[eng]:
            if it[0] == "wait":
                e.wait_ge(it[1], it[2])
            else:
                it[1](e).then_inc(it[2], it[3])


class StopBuild(Exception):
    pass


def build_nc(debug=(), stop=None, taps=()):
    nc = bass.Bass("TRN2", target_bir_lowering=False)

    def din(name, shape):
        return nc.dram_tensor(name, list(shape), F32, kind="ExternalInput").ap()
    x_d = din("x", [S, D])
    cT_d = din("cT", [128, KC])
    nmg_d = din("nmg", [128, DEPTH * KC])
    nfg_d = din("nfg", [128, DEPTH * KC])
    aog_d = din("aog", [128, DEPTH * 4])
    rog_d = din("rog", [128, DEPTH * 4])
    fng_d = din("fng", [128, KC])
    adaw_d = din("adaw", [DEPTH, KC, 128, 6 * D])
    adab_d = din("adab", [128, DEPTH * 48])
    adabr_d = din("adabr", [DEPTH, 6 * D])
    win_d = din("win", [DEPTH, 128, KC, 3584])
    wout_d = din("wout", [DEPTH, 128, KC, D])
    fg_d = din("fg", [128, KC, DFF])
    fu_d = din("fu", [128, KC, DFF])
    fd_d = din("fd", [128, NFC, D])
    rw_d = din("rw", [128, KC * NE])
    mg_d = din("mg", [NE, 128, KC, DFF])
    mu_d = din("mu", [NE, 128, KC, DFF])
    md_d = din("md", [NE, 128, NFC, D])
    idn_d = din("idn", [128, 128])
    cos_d = din("cos", [128, NT * 64])
    sin_d = din("sin", [128, NT * 64])
    dec_d = din("dec", [128, 12])
    m01_d = din("m01", [128, 128])
    mneg_d = din("mneg", [128, 128])
    en_d = din("en", [128, 8 * 128])
    past_d = din("past", [128, 256])
    nb_d = din("nb", [128, 256])
    y_d = nc.dram_tensor("y", [S, D], F32, kind="ExternalOutput").ap()
    tap_d = {}
    for name, (shp, dt_) in dict(taps).items():
        tap_d[name] = nc.dram_tensor("tap_" + name, list(shp), dt_, kind="ExternalOutput").ap()
    dbg_d = {}
    for name in debug:
        dbg_d[name] = nc.dram_tensor("dbg_" + name, [S, D], F32, kind="ExternalOutput").ap()

    with ExitStack() as st:
        TOT = 52800
        A = st.enter_context(nc.sbuf_tensor("arena", [128, TOT], F32))
        banks = [st.enter_context(nc.psum_tensor(f"bank{i}", [128, 512], F32)) for i in range(8)]
        sems = [st.enter_context(nc.semaphore(f"es{i}")) for i in range(5)]
        NCH = 24
        dsems = [st.enter_context(nc.semaphore(f"ds{i}")) for i in range(NCH)]
        P = Prog(nc, sems, dsems)

        cur = [0]

        def carve(n, dt=F32, at=None):
            if at is None:
                off = cur[0]
                cur[0] += n
            else:
                off = at
            assert off + n <= TOT, (off, n)
            ap = A[:, off:off + n]
            if dt != F32:
                ap = ap.bitcast(dt)
            return ap

        X = carve(NT * D).rearrange("p (t d) -> p t d", d=D)
        HT = carve(KC * S // 2, BF16).rearrange("p (k s) -> p k s", s=S)
        identb = carve(64, BF16)
        identf = carve(128)
        onesf = carve(128)
        cosT = carve(NT * 64).rearrange("p (t c) -> p t c", c=64)
        sinT = carve(NT * 64).rearrange("p (t c) -> p t c", c=64)
        dec = carve(12)
        m01 = carve(64, BF16)
        mneg = carve(64, BF16)
        EN = carve(512, BF16).rearrange("p (n k) -> p n k", k=128)
        pastm = carve(256)
        NB = carve(256)
        modT = carve(48)
        adab = carve(DEPTH * 48)
        nmg = carve(DEPTH * KC)
        nfg = carve(DEPTH * KC)
        aog = carve(DEPTH * 4)
        rog = carve(DEPTH * 4)
        fng = carve(KC)
        s1 = carve(KC)
        s2 = carve(KC)
        GBC = carve(D)
        cTt = carve(KC)
        csb = carve(KC // 2, BF16)
        csf = carve(KC)
        ss = carve(NT)
        rstd = carve(NT)
        tmp16 = carve(NT)
        ssa = carve(4 * NT)
        rstda = carve(NT)
        rwb = carve(KC * NE // 2, BF16).rearrange("p (k e) -> p k e", e=NE)
        lg = carve(NT * NE)
        wgt = carve(NT * NE)
        r1 = carve(NT * NE)
        r2 = carve(NT * NE)
        r3 = carve(NT)
        r4 = carve(NT)
        PH = cur[0]
        PHN = TOT - PH

        def bankv(b, n, dt=F32, off=0):
            ap = banks[b][:, off:off + n]
            if dt != F32:
                ap = ap.bitcast(dt)
            return ap

        def bk(b):
            return [(b, 0), (b, 1)]

        def mm(out, lhsT, rhs, start, stop, reads, writes, sgc=False):
            P.op("pe", lambda e: e.matmul(out, lhsT=lhsT, rhs=rhs, start=start, stop=stop, skip_group_check=sgc),
                 reads, writes)

        def tr(out, in_, reads, writes):
            P.op("pe", lambda e: e.transpose(out, in_, identb), list(reads) + ["identb"], writes)

        def act(out, in_, func, reads, writes, **kw):
            P.op("act", lambda e: e.activation(out=out, in_=in_, func=func, **kw), reads, writes)

        def tt(eng, out, in0, in1, op, reads, writes):
            P.op(eng, lambda e: e.tensor_tensor(out=out, in0=in0, in1=in1, op=op), reads, writes)

        def ts(eng, out, in0, s1_, s2_, op0, op1, reads, writes):
            if op1 is None:
                P.op(eng, lambda e: e.tensor_scalar(out=out, in0=in0, scalar1=s1_, scalar2=None, op0=op0), reads, writes)
            else:
                P.op(eng, lambda e: e.tensor_scalar(out=out, in0=in0, scalar1=s1_, scalar2=s2_, op0=op0, op1=op1), reads, writes)

        def stt(out, in0, scalar, in1, op0, op1, reads, writes):
            P.op("dve", lambda e: e.scalar_tensor_tensor(out=out, in0=in0, scalar=scalar, in1=in1, op0=op0, op1=op1),
                 reads, writes)

        def cp(eng, out, in_, reads, writes):
            if eng == "act":
                act(out, in_, AF.Copy, reads, writes)
            else:
                P.op(eng, lambda e: e.tensor_copy(out=out, in_=in_), reads, writes)

        cch = [0]

        def cload(out, in_, key, eng="sp"):
            ch = 16 + (cch[0] % 4)
            cch[0] += 1
            P.dma(eng, ch, lambda e: e.dma_start(out=out, in_=in_), (), [key])

        def castload(chan, out, in_, reads, writes):
            P.dma("pool", chan, lambda e: e.dma_start(out=out, in_=in_, max_dma_last_dim=4096), reads, writes)

        cload(identf, idn_d[:, :], "identf")
        cload(cosT, cos_d[:, :].rearrange("p (t c) -> p t c", c=64), "cos")
        cload(sinT, sin_d[:, :].rearrange("p (t c) -> p t c", c=64), "sin")
        cload(dec, dec_d[:, :], "dec")
        cload(pastm, past_d[:, :], "past")
        cload(NB, nb_d[:, :], "NB")
        cload(adab, adab_d[:, :], "adab")
        cload(nmg, nmg_d[:, :], "nmg")
        cload(nfg, nfg_d[:, :], "nfg")
        cload(aog, aog_d[:, :], "aog")
        cload(rog, rog_d[:, :], "rog")
        cload(fng, fng_d[:, :], "fng")
        cload(cTt, cT_d[:, :], "cT")
        castload(20, identb, idn_d[:, :], (), ["identb"])
        castload(21, m01, m01_d[:, :], (), ["m01"])
        castload(22, mneg, mneg_d[:, :], (), ["mneg"])
        castload(23, EN.rearrange("p n k -> p (n k)"), en_d[:, :], (), ["EN"])
        castload(20, rwb.rearrange("p k e -> p (k e)"), rw_d[:, :], (), ["rwb"])
        P.op("dve", lambda e: e.memset(onesf, 1.0), (), ["onesf"])
        for t4 in range(4):
            P.dma("sp", t4, lambda e, t4=t4: e.dma_start(
                out=X[:, 4 * t4:4 * t4 + 4, :],
                in_=x_d[512 * t4:512 * (t4 + 1), :].rearrange("(t p) d -> p t d", p=128)),
                (), [("X", 4 * t4 + i) for i in range(4)])
        act(csb, cTt, AF.Silu, ["cT"], ["csb"])
        act(csf, cTt, AF.Silu, ["cT"], ["csf"])

        def barrier():
            alle = ["pe", "act", "dve", "pool", "sp"]
            for e in alle:
                for o in alle:
                    if o == e or P.cnt[o] == 0:
                        continue
                    k = id(P.sem[o])
                    if P.waited[e].get(k, 0) < P.cnt[o]:
                        P.waited[e][k] = P.cnt[o]
                        P.items[e].append(("wait", P.sem[o], P.cnt[o]))
                for ch in range(NCH):
                    v = P.dma_cnt[ch] * 16
                    k = id(P.dma_sems[ch])
                    if v and P.waited[e].get(k, 0) < v:
                        P.waited[e][k] = v
                        P.items[e].append(("wait", P.dma_sems[ch], v))

        def dump(name):
            if name in dbg_d:
                for t4 in range(4):
                    P.dma("sp", 2 + (t4 % 2), lambda e, t4=t4: e.dma_start(
                        out=dbg_d[name][512 * t4:512 * (t4 + 1), :].rearrange("(t p) d -> p t d", p=128),
                        in_=X[:, 4 * t4:4 * t4 + 4, :]),
                        [("X", 4 * t4 + i) for i in range(4)], [("dbg", name, t4)])

        def compute_mod(l):
            ADA = [carve(3072, at=PH + 3000 + i * 3072) for i in range(2)]
            dtmp = carve(2048, at=PH + 9200)
            csrep = carve(KC * 128, at=PH + 11300).rearrange("p (k m) -> p k m", m=128)
            for kc in range(KC):
                ts("dve", csrep[:, kc, :], onesf, csf[:, kc:kc + 1], None, ALU.mult, None, ["onesf", "csf"], ["csrep"])
            it = 0
            for hf in range(2):
                for kc in range(KC):
                    sl = it % 2
                    it += 1
                    P.dma("sp", 4 + sl, lambda e, sl=sl, kc=kc, hf=hf: e.dma_start(
                        out=ADA[sl], in_=adaw_d[l, kc, :, hf * 3072:(hf + 1) * 3072]), (), [("ADA", sl)])
                    for j in range(6):
                        mm(bankv(j, 512), csrep[:, kc, :], ADA[sl][:, j * 512:(j + 1) * 512], kc == 0, kc == KC - 1,
                           [("ADA", sl), "csrep"], bk(j))
                for j in range(6):
                    d3 = dtmp[:, (j % 4) * 512:(j % 4 + 1) * 512].rearrange("p (a b) -> p a b", b=128)
                    tt("dve", d3, bankv(j, 512).rearrange("p (a b) -> p a b", b=128),
                       identf.unsqueeze(1).broadcast_to([128, 4, 128]), ALU.mult, bk(j) + ["identf"], [("dtmp", j % 4)])
                    c0 = hf * 24 + j * 4
                    P.op("dve", lambda e, d3=d3, c0=c0: e.tensor_reduce(out=modT[:, c0:c0 + 4], in_=d3, axis=AX.X, op=ALU.add),
                         [("dtmp", j % 4)], ["modT"])
            tt("dve", modT, modT, adab[:, l * 48:(l + 1) * 48], ALU.add, ["modT", "adab"], ["modT"])
            stt(s1, modT[:, 8:16], 1.0, nmg[:, l * KC:(l + 1) * KC], ALU.add, ALU.mult, ["modT", "nmg"], ["s1"])
            stt(s2, modT[:, 32:40], 1.0, nfg[:, l * KC:(l + 1) * KC], ALU.add, ALU.mult, ["modT", "nfg"], ["s2"])

        def make_gbc(gT, gkey):
            diag = carve(2 * 128, at=PH + 2600).rearrange("p (a b) -> p a b", b=128)
            for kc in range(KC):
                sl = kc % 2
                ts("dve", diag[:, sl, :], identf, gT[:, kc:kc + 1], None, ALU.mult, None,
                   ["identf", gkey], [("diag", sl)])
                b = 5 + kc // 4
                mm(bankv(b, 128, off=(kc % 4) * 128), onesf, diag[:, sl, :], True, True,
                   ["onesf", ("diag", sl)], bk(b))
            for h in range(2):
                cp("dve", GBC[:, h * 512:(h + 1) * 512], bankv(5 + h, 512), bk(5 + h), ["GBC"])

        def make_hT(sT, shT, skeys):
            junk = carve(512, BF16, at=PH)
            xn = carve(2048, BF16, at=PH + 512).rearrange("p (i d) -> p i d", d=D)
            for tq in range(4):
                for i in range(4):
                    t = tq * 4 + i
                    act(junk, X[:, t, :], AF.Square, [("X", t)], ["junk", ("ss", tq)], accum_out=ss[:, t:t + 1])
                q = slice(tq * 4, tq * 4 + 4)
                ts("dve", tmp16[:, q], ss[:, q], 1.0 / D, EPS, ALU.mult, ALU.add, [("ss", tq)], [("tmp16", tq)])
                act(tmp16[:, q], tmp16[:, q], AF.Sqrt, [("tmp16", tq)], [("tmp16", tq)])
                P.op("dve", lambda e, q=q: e.reciprocal(out=rstd[:, q], in_=tmp16[:, q]), [("tmp16", tq)], [("rstd", tq)])
                for i in range(4):
                    t = tq * 4 + i
                    if i % 2 == 0:
                        act(xn[:, i, :], X[:, t, :], AF.Identity, [("X", t), ("rstd", tq)], [("xn", i)], scale=rstd[:, t:t + 1])
                    else:
                        ts("pool", xn[:, i, :], X[:, t, :], rstd[:, t:t + 1], 0.0, ALU.mult, ALU.add,
                           [("X", t), ("rstd", tq)], [("xn", i)])
                for kc in range(KC):
                    b = kc % 4
                    pT = bankv(b, 256, BF16)
                    for i in range(4):
                        tr(pT[:, i * 128:(i + 1) * 128], xn[:, i, kc * 128:(kc + 1) * 128], [("xn", i)], bk(b))
                    o = HT[:, kc, tq * 512:(tq + 1) * 512]
                    if kc % 2 == 0:
                        act(o, pT, AF.Identity, bk(b) + skeys, [("HT", kc, tq)],
                            scale=sT[:, kc:kc + 1], bias=shT[:, kc:kc + 1])
                    else:
                        ts("dve", o, pT, sT[:, kc:kc + 1], shT[:, kc:kc + 1], ALU.mult, ALU.add,
                           bk(b) + skeys, [("HT", kc, tq)])

        HT_ALL = [("HT", kc, tq) for kc in range(KC) for tq in range(4)]

        def HTk(tq):
            return [("HT", kc, tq) for kc in range(KC)]

        def mixer(l):
            o = PH
            CT = carve(8192, BF16, at=o).rearrange("p (k s) -> p k s", s=S); o += 8192
            WIN = [carve(2048, BF16, at=o + i * 2048).rearrange("p (k c) -> p k c", c=512) for i in range(2)]
            WO = carve(4096, BF16, at=o).rearrange("p (k c) -> p k c", c=D); o += 4096
            U = o

            def load_unit(u):
                sl = u % 2
                if u < 4:
                    castload(6 + sl, WIN[sl][:, :, 0:384], win_d[l, :, :, u * 384:(u + 1) * 384], (), [("WIN", sl)])
                else:
                    r = u - 4
                    castload(6 + sl, WIN[sl][:, :, :], win_d[l, :, :, 1536 + r * 512:1536 + (r + 1) * 512], (), [("WIN", sl)])

            def att_unit(j):
                sl = j % 2
                W = WIN[sl]
                o = U
                QZ = [carve(1024, BF16, at=o + h * 1024) for h in range(2)]; o += 2048
                KZ = [carve(1024, BF16, at=o + h * 1024) for h in range(2)]; o += 2048
                VA = carve(1040, BF16, at=o).rearrange("p (t h c) -> p t h c", h=2, c=65); o += 1040
                bias2 = carve(1024, BF16, at=o).rearrange("p (t c) -> p t c", c=128); o += 1024
                PT = [carve(128, BF16, at=o + i * 128) for i in range(8)]; o += 1024
                otok = carve(1024, BF16, at=o).rearrange("p (t c) -> p t c", c=128); o += 1024
                gm = carve(256, at=o); o += 256
                cmp = carve(1024, at=o); o += 1024
                cnt = carve(256, at=o); o += 256
                kms = carve(8, at=o); o += 8
                kmbz = [carve(4, BF16, at=o + h * 4) for h in range(2)]; o += 8
                rec = carve(2, at=o); o += 2
                recb = carve(2, at=o); o += 2
                sq = carve(64, BF16, at=o); o += 64
                assert o <= TOT, o
                wk = ("WIN", sl)
                own = [slice(0, 64), slice(64, 128)]
                oth = [slice(64, 128), slice(0, 64)]
                aug = [slice(64, 72), slice(0, 8)]
                for h in range(2):
                    P.op("pool", lambda e, h=h: e.memset(QZ[h][oth[h], :], 0.0), (), [("QZz", h, 0), ("QZz", h, 1)])
                    P.op("pool", lambda e, h=h: e.memset(KZ[h][oth[h], :], 0.0), (), [("KZz", h)])
                    P.op("pool", lambda e, h=h: e.tensor_copy(
                        out=KZ[h][aug[h], :].rearrange("p (n r k) -> p n r k", r=2, k=128),
                        in_=EN[aug[h], :, :].unsqueeze(2).broadcast_to([8, 8, 2, 128])), ["EN", ("KZz", h)], [("KZz", h)])
                    P.op("pool", lambda e, h=h: e.memset(kmbz[h], 0.0), (), [("kmbz", h)])
                for which, dst, nm in ((0, QZ, "QZ"), (1, KZ, "KZ")):
                    for tc in range(4):
                        b = tc % 2
                        for kc in range(KC):
                            mm(bankv(b, 512), W[:, kc, which * 128:(which + 1) * 128], HT[:, kc, tc * 512:(tc + 1) * 512],
                               kc == 0, kc == KC - 1, [wk, ("HT", kc, tc)], bk(b))
                        for h in range(2):
                            cp("act" if h == 0 else "dve", dst[h][own[h], tc * 512:(tc + 1) * 512], bankv(b, 512)[own[h], :],
                               bk(b), [(nm, h, tc)])
                P.op("pool", lambda e: e.memset(VA[:, :, :, 64:65], 1.0), (), ["VAone"])
                for tq in range(4):
                    b = 2 + tq % 2
                    for i in range(4):
                        t = tq * 4 + i
                        for kc in range(KC):
                            mm(bankv(b, 128, off=i * 128), HT[:, kc, t * 128:(t + 1) * 128], W[:, kc, 256:384],
                               kc == 0, kc == KC - 1, [wk, ("HT", kc, tq)], bk(b))
                    cp("act" if tq % 2 == 0 else "dve", VA[:, tq * 4:tq * 4 + 4, :, 0:64],
                       bankv(b, 512).rearrange("p (t h c) -> p t h c", h=2, c=64), bk(b), [("VA", tq)])
                P.op("pool", lambda e: e.memset(bias2, 0.0), (), ["bias2"])
                for h in range(2):
                    P.op("dve", lambda e, h=h: e.tensor_reduce(out=kms[own[h], :], in_=KZ[h][own[h], :].rearrange("p (n k) -> p n k", k=256),
                                                              axis=AX.X, op=ALU.add),
                         [("KZ", h, tc) for tc in range(4)], [("kms", h)])
                    cp("dve", kmbz[h][own[h], :], kms[own[h], :], [("kms", h), ("kmbz", h)], [("kmbz", h)])
                    gp = bankv(h, 128)
                    for t in range(NT):
                        mm(gp[:, t * 8:t * 8 + 8], QZ[h][:, t * 128:(t + 1) * 128], kmbz[h],
                           True, True, [("QZ", h, t // 4), ("QZz", h, t // 8), ("kmbz", h)], bk(h))
                    tt("dve", gm[:, h * 128:(h + 1) * 128], gp, pastm[:, h * 128:(h + 1) * 128], ALU.add,
                       bk(h) + ["past"], [("gm", h)])
                    g3 = gm[:, h * 128:(h + 1) * 128].rearrange("p (g n) -> p g n", n=8)
                    cmp4 = cmp.rearrange("p (g n m) -> p g n m", n=8, m=8)
                    tt("dve", cmp4, g3.unsqueeze(2).broadcast_to([128, 16, 8, 8]), g3.unsqueeze(3).broadcast_to([128, 16, 8, 8]),
                       ALU.is_gt, [("gm", h)], ["cmp"])
                    P.op("dve", lambda e, h=h, cmp4=cmp4: e.tensor_reduce(
                        out=cnt[:, h * 128:(h + 1) * 128].rearrange("p (g n) -> p g n", n=8), in_=cmp4, axis=AX.X, op=ALU.add),
                        ["cmp"], [("cnt", h)])
                    c0 = 64 if h == 0 else 0
                    stt(bias2[:, :, c0:c0 + 8], cnt[:, h * 128:(h + 1) * 128].rearrange("p (t n) -> p t n", n=8), 2.5,
                        NB[:, h * 128:(h + 1) * 128].rearrange("p (t n) -> p t n", n=8), ALU.is_gt, ALU.mult,
                        [("cnt", h), "NB", "bias2"], ["bias2"])

                def emit_bias_T():
                    for tq in range(2, 4):
                        b = tq % 2
                        pT = bankv(b, 256, BF16)
                        for i in range(4):
                            t = tq * 4 + i
                            tr(pT[:, i * 128:(i + 1) * 128], bias2[:, t, :], ["bias2"], bk(b))
                        for h in range(2):
                            cp("act", QZ[h][aug[h], tq * 512:(tq + 1) * 512], pT[aug[h], :], bk(b), [("QZz", h, 1)])

                def emit_tail(qb):
                    for t in (2 * qb, 2 * qb + 1):
                        act(sq, otok[:, t, :], AF.Square, [("otok", t, 0), ("otok", t, 1)], ["sq", ("ssa", j)],
                            accum_out=ssa[:, j * NT + t:j * NT + t + 1])
                    if qb % 2 == 1:
                        tq = qb // 2
                        b = tq % 2
                        pT = bankv(b, 256, BF16)
                        for i in range(4):
                            t = tq * 4 + i
                            tr(pT[:, i * 128:(i + 1) * 128], otok[:, t, :], [("otok", t, 0), ("otok", t, 1)], bk(b))
                        ts("dve", CT[:, j, tq * 512:(tq + 1) * 512], pT, aog[:, l * 4 + j:l * 4 + j + 1], None, ALU.mult, None,
                           bk(b) + ["aog"], [("CT", j, tq)])

                early = [(h, qb, kt) for h in range(2) for qb in range(4) for kt in range(2 * qb + 2)]
                late = [(h, qb, kt) for h in range(2) for qb in range(4, 8) for kt in range(2 * qb + 2)]
                tiles = early + late
                rec2 = [rec, recb]

                def emit_st(i):
                    h, qb, kt = tiles[i]
                    hr = slice(h * 64, (h + 1) * 64)
                    slot = i % 8
                    sb_ = 2 + (i % 4)
                    hb_ = (i // 4) % 2
                    ST = bankv(sb_, 256, off=hb_ * 256)
                    skey = (sb_, hb_)
                    qlo = 128 if kt == 2 * qb + 1 else 0
                    qs = slice(qb * 256 + qlo, qb * 256 + 256)
                    diag = kt >= 2 * qb
                    mm(ST[:, qlo:256], KZ[h][:, kt * 128:(kt + 1) * 128], QZ[h][:, qs], True, not diag,
                       [("KZ", h, kt // 4), ("KZz", h), ("QZ", h, qb // 2), ("QZz", h, qb // 4)], [skey])
                    if diag:
                        dq0 = 0 if kt == 2 * qb else 128
                        mm(ST[:, dq0:dq0 + 128], identb, mneg, False, True, ["identb", "mneg"], [skey])
                    act(PT[slot][:, qlo:256], ST[:, qlo:256], AF.Exp, [skey], [("PT", slot)], scale=0.125)

                def emit_pv(i):
                    h, qb, kt = tiles[i]
                    slot = i % 8
                    pt = PT[slot]
                    ob = 6 + (h * 8 + qb) % 2
                    O = bankv(ob, 130).rearrange("p (q c) -> p q c", c=65)
                    nkt = 2 * qb + 2
                    qlo = 128 if kt == 2 * qb + 1 else 0
                    for qi in range(2):
                        if qi * 128 < qlo:
                            continue
                        mm(O[:, qi, :], pt[:, qi * 128:(qi + 1) * 128], VA[:, kt, h, :], (kt == 0 and qi == 0),
                           (kt == 2 * qb + qi), [("PT", slot), ("VA", kt // 4), "VAone"], bk(ob), sgc=True)
                    if kt == nkt - 1:
                        rc = rec2[ob % 2]
                        rk = ("rec", ob % 2)
                        P.op("dve", lambda e, O=O, rc=rc: e.reciprocal(out=rc.rearrange("p (q c) -> p q c", c=1), in_=O[:, :, 64:65]),
                             bk(ob), [rk])
                        for qi in range(2):
                            t = qb * 2 + qi
                            ts("dve", otok[:, t, h * 64:(h + 1) * 64], O[:, qi, 0:64], rc[:, qi:qi + 1], None,
                               ALU.mult, None, bk(ob) + [rk], [("otok", t, h)])
                        if h == 1:
                            emit_tail(qb)

                LAG = 4
                for i in range(len(tiles) + LAG):
                    if i == len(early):
                        emit_bias_T()
                    if i < len(tiles):
                        emit_st(i)
                    if i >= LAG:
                        emit_pv(i - LAG)

            def ret_unit(r):
                u = 4 + r
                sl = u % 2
                W = WIN[sl]
                wk = ("WIN", sl)
                o = U
                raS = [carve(256, at=o + i * 256) for i in range(2)]; o += 512
                rbS = [carve(256, at=o + i * 256) for i in range(2)]; o += 512
                rrS = [carve(256, at=o + i * 256) for i in range(2)]; o += 512
                qd = carve(256, BF16, at=o).rearrange("p (i c) -> p i c", c=128); o += 256
                ki = carve(256, BF16, at=o).rearrange("p (i c) -> p i c", c=128); o += 256
                kd = carve(1024, BF16, at=o).rearrange("p (t c) -> p t c", c=128); o += 1024
                vr = carve(1024, BF16, at=o).rearrange("p (t c) -> p t c", c=128); o += 1024
                sg = carve(1024, BF16, at=o).rearrange("p (t c) -> p t c", c=128); o += 1024
                qdT = carve(1024, BF16, at=o); o += 1024
                kiT = carve(1024, BF16, at=o); o += 1024
                PTr = [carve(64, BF16, at=o + i * 64) for i in range(2)]; o += 128
                stf = [carve(128, at=o + i * 128) for i in range(2)]; o += 256
                stb = [carve(64, BF16, at=o + i * 64) for i in range(2)]; o += 128
                Of = carve(2048, at=o).rearrange("p (t c) -> p t c", c=128); o += 2048
                on = [carve(64, BF16, at=o + i * 64) for i in range(2)]; o += 128
                sqr = carve(64, BF16, at=o); o += 64
                ssr = carve(NT, at=o); o += NT
                rsr = carve(NT, at=o); o += NT
                assert o <= TOT
                dq = dec[:, r * 3 + 0:r * 3 + 1]
                dki = dec[:, r * 3 + 1:r * 3 + 2]
                dkd = dec[:, r * 3 + 2:r * 3 + 3]
                gamma_c = float((1.0 - 2.0 ** (-5.0 - r)) ** 128)
                def emit_inproj_tile(t):
                    tq, i = t // 4, t % 4
                    b = t % 2
                    pp = bankv(b, 512)
                    for kc in range(KC):
                        mm(pp, HT[:, kc, t * 128:(t + 1) * 128], W[:, kc, :], kc == 0, kc == KC - 1,
                           [wk, ("HT", kc, tq)], bk(b))
                    bkk = bk(b)
                    ra, rb, rr = raS[t % 2], rbS[t % 2], rrS[t % 2]
                    p2 = t % 2
                    QK = pp[:, 0:256].rearrange("p (a h c) -> p a h c", a=2, h=2)
                    ra4 = ra.rearrange("p (a h c) -> p a h c", a=2, h=2)
                    rb3 = rb.rearrange("p (a h c) -> p a h c", a=2, h=2)
                    rr4 = rr.rearrange("p (a h c) -> p a h c", a=2, h=2)
                    cb4 = cosT[:, t, :].unsqueeze(1).unsqueeze(1).broadcast_to([128, 2, 2, 64])
                    sb3 = sinT[:, t, :].unsqueeze(1).broadcast_to([128, 2, 64])
                    tt("dve", ra4, QK, cb4, ALU.mult, bkk + ["cos"], [("ra", p2)])
                    tt("dve", rb3[:, :, 0, :], QK[:, :, 1, :], sb3, ALU.mult, bkk + ["sin"], [("rb0", p2)])
                    tt("dve", rb3[:, :, 1, :], QK[:, :, 0, :], sb3, ALU.mult, bkk + ["sin"], [("rb1", p2)])
                    tt("dve", rr4[:, :, 0, :], ra4[:, :, 0, :], rb3[:, :, 0, :], ALU.subtract, [("ra", p2), ("rb0", p2)], [("rr0", p2)])
                    tt("dve", rr4[:, :, 1, :], ra4[:, :, 1, :], rb3[:, :, 1, :], ALU.add, [("ra", p2), ("rb1", p2)], [("rr1", p2)])
                    rk = [("rr0", p2), ("rr1", p2)]
                    ts("pool", qd[:, i, :], rr[:, 0:128], dq, 0.0, ALU.mult, ALU.add, rk + ["dec"], [("qd", i)])
                    ts("pool", ki[:, i, :], rr[:, 128:256], dki, 0.0, ALU.mult, ALU.add, rk + ["dec"], [("ki", i)])
                    ts("pool", kd[:, t, :], rr[:, 128:256], dkd, 0.0, ALU.mult, ALU.add, rk + ["dec"], [("kd", t)])
                    cp("act", vr[:, t, :], pp[:, 256:384], bkk, [("vr", t)])
                    act(sg[:, t, :], pp[:, 384:512], AF.Silu, bkk, [("sg", t)])
                    if i == 3:
                        for src, dst, nm in ((qd, qdT, "qdT"), (ki, kiT, "kiT")):
                            bb = 0 if nm == "qdT" else 1
                            pT = bankv(bb, 256, BF16)
                            for ii in range(4):
                                tr(pT[:, ii * 128:(ii + 1) * 128], src[:, ii, :], [(nm[:2], ii)], bk(bb))
                            cp("act" if nm == "qdT" else "dve", dst[:, tq * 512:(tq + 1) * 512], pT, bk(bb), [(nm, tq)])

                for t in range(4):
                    emit_inproj_tile(t)
                def emit_sc(n):
                    cs_ = slice(n * 128, (n + 1) * 128)
                    sl2 = n % 2
                    SC = bankv(2 + sl2, 128)
                    mm(SC, kiT[:, cs_], qdT[:, cs_], True, True, [("kiT", n // 4), ("qdT", n // 4)], [(2 + sl2, 0)])
                    tt("dve", PTr[sl2], SC, m01, ALU.mult, [(2 + sl2, 0), "m01"], [("PTr", sl2)])
                    if n < NT - 1:
                        KV = bankv(6 + sl2, 128)
                        mm(KV, kd[:, n, :], vr[:, n, :], True, True, [("kd", n), ("vr", n)], [(6 + sl2, 0)])

                emit_sc(0)
                for n in range(NT):
                    cs_ = slice(n * 128, (n + 1) * 128)
                    sl2 = n % 2
                    if n + 4 < NT:
                        emit_inproj_tile(n + 4)
                    if n + 1 < NT:
                        emit_sc(n + 1)
                    OB = bankv(4 + sl2, 128)
                    mm(OB, PTr[sl2], vr[:, n, :], True, n == 0, [("PTr", sl2), ("vr", n)], [(4 + sl2, 0)])
                    if n > 0:
                        mm(OB, qdT[:, cs_], stb[(n - 1) % 2], False, True, [("qdT", n // 4), ("stb", (n - 1) % 2)],
                           [(4 + sl2, 0)])
                    if n < NT - 1:
                        KV = bankv(6 + sl2, 128)
                        if n == 0:
                            cp("dve", stf[0], KV, [(6 + sl2, 0)], [("stf", 0)])
                        else:
                            stt(stf[n % 2], stf[(n - 1) % 2], gamma_c, KV, ALU.mult, ALU.add,
                                [("stf", (n - 1) % 2), (6 + sl2, 0)], [("stf", n % 2)])
                        cp("pool", stb[n % 2], stf[n % 2], [("stf", n % 2)], [("stb", n % 2)])
                    cp("act", Of[:, n, :], OB, [(4 + sl2, 0)], [("Of", n)])
                    act(sqr, OB, AF.Square, [(4 + sl2, 0)], ["sqr", "ssr"], accum_out=ssr[:, n:n + 1])
                ts("dve", rsr, ssr, 1.0 / 128, EPS, ALU.mult, ALU.add, ["ssr"], ["rsr"])
                act(rsr, rsr, AF.Sqrt, ["rsr"], ["rsr"])
                P.op("dve", lambda e: e.reciprocal(out=rsr, in_=rsr), ["rsr"], ["rsr"])
                for tq in range(4):
                    b = 6 + tq % 2
                    pT = bankv(b, 256, BF16)
                    for i in range(4):
                        t = tq * 4 + i
                        stt(on[t % 2], Of[:, t, :], rsr[:, t:t + 1], sg[:, t, :], ALU.mult, ALU.mult,
                            [("Of", t), "rsr", ("sg", t)], [("on", t % 2)])
                        tr(pT[:, i * 128:(i + 1) * 128], on[t % 2], [("on", t % 2)], bk(b))
                    ts("dve", CT[:, 4 + r, tq * 512:(tq + 1) * 512], pT, rog[:, l * 4 + r:l * 4 + r + 1], None, ALU.mult, None,
                       bk(b) + ["rog"], [("CT", 4 + r, tq)])

            load_unit(0)
            for u in range(8):
                if u + 1 < 8:
                    load_unit(u + 1)
                if u < 4:
                    att_unit(u)
                else:
                    ret_unit(u - 4)
                tap(f"CT{u}_{l}", CT[:, u, :], [("CT", u, tq) for tq in range(4)])
                ck(f"u{u}_{l}")
            castload(6, WO[:, 0:4, :], wout_d[l, :, 0:4, :], (), [("WIN", 0)])
            castload(7, WO[:, 4:8, :], wout_d[l, :, 4:8, :], (), [("WIN", 1)])
            for h in range(2):
                tt("pool", WO[:, 4 * h:4 * h + 4, :], WO[:, 4 * h:4 * h + 4, :], GBC.unsqueeze(1).broadcast_to([128, 4, D]), ALU.mult,
                   [("WIN", h), "GBC"], [("WIN", h)])
            P.op("dve", lambda e: e.tensor_reduce(out=rstda, in_=ssa.rearrange("p (u t) -> p t u", t=NT), axis=AX.X, op=ALU.add),
                 [("ssa", j) for j in range(4)], ["rstda"])
            ts("dve", rstda, rstda, 1.0 / 512, EPS, ALU.mult, ALU.add, ["rstda"], ["rstda"])
            act(rstda, rstda, AF.Sqrt, ["rstda"], ["rstda"])
            P.op("dve", lambda e: e.reciprocal(out=rstda, in_=rstda), ["rstda"], ["rstda"])
            bi = 0
            for t in range(NT):
                for hf in range(2):
                    xs = X[:, t, hf * 512:(hf + 1) * 512]
                    b = bi % 4; bi += 1
                    for kc in range(4):
                        mm(bankv(b, 512), CT[:, kc, t * 128:(t + 1) * 128], WO[:, kc, hf * 512:(hf + 1) * 512],
                           kc == 0, kc == 3, [("CT", kc, t // 4), ("WIN", 0)], bk(b))
                    stt(xs, bankv(b, 512), rstda[:, t:t + 1], xs, ALU.mult, ALU.add, bk(b) + ["rstda", ("X", t)], [("X", t)])
                    b = bi % 4; bi += 1
                    for kc in range(4, 8):
                        mm(bankv(b, 512), CT[:, kc, t * 128:(t + 1) * 128], WO[:, kc, hf * 512:(hf + 1) * 512],
                           kc == 4, kc == 7, [("CT", kc, t // 4), ("WIN", 1)], bk(b))
                    tt("dve", xs, bankv(b, 512), xs, ALU.add, bk(b) + [("X", t)], [("X", t)])

        GROUPS = [(0, 4), (4, 4), (8, 4), (12, 4), (16, 4), (20, 2)]

        def ffn(experts, stage):
            o = PH
            AT = carve(4096, BF16, at=o).rearrange("p (j s) -> p j s", s=S); o += 4096
            WG = [carve(2048, BF16, at=o + i * 6144).rearrange("p (k c) -> p k c", c=512) for i in range(2)]
            WU = [carve(2048, BF16, at=o + 2048 + i * 6144).rearrange("p (k c) -> p k c", c=512) for i in range(2)]
            WD = [carve(2048, BF16, at=o + 4096 + i * 6144).rearrange("p (j c) -> p j c", c=D) for i in range(2)]
            o += 12288
            SG = [carve(512, at=o + i * 512) for i in range(2)]; o += 1024
            assert o <= TOT
            work = [(e, g) for e in range(len(experts)) for g in range(len(GROUPS))]

            def load(idx):
                e, g = work[idx]
                gd, ud, dd, _ = experts[e]
                j0, nj = GROUPS[g]
                sl = idx % 2
                castload(8 + sl, WG[sl][:, :, 0:nj * 128], gd[:, :, j0 * 128:(j0 + nj) * 128], (), [("WG", sl)])
                castload(10 + sl, WU[sl][:, :, 0:nj * 128], ud[:, :, j0 * 128:(j0 + nj) * 128], (), [("WU", sl)])
                castload(12 + sl, WD[sl][:, 0:nj, :], dd[:, j0:j0 + nj, :], (), [("WD", sl)])
                tt("pool", WD[sl][:, 0:nj, :], WD[sl][:, 0:nj, :], GBC.unsqueeze(1).broadcast_to([128, nj, D]), ALU.mult,
                   [("WD", sl), "GBC"], [("WD", sl)])

            if stage == "pre":
                load(0)
                return
            gi = 0
            di = 0
            for idx, (e, g) in enumerate(work):
                if idx + 1 < len(work):
                    load(idx + 1)
                sl = idx % 2
                j0, nj = GROUPS[g]
                wcol = experts[e][3]
                for tc in range(4):
                    for j in range(nj):
                        bg = gi % 2
                        bu = 2 + gi % 2
                        gi += 1
                        for kc in range(KC):
                            mm(bankv(bg, 512), WG[sl][:, kc, j * 128:(j + 1) * 128], HT[:, kc, tc * 512:(tc + 1) * 512],
                               kc == 0, kc == KC - 1, [("WG", sl), ("HT", kc, tc)], bk(bg))
                        for kc in range(KC):
                            mm(bankv(bu, 512), WU[sl][:, kc, j * 128:(j + 1) * 128], HT[:, kc, tc * 512:(tc + 1) * 512],
                               kc == 0, kc == KC - 1, [("WU", sl), ("HT", kc, tc)], bk(bu))
                        sgt = SG[bg]
                        act(sgt, bankv(bg, 512), AF.Silu, bk(bg), [("SG", bg)])
                        tt("dve", AT[:, j, tc * 512:(tc + 1) * 512], sgt, bankv(bu, 512), ALU.mult,
                           [("SG", bg)] + bk(bu), [("AT", j, tc)])
                for t in range(NT):
                    for hf in range(2):
                        b = 4 + di % 4
                        di += 1
                        for j in range(nj):
                            mm(bankv(b, 512), AT[:, j, t * 128:(t + 1) * 128], WD[sl][:, j, hf * 512:(hf + 1) * 512],
                               j == 0, j == nj - 1, [("AT", j, t // 4), ("WD", sl)], bk(b))
                        xs = X[:, t, hf * 512:(hf + 1) * 512]
                        if wcol is None:
                            tt("dve", xs, bankv(b, 512), xs, ALU.add, bk(b) + [("X", t)], [("X", t)])
                        else:
                            stt(xs, bankv(b, 512), wgt[:, t * NE + wcol:t * NE + wcol + 1], xs, ALU.mult, ALU.add,
                                bk(b) + [("X", t), "wgt"], [("X", t)])

        def router():
            lp = bankv(7, 128)
            for t in range(NT):
                for kc in range(KC):
                    mm(lp[:, t * 8:(t + 1) * 8], HT[:, kc, t * 128:(t + 1) * 128], rwb[:, kc, :], kc == 0, kc == KC - 1,
                       [("HT", kc, t // 4), "rwb"], bk(7))
            cp("dve", lg, lp, bk(7), ["lg"])
            l3 = lg.rearrange("p (t e) -> p t e", e=NE)
            a3 = r1.rearrange("p (t e) -> p t e", e=NE)
            b3 = r2.rearrange("p (t e) -> p t e", e=NE)
            w3 = wgt.rearrange("p (t e) -> p t e", e=NE)
            m1b = r3.unsqueeze(2).broadcast_to([128, NT, NE])
            m2b = r4.unsqueeze(2).broadcast_to([128, NT, NE])
            P.op("dve", lambda e: e.tensor_reduce(out=r3, in_=l3, axis=AX.X, op=ALU.max), ["lg"], ["r3"])
            tt("dve", a3, l3, m1b, ALU.is_equal, ["lg", "r3"], ["r1"])
            stt(b3, a3, -1e30, l3, ALU.mult, ALU.add, ["r1", "lg"], ["r2"])
            P.op("dve", lambda e: e.tensor_reduce(out=r4, in_=b3, axis=AX.X, op=ALU.max), ["r2"], ["r4"])
            tt("dve", a3, l3, m2b, ALU.is_ge, ["lg", "r4", "r1"], ["r1"])
            tt("dve", b3, l3, m1b, ALU.subtract, ["lg", "r3", "r2"], ["r2"])
            act(r2, r2, AF.Exp, ["r2"], ["r2"])
            tt("dve", b3, b3, a3, ALU.mult, ["r2", "r1"], ["r2"])
            P.op("dve", lambda e: e.tensor_reduce(out=r3, in_=b3, axis=AX.X, op=ALU.add), ["r2", "r3"], ["r3"])
            P.op("dve", lambda e: e.reciprocal(out=r3, in_=r3), ["r3"], ["r3"])
            tt("dve", w3, b3, m1b, ALU.mult, ["r2", "r3"], ["wgt"])

        def ck(name):
            if stop == name:
                raise StopBuild()

        def tap(name, ap, keys):
            if name not in tap_d:
                return
            dt_ = tap_d[name]
            P.dma("sp", 3, lambda e: e.dma_start(out=dt_, in_=ap), list(keys), [("dbg", "tap", name)])

        try:
            ck("consts")
            for l in range(DEPTH):
                compute_mod(l)
                tap(f"modT{l}", modT, ["modT"])
                ck(f"mod{l}")
                make_gbc(modT[:, 16:24], "modT")
                tap(f"gbc{l}", GBC, ["GBC"])
                ck(f"gbc{l}")
                make_hT(s1, modT[:, 0:8], ["s1", "modT"])
                tap(f"hT{l}", HT.rearrange("p k s -> p (k s)"), HT_ALL)
                ck(f"hT{l}")
                barrier()
                mixer(l)
                dump(f"mix{l}")
                ck(f"mix{l}")
                barrier()
                make_gbc(modT[:, 40:48], "modT")
                if l % 2 == 0:
                    experts = [(fg_d, fu_d, fd_d, None)]
                else:
                    experts = [(mg_d[e], mu_d[e], md_d[e], e) for e in range(NE)]
                ffn(experts, "pre")
                make_hT(s2, modT[:, 24:32], ["s2", "modT"])
                barrier()
                if l % 2 == 1:
                    router()
                ffn(experts, "main")
                dump(f"ffn{l}")
                ck(f"ffn{l}")
                barrier()
        except StopBuild:
            barrier()
        make_gbc(fng, "fng")
        yt = [carve(D, at=PH + 8000 + i * D) for i in range(2)]
        junk = carve(512, BF16, at=PH)
        for t in range(NT):
            act(junk, X[:, t, :], AF.Square, [("X", t)], ["junk", "ssf"], accum_out=ss[:, t:t + 1])
        ts("dve", rstd, ss, 1.0 / D, EPS, ALU.mult, ALU.add, ["ssf"], ["rstdf"])
        act(rstd, rstd, AF.Sqrt, ["rstdf"], ["rstdf"])
        P.op("dve", lambda e: e.reciprocal(out=rstd, in_=rstd), ["rstdf"], ["rstdf"])
        for t in range(NT):
            stt(yt[t % 2], X[:, t, :], rstd[:, t:t + 1], GBC, ALU.mult, ALU.mult, [("X", t), "rstdf", "GBC"], [("yt", t % 2)])
            P.dma("sp", 14 + t % 2, lambda e, t=t: e.dma_start(out=y_d[t * 128:(t + 1) * 128, :], in_=yt[t % 2]),
                  [("yt", t % 2)], [("y", t)])
        P.final_wait("sp", [("y", t) for t in range(NT)] + [k for k in P.lastw if isinstance(k, tuple) and k[0] == "dbg"])

        with nc.Block() as block:
            @block.sync
            def _(e):
                P.replay("sp", e)

            @block.tensor
            def _(e):
                P.replay("pe", e)

            @block.vector
            def _(e):
                P.replay("dve", e)

            @block.scalar
            def _(e):
                P.replay("act", e)

            @block.gpsimd
            def _(e):
                P.replay("pool", e)
    return nc


def host_consts():
    f = np.float32
    c = {}
    c["idn"] = np.eye(128, dtype=f)
    half = 64
    inv_freq = (10000.0 ** (-np.arange(half, dtype=np.float32) / half)).astype(f)
    pos = np.arange(S, dtype=np.float32)
    ang = (pos[:, None] * inv_freq[None, :]).astype(f)
    cos = np.cos(ang).astype(f).reshape(NT, 128, half).transpose(1, 0, 2).reshape(128, NT * half)
    sin = np.sin(ang).astype(f).reshape(NT, 128, half).transpose(1, 0, 2).reshape(128, NT * half)
    c["cos"] = np.ascontiguousarray(cos)
    c["sin"] = np.ascontiguousarray(sin)
    dec = np.zeros((128, 12), f)
    idx = np.arange(128, dtype=np.float64)
    for r in range(4):
        lgm = np.log(1.0 - 2.0 ** (-5.0 - r))
        dec[:, r * 3 + 0] = np.exp((idx + 1.0) * lgm)
        dec[:, r * 3 + 1] = np.exp(-(idx + 1.0) * lgm) * (128.0 ** -0.5)
        dec[:, r * 3 + 2] = np.exp((127.0 - idx) * lgm) * (128.0 ** -0.5)
    c["dec"] = dec
    kk = np.arange(128)
    c["m01"] = (kk[None, :] >= kk[:, None]).astype(f)
    c["mneg"] = np.where(kk[:, None] > kk[None, :], NEG, 0.0).astype(f)
    en = np.zeros((128, 8, 128), f)
    for n in range(8):
        en[n, n, :] = 1.0
        en[64 + n, n, :] = 1.0
    c["en"] = en.reshape(128, 8 * 128)
    past = np.zeros((2, NT, 8), f)
    nb = np.zeros((2, NT, 8), f)
    for t in range(NT):
        qb = t // 2
        for n in range(8):
            past[:, t, n] = 0.0 if n < qb else -1e30
            nb[:, t, n] = 0.0 if n == qb else NEG
    c["past"] = np.ascontiguousarray(np.broadcast_to(past.reshape(1, 256), (128, 256)))
    c["nb"] = np.ascontiguousarray(np.broadcast_to(nb.reshape(1, 256), (128, 256)))
    return c


def fm(v, n):
    return np.ascontiguousarray(np.asarray(v, np.float32).reshape(n, 128).T)


def prep_shared(inp):
    f = np.float32
    sh = dict(host_consts())
    sh["nmg"] = np.concatenate([fm(inp["norm_mix_g"][l], KC) for l in range(DEPTH)], axis=1)
    sh["nfg"] = np.concatenate([fm(inp["norm_ffn_g"][l], KC) for l in range(DEPTH)], axis=1)
    sh["aog"] = np.concatenate([fm(inp["att_out_g"][l], 4) for l in range(DEPTH)], axis=1)
    sh["rog"] = np.concatenate([fm(inp["ret_out_g"][l], 4) for l in range(DEPTH)], axis=1)
    sh["fng"] = fm(inp["final_norm_g"], KC)
    sh["adaw"] = np.ascontiguousarray(np.asarray(inp["ada_w"], f).reshape(DEPTH, KC, 128, 6 * D))
    sh["adabr"] = np.ascontiguousarray(np.asarray(inp["ada_b"], f))
    sh["adab"] = np.concatenate([fm(inp["ada_b"][l], 48) for l in range(DEPTH)], axis=1)
    cols = []
    for j in range(4):
        for w in range(3):
            cols += list(range(w * 512 + j * 128, w * 512 + (j + 1) * 128))
    for r in range(4):
        for w in range(4):
            cols += list(range(1536 + w * 512 + r * 128, 1536 + w * 512 + (r + 1) * 128))
    cols = np.array(cols)

    def pk(w):
        w = np.asarray(w, f)
        k = w.shape[0] // 128
        return np.ascontiguousarray(w.reshape(k, 128, w.shape[1]).transpose(1, 0, 2))
    sh["win"] = np.stack([pk(np.asarray(inp["w_in"][l], f)[:, cols]) for l in range(DEPTH)])
    sh["wout"] = np.stack([pk(inp["w_out"][l]) for l in range(DEPTH)])
    sh["fg"] = pk(inp["ffn_w_gate"][0])
    sh["fu"] = pk(inp["ffn_w_up"][0])
    sh["fd"] = pk(inp["ffn_w_down"][0])
    sh["rw"] = pk(inp["router_w"][0]).reshape(128, KC * NE)
    sh["mg"] = np.stack([pk(inp["moe_w_gate"][0][e]) for e in range(NE)])
    sh["mu"] = np.stack([pk(inp["moe_w_up"][0][e]) for e in range(NE)])
    sh["md"] = np.stack([pk(inp["moe_w_down"][0][e]) for e in range(NE)])
    return sh


def kernel(**inputs):
    inp = {k: np.asarray(v) for k, v in inputs.items()}
    sh = prep_shared(inp)
    nc = build_nc()
    in_maps = []
    for b in range(8):
        m = dict(sh)
        m["x"] = np.ascontiguousarray(inp["x"][b], dtype=np.float32)
        m["cT"] = fm(inp["c"][b], KC)
        in_maps.append(m)
    res = run_bass_kernel_spmd(nc, in_maps, core_ids=list(range(8)))
    return np.stack([np.asarray(r["y"], dtype=np.float32) for r in res.results], axis=0)
```

```python
import numpy as np
from contextlib import ExitStack
import concourse.bass as bass
import concourse.mybir as mybir
from concourse.bass_utils import run_bass_kernel_spmd
from concourse.alu_op_type import AluOpType as ALU

F32 = mybir.dt.float32
BF16 = mybir.dt.bfloat16
AF = mybir.ActivationFunctionType
AX = mybir.AxisListType

DEPTH = 2
S = 2048
D = 1024
NT = 16
KC = 8
DFF = 2816
NFC = 22
NE = 8
EPS = 1e-6
NEG = -240000.0
SAME_ENGINE_SYNC = True


class Prog:
    ENG = ["pe", "act", "dve", "pool", "sp"]

    def __init__(self, nc, sems, dma_sems):
        self.nc = nc
        self.items = {e: [] for e in self.ENG}
        self.sem = {e: sems[i] for i, e in enumerate(self.ENG)}
        self.cnt = {e: 0 for e in self.ENG}
        self.waited = {e: {} for e in self.ENG}
        self.lastw = {}
        self.readers = {}
        self.dma_sems = dma_sems
        self.dma_cnt = [0] * len(dma_sems)
        self.lock_last = {}

    @staticmethod
    def _locks(reads, writes):
        return {k[0] for k in list(reads) + list(writes)
                if isinstance(k, tuple) and len(k) == 2 and isinstance(k[0], int)}

    def _deps(self, eng, reads, writes):
        toks = {}

        def add(t):
            if t is None:
                return
            s, v = t
            k = id(s)
            if k not in toks or toks[k][1] < v:
                toks[k] = (s, v)
        for r in reads:
            add(self.lastw.get(r))
        for w in writes:
            add(self.lastw.get(w))
            for t in self.readers.get(w, {}).values():
                add(t)
        for b in self._locks(reads, writes):
            for e2, t in self.lock_last.get(b, {}).items():
                if e2 != eng:
                    add(t)
        out = []
        for k, (s, v) in toks.items():
            if s is self.sem[eng] and (eng == "pe" or not SAME_ENGINE_SYNC):
                continue
            if self.waited[eng].get(k, 0) < v:
                self.waited[eng][k] = v
                out.append((s, v))
        return out

    def _commit(self, tok, reads, writes):
        k = id(tok[0])
        for r in reads:
            d = self.readers.setdefault(r, {})
            if k not in d or d[k][1] < tok[1]:
                d[k] = tok
        for w in writes:
            self.lastw[w] = tok
            self.readers[w] = {}

    def op(self, eng, fn, reads=(), writes=()):
        for s, v in self._deps(eng, reads, writes):
            self.items[eng].append(("wait", s, v))
        self.cnt[eng] += 1
        self.items[eng].append(("op", fn, self.sem[eng], 1))
        tok = (self.sem[eng], self.cnt[eng])
        self._commit(tok, reads, writes)
        for b in self._locks(reads, writes):
            self.lock_last.setdefault(b, {})[eng] = tok

    def dma(self, eng, chan, fn, reads=(), writes=()):
        s = self.dma_sems[chan]
        deps = self._deps(eng, reads, writes)
        prev = self.dma_cnt[chan] * 16
        if prev and self.waited[eng].get(id(s), 0) < prev:
            self.waited[eng][id(s)] = prev
            deps.append((s, prev))
        for ss, v in deps:
            self.items[eng].append(("wait", ss, v))
        self.dma_cnt[chan] += 1
        self.items[eng].append(("op", fn, s, 16))
        self._commit((s, self.dma_cnt[chan] * 16), reads, writes)

    def final_wait(self, eng, keys):
        for s, v in self._deps(eng, keys, ()):
            self.items[eng].append(("wait", s, v))

    def replay(self, eng, e):
        for it in self.items[eng]:
            if it[0] == "wait":
                e.wait_ge(it[1], it[2])
            else:
                it[1](e).then_inc(it[2], it[3])


class StopBuild(Exception):
    pass


def build_nc(debug=(), stop=None, taps=()):
    nc = bass.Bass("TRN2", target_bir_lowering=False)

    def din(name, shape):
        return nc.dram_tensor(name, list(shape), F32, kind="ExternalInput").ap()
    x_d = din("x", [S, D])
    cT_d = din("cT", [128, KC])
    nmg_d = din("nmg", [128, DEPTH * KC])
    nfg_d = din("nfg", [128, DEPTH * KC])
    aog_d = din("aog", [128, DEPTH * 4])
    rog_d = din("rog", [128, DEPTH * 4])
    fng_d = din("fng", [128, KC])
    adaw_d = din("adaw", [DEPTH, KC, 128, 6 * D])
    adab_d = din("adab", [128, DEPTH * 48])
    adabr_d = din("adabr", [DEPTH, 6 * D])
    win_d = din("win", [DEPTH, 128, KC, 3584])
    wout_d = din("wout", [DEPTH, 128, KC, D])
    fg_d = din("fg", [128, KC, DFF])
    fu_d = din("fu", [128, KC, DFF])
    fd_d = din("fd", [128, NFC, D])
    rw_d = din("rw", [128, KC * NE])
    mg_d = din("mg", [NE, 128, KC, DFF])
    mu_d = din("mu", [NE, 128, KC, DFF])
    md_d = din("md", [NE, 128, NFC, D])
    idn_d = din("idn", [128, 128])
    cos_d = din("cos", [128, NT * 64])
    sin_d = din("sin", [128, NT * 64])
    dec_d = din("dec", [128, 12])
    m01_d = din("m01", [128, 128])
    mneg_d = din("mneg", [128, 128])
    en_d = din("en", [128, 8 * 128])
    past_d = din("past", [128, 256])
    nb_d = din("nb", [128, 256])
    y_d = nc.dram_tensor("y", [S, D], F32, kind="ExternalOutput").ap()
    tap_d = {}
    for name, (shp, dt_) in dict(taps).items():
        tap_d[name] = nc.dram_tensor("tap_" + name, list(shp), dt_, kind="ExternalOutput").ap()
    dbg_d = {}
    for name in debug:
        dbg_d[name] = nc.dram_tensor("dbg_" + name, [S, D], F32, kind="ExternalOutput").ap()

    with ExitStack() as st:
        TOT = 52800
        A = st.enter_context(nc.sbuf_tensor("arena", [128, TOT], F32))
        banks = [st.enter_context(nc.psum_tensor(f"bank{i}", [128, 512], F32)) for i in range(8)]
        sems = [st.enter_context(nc.semaphore(f"es{i}")) for i in range(5)]
        NCH = 24
        dsems = [st.enter_context(nc.semaphore(f"ds{i}")) for i in range(NCH)]
        P = Prog(nc, sems, dsems)

        cur = [0]

        def carve(n, dt=F32, at=None):
            if at is None:
                off = cur[0]
                cur[0] += n
            else:
                off = at
            assert off + n <= TOT, (off, n)
            ap = A[:, off:off + n]
            if dt != F32:
                ap = ap.bitcast(dt)
            return ap

        X = carve(NT * D).rearrange("p (t d) -> p t d", d=D)
        HT = carve(KC * S // 2, BF16).rearrange("p (k s) -> p k s", s=S)
        identb = carve(64, BF16)
        identf = carve(128)
        onesf = carve(128)
        cosT = carve(NT * 64).rearrange("p (t c) -> p t c", c=64)
        sinT = carve(NT * 64).rearrange("p (t c) -> p t c", c=64)
        dec = carve(12)
        m01 = carve(64, BF16)
        mneg = carve(64, BF16)
        EN = carve(512, BF16).rearrange("p (n k) -> p n k", k=128)
        pastm = carve(256)
        NB = carve(256)
        modT = carve(48)
        adab = carve(DEPTH * 48)
        nmg = carve(DEPTH * KC)
        nfg = carve(DEPTH * KC)
        aog = carve(DEPTH * 4)
        rog = carve(DEPTH * 4)
        fng = carve(KC)
        s1 = carve(KC)
        s2 = carve(KC)
        GBC = carve(D)
        cTt = carve(KC)
        csb = carve(KC // 2, BF16)
        csf = carve(KC)
        ss = carve(NT)
        rstd = carve(NT)
        tmp16 = carve(NT)
        ssa = carve(4 * NT)
        rstda = carve(NT)
        rwb = carve(KC * NE // 2, BF16).rearrange("p (k e) -> p k e", e=NE)
        lg = carve(NT * NE)
        wgt = carve(NT * NE)
        r1 = carve(NT * NE)
        r2 = carve(NT * NE)
        r3 = carve(NT)
        r4 = carve(NT)
        PH = cur[0]
        PHN = TOT - PH

        def bankv(b, n, dt=F32, off=0):
            ap = banks[b][:, off:off + n]
            if dt != F32:
                ap = ap.bitcast(dt)
            return ap

        def bk(b):
            return [(b, 0), (b, 1)]

        def mm(out, lhsT, rhs, start, stop, reads, writes, sgc=False):
            P.op("pe", lambda e: e.matmul(out, lhsT=lhsT, rhs=rhs, start=start, stop=stop, skip_group_check=sgc),
                 reads, writes)

        def tr(out, in_, reads, writes):
            P.op("pe", lambda e: e.transpose(out, in_, identb), list(reads) + ["identb"], writes)

        def act(out, in_, func, reads, writes, **kw):
            P.op("act", lambda e: e.activation(out=out, in_=in_, func=func, **kw), reads, writes)

        def tt(eng, out, in0, in1, op, reads, writes):
            P.op(eng, lambda e: e.tensor_tensor(out=out, in0=in0, in1=in1, op=op), reads, writes)

        def ts(eng, out, in0, s1_, s2_, op0, op1, reads, writes):
            if op1 is None:
                P.op(eng, lambda e: e.tensor_scalar(out=out, in0=in0, scalar1=s1_, scalar2=None, op0=op0), reads, writes)
            else:
                P.op(eng, lambda e: e.tensor_scalar(out=out, in0=in0, scalar1=s1_, scalar2=s2_, op0=op0, op1=op1), reads, writes)

        def stt(out, in0, scalar, in1, op0, op1, reads, writes):
            P.op("dve", lambda e: e.scalar_tensor_tensor(out=out, in0=in0, scalar=scalar, in1=in1, op0=op0, op1=op1),
                 reads, writes)

        def cp(eng, out, in_, reads, writes):
            if eng == "act":
                act(out, in_, AF.Copy, reads, writes)
            else:
                P.op(eng, lambda e: e.tensor_copy(out=out, in_=in_), reads, writes)

        cch = [0]

        def cload(out, in_, key, eng="sp"):
            ch = 16 + (cch[0] % 4)
            cch[0] += 1
            P.dma(eng, ch, lambda e: e.dma_start(out=out, in_=in_), (), [key])

        def castload(chan, out, in_, reads, writes):
            P.dma("pool", chan, lambda e: e.dma_start(out=out, in_=in_, max_dma_last_dim=4096), reads, writes)

        cload(identf, idn_d[:, :], "identf")
        cload(cosT, cos_d[:, :].rearrange("p (t c) -> p t c", c=64), "cos")
        cload(sinT, sin_d[:, :].rearrange("p (t c) -> p t c", c=64), "sin")
        cload(dec, dec_d[:, :], "dec")
        cload(pastm, past_d[:, :], "past")
        cload(NB, nb_d[:, :], "NB")
        cload(adab, adab_d[:, :], "adab")
        cload(nmg, nmg_d[:, :], "nmg")
        cload(nfg, nfg_d[:, :], "nfg")
        cload(aog, aog_d[:, :], "aog")
        cload(rog, rog_d[:, :], "rog")
        cload(fng, fng_d[:, :], "fng")
        cload(cTt, cT_d[:, :], "cT")
        castload(20, identb, idn_d[:, :], (), ["identb"])
        castload(21, m01, m01_d[:, :], (), ["m01"])
        castload(22, mneg, mneg_d[:, :], (), ["mneg"])
        castload(23, EN.rearrange("p n k -> p (n k)"), en_d[:, :], (), ["EN"])
        castload(20, rwb.rearrange("p k e -> p (k e)"), rw_d[:, :], (), ["rwb"])
        P.op("dve", lambda e: e.memset(onesf, 1.0), (), ["onesf"])
        for t4 in range(4):
            P.dma("sp", t4, lambda e, t4=t4: e.dma_start(
                out=X[:, 4 * t4:4 * t4 + 4, :],
                in_=x_d[512 * t4:512 * (t4 + 1), :].rearrange("(t p) d -> p t d", p=128)),
                (), [("X", 4 * t4 + i) for i in range(4)])
        act(csb, cTt, AF.Silu, ["cT"], ["csb"])
        act(csf, cTt, AF.Silu, ["cT"], ["csf"])

        def barrier():
            alle = ["pe", "act", "dve", "pool", "sp"]
            for e in alle:
                for o in alle:
                    if o == e or P.cnt[o] == 0:
                        continue
                    k = id(P.sem[o])
                    if P.waited[e].get(k, 0) < P.cnt[o]:
                        P.waited[e][k] = P.cnt[o]
                        P.items[e].append(("wait", P.sem[o], P.cnt[o]))
                for ch in range(NCH):
                    v = P.dma_cnt[ch] * 16
                    k = id(P.dma_sems[ch])
                    if v and P.waited[e].get(k, 0) < v:
                        P.waited[e][k] = v
                        P.items[e].append(("wait", P.dma_sems[ch], v))

        def dump(name):
            if name in dbg_d:
                for t4 in range(4):
                    P.dma("sp", 2 + (t4 % 2), lambda e, t4=t4: e.dma_start(
                        out=dbg_d[name][512 * t4:512 * (t4 + 1), :].rearrange("(t p) d -> p t d", p=128),
                        in_=X[:, 4 * t4:4 * t4 + 4, :]),
                        [("X", 4 * t4 + i) for i in range(4)], [("dbg", name, t4)])

        def compute_mod(l):
            ADA = [carve(3072, at=PH + 3000 + i * 3072) for i in range(2)]
            dtmp = carve(2048, at=PH + 9200)
            csrep = carve(KC * 128, at=PH + 11300).rearrange("p (k m) -> p k m", m=128)
            for kc in range(KC):
                ts("dve", csrep[:, kc, :], onesf, csf[:, kc:kc + 1], None, ALU.mult, None, ["onesf", "csf"], ["csrep"])
            it = 0
            for hf in range(2):
                for kc in range(KC):
                    sl = it % 2
                    it += 1
                    P.dma("sp", 4 + sl, lambda e, sl=sl, kc=kc, hf=hf: e.dma_start(
                        out=ADA[sl], in_=adaw_d[l, kc, :, hf * 3072:(hf + 1) * 3072]), (), [("ADA", sl)])
                    for j in range(6):
                        mm(bankv(j, 512), csrep[:, kc, :], ADA[sl][:, j * 512:(j + 1) * 512], kc == 0, kc == KC - 1,
                           [("ADA", sl), "csrep"], bk(j))
                for j in range(6):
                    d3 = dtmp[:, (j % 4) * 512:(j % 4 + 1) * 512].rearrange("p (a b) -> p a b", b=128)
                    tt("dve", d3, bankv(j, 512).rearrange("p (a b) -> p a b", b=128),
                       identf.unsqueeze(1).broadcast_to([128, 4, 128]), ALU.mult, bk(j) + ["identf"], [("dtmp", j % 4)])
                    c0 = hf * 24 + j * 4
                    P.op("dve", lambda e, d3=d3, c0=c0: e.tensor_reduce(out=modT[:, c0:c0 + 4], in_=d3, axis=AX.X, op=ALU.add),
                         [("dtmp", j % 4)], ["modT"])
            tt("dve", modT, modT, adab[:, l * 48:(l + 1) * 48], ALU.add, ["modT", "adab"], ["modT"])
            stt(s1, modT[:, 8:16], 1.0, nmg[:, l * KC:(l + 1) * KC], ALU.add, ALU.mult, ["modT", "nmg"], ["s1"])
            stt(s2, modT[:, 32:40], 1.0, nfg[:, l * KC:(l + 1) * KC], ALU.add, ALU.mult, ["modT", "nfg"], ["s2"])

        def make_gbc(gT, gkey):
            diag = carve(2 * 128, at=PH + 2600).rearrange("p (a b) -> p a b", b=128)
            for kc in range(KC):
                sl = kc % 2
                ts("dve", diag[:, sl, :], identf, gT[:, kc:kc + 1], None, ALU.mult, None,
                   ["identf", gkey], [("diag", sl)])
                b = 5 + kc // 4
                mm(bankv(b, 128, off=(kc % 4) * 128), onesf, diag[:, sl, :], True, True,
                   ["onesf", ("diag", sl)], bk(b))
            for h in range(2):
                cp("dve", GBC[:, h * 512:(h + 1) * 512], bankv(5 + h, 512), bk(5 + h), ["GBC"])

        def make_hT(sT, shT, skeys):
            junk = carve(512, BF16, at=PH)
            xn = carve(2048, BF16, at=PH + 512).rearrange("p (i d) -> p i d", d=D)
            for tq in range(4):
                for i in range(4):
                    t = tq * 4 + i
                    act(junk, X[:, t, :], AF.Square, [("X", t)], ["junk", ("ss", tq)], accum_out=ss[:, t:t + 1])
                q = slice(tq * 4, tq * 4 + 4)
                ts("dve", tmp16[:, q], ss[:, q], 1.0 / D, EPS, ALU.mult, ALU.add, [("ss", tq)], [("tmp16", tq)])
                act(tmp16[:, q], tmp16[:, q], AF.Sqrt, [("tmp16", tq)], [("tmp16", tq)])
                P.op("dve", lambda e, q=q: e.reciprocal(out=rstd[:, q], in_=tmp16[:, q]), [("tmp16", tq)], [("rstd", tq)])
                for i in range(4):
                    t = tq * 4 + i
                    if i % 2 == 0:
                        act(xn[:, i, :], X[:, t, :], AF.Identity, [("X", t), ("rstd", tq)], [("xn", i)], scale=rstd[:, t:t + 1])
                    else:
                        ts("pool", xn[:, i, :], X[:, t, :], rstd[:, t:t + 1], 0.0, ALU.mult, ALU.add,
                           [("X", t), ("rstd", tq)], [("xn", i)])
                for kc in range(KC):
                    b = kc % 4
                    pT = bankv(b, 256, BF16)
                    for i in range(4):
                        tr(pT[:, i * 128:(i + 1) * 128], xn[:, i, kc * 128:(kc + 1) * 128], [("xn", i)], bk(b))
                    o = HT[:, kc, tq * 512:(tq + 1) * 512]
                    if kc % 2 == 0:
                        act(o, pT, AF.Identity, bk(b) + skeys, [("HT", kc, tq)],
                            scale=sT[:, kc:kc + 1], bias=shT[:, kc:kc + 1])
                    else:
                        ts("dve", o, pT, sT[:, kc:kc + 1], shT[:, kc:kc + 1], ALU.mult, ALU.add,
                           bk(b) + skeys, [("HT", kc, tq)])

        HT_ALL = [("HT", kc, tq) for kc in range(KC) for tq in range(4)]

        def HTk(tq):
            return [("HT", kc, tq) for kc in range(KC)]

        def mixer(l):
            o = PH
            CT = carve(8192, BF16, at=o).rearrange("p (k s) -> p k s", s=S); o += 8192
            WIN = [carve(2048, BF16, at=o + i * 2048).rearrange("p (k c) -> p k c", c=512) for i in range(2)]
            WO = carve(4096, BF16, at=o).rearrange("p (k c) -> p k c", c=D); o += 4096
            U = o

            def load_unit(u):
                sl = u % 2
                if u < 4:
                    castload(6 + sl, WIN[sl][:, :, 0:384], win_d[l, :, :, u * 384:(u + 1) * 384], (), [("WIN", sl)])
                else:
                    r = u - 4
                    castload(6 + sl, WIN[sl][:, :, :], win_d[l, :, :, 1536 + r * 512:1536 + (r + 1) * 512], (), [("WIN", sl)])

            def att_unit(j):
                sl = j % 2
                W = WIN[sl]
                o = U
                QZ = [carve(1024, BF16, at=o + h * 1024) for h in range(2)]; o += 2048
                KZ = [carve(1024, BF16, at=o + h * 1024) for h in range(2)]; o += 2048
                VA = carve(1040, BF16, at=o).rearrange("p (t h c) -> p t h c", h=2, c=65); o += 1040
                bias2 = carve(1024, BF16, at=o).rearrange("p (t c) -> p t c", c=128); o += 1024
                PT = [carve(128, BF16, at=o + i * 128) for i in range(8)]; o += 1024
                otok = carve(1024, BF16, at=o).rearrange("p (t c) -> p t c", c=128); o += 1024
                gm = carve(256, at=o); o += 256
                cmp = carve(1024, at=o); o += 1024
                cnt = carve(256, at=o); o += 256
                kms = carve(8, at=o); o += 8
                kmbz = [carve(4, BF16, at=o + h * 4) for h in range(2)]; o += 8
                rec = carve(2, at=o); o += 2
                recb = carve(2, at=o); o += 2
                sq = carve(64, BF16, at=o); o += 64
                assert o <= TOT, o
                wk = ("WIN", sl)
                own = [slice(0, 64), slice(64, 128)]
                oth = [slice(64, 128), slice(0, 64)]
                aug = [slice(64, 72), slice(0, 8)]
                for h in range(2):
                    P.op("pool", lambda e, h=h: e.memset(QZ[h][oth[h], :], 0.0), (), [("QZz", h, 0), ("QZz", h, 1)])
                    P.op("pool", lambda e, h=h: e.memset(KZ[h][oth[h], :], 0.0), (), [("KZz", h)])
                    P.op("pool", lambda e, h=h: e.tensor_copy(
                        out=KZ[h][aug[h], :].rearrange("p (n r k) -> p n r k", r=2, k=128),
                        in_=EN[aug[h], :, :].unsqueeze(2).broadcast_to([8, 8, 2, 128])), ["EN", ("KZz", h)], [("KZz", h)])
                    P.op("pool", lambda e, h=h: e.memset(kmbz[h], 0.0), (), [("kmbz", h)])
                for which, dst, nm in ((0, QZ, "QZ"), (1, KZ, "KZ")):
                    for tc in range(4):
                        b = tc % 2
                        for kc in range(KC):
                            mm(bankv(b, 512), W[:, kc, which * 128:(which + 1) * 128], HT[:, kc, tc * 512:(tc + 1) * 512],
                               kc == 0, kc == KC - 1, [wk, ("HT", kc, tc)], bk(b))
                        for h in range(2):
                            cp("dve", dst[h][own[h], tc * 512:(tc + 1) * 512], bankv(b, 512)[own[h], :],
                               bk(b), [(nm, h, tc)])
                P.op("pool", lambda e: e.memset(VA[:, :, :, 64:65], 1.0), (), ["VAone"])
                for tq in range(4):
                    b = 2 + tq % 2
                    for i in range(4):
                        t = tq * 4 + i
                        for kc in range(KC):
                            mm(bankv(b, 128, off=i * 128), HT[:, kc, t * 128:(t + 1) * 128], W[:, kc, 256:384],
                               kc == 0, kc == KC - 1, [wk, ("HT", kc, tq)], bk(b))
                    cp("act" if tq % 2 == 0 else "dve", VA[:, tq * 4:tq * 4 + 4, :, 0:64],
                       bankv(b, 512).rearrange("p (t h c) -> p t h c", h=2, c=64), bk(b), [("VA", tq)])
                P.op("pool", lambda e: e.memset(bias2, 0.0), (), ["bias2"])
                for h in range(2):
                    P.op("dve", lambda e, h=h: e.tensor_reduce(out=kms[own[h], :], in_=KZ[h][own[h], :].rearrange("p (n k) -> p n k", k=256),
                                                              axis=AX.X, op=ALU.add),
                         [("KZ", h, tc) for tc in range(4)], [("kms", h)])
                    cp("dve", kmbz[h][own[h], :], kms[own[h], :], [("kms", h), ("kmbz", h)], [("kmbz", h)])
                    gp = bankv(h, 128)
                    for t in range(NT):
                        mm(gp[:, t * 8:t * 8 + 8], QZ[h][:, t * 128:(t + 1) * 128], kmbz[h],
                           True, True, [("QZ", h, t // 4), ("QZz", h, t // 8), ("kmbz", h)], bk(h))
                    tt("dve", gm[:, h * 128:(h + 1) * 128], gp, pastm[:, h * 128:(h + 1) * 128], ALU.add,
                       bk(h) + ["past"], [("gm", h)])
                    g3 = gm[:, h * 128:(h + 1) * 128].rearrange("p (g n) -> p g n", n=8)
                    cmp4 = cmp.rearrange("p (g n m) -> p g n m", n=8, m=8)
                    tt("dve", cmp4, g3.unsqueeze(2).broadcast_to([128, 16, 8, 8]), g3.unsqueeze(3).broadcast_to([128, 16, 8, 8]),
                       ALU.is_gt, [("gm", h)], ["cmp"])
                    P.op("dve", lambda e, h=h, cmp4=cmp4: e.tensor_reduce(
                        out=cnt[:, h * 128:(h + 1) * 128].rearrange("p (g n) -> p g n", n=8), in_=cmp4, axis=AX.X, op=ALU.add),
                        ["cmp"], [("cnt", h)])
                    c0 = 64 if h == 0 else 0
                    stt(bias2[:, :, c0:c0 + 8], cnt[:, h * 128:(h + 1) * 128].rearrange("p (t n) -> p t n", n=8), 2.5,
                        NB[:, h * 128:(h + 1) * 128].rearrange("p (t n) -> p t n", n=8), ALU.is_gt, ALU.mult,
                        [("cnt", h), "NB", "bias2"], ["bias2"])

                def emit_bias_T():
                    for tq in range(2, 4):
                        b = tq % 2
                        pT = bankv(b, 256, BF16)
                        for i in range(4):
                            t = tq * 4 + i
                            tr(pT[:, i * 128:(i + 1) * 128], bias2[:, t, :], ["bias2"], bk(b))
                        for h in range(2):
                            cp("dve", QZ[h][aug[h], tq * 512:(tq + 1) * 512], pT[aug[h], :], bk(b), [("QZz", h, 1)])

                def emit_tail(qb):
                    for t in (2 * qb, 2 * qb + 1):
                        act(sq, otok[:, t, :], AF.Square, [("otok", t, 0), ("otok", t, 1)], ["sq", ("ssa", j)],
                            accum_out=ssa[:, j * NT + t:j * NT + t + 1])
                    if qb % 2 == 1:
                        tq = qb // 2
                        b = tq % 2
                        pT = bankv(b, 256, BF16)
                        for i in range(4):
                            t = tq * 4 + i
                            tr(pT[:, i * 128:(i + 1) * 128], otok[:, t, :], [("otok", t, 0), ("otok", t, 1)], bk(b))
                        ts("dve", CT[:, j, tq * 512:(tq + 1) * 512], pT, aog[:, l * 4 + j:l * 4 + j + 1], None, ALU.mult, None,
                           bk(b) + ["aog"], [("CT", j, tq)])

                early = [(h, qb, kt) for h in range(2) for qb in range(4) for kt in range(2 * qb + 2)]
                late = [(h, qb, kt) for h in range(2) for qb in range(4, 8) for kt in range(2 * qb + 2)]
                tiles = early + late
                rec2 = [rec, recb]

                def emit_st(i):
                    h, qb, kt = tiles[i]
                    hr = slice(h * 64, (h + 1) * 64)
                    slot = i % 8
                    sb_ = 2 + (i % 4)
                    hb_ = (i // 4) % 2
                    ST = bankv(sb_, 256, off=hb_ * 256)
                    skey = (sb_, hb_)
                    qlo = 128 if kt == 2 * qb + 1 else 0
                    qs = slice(qb * 256 + qlo, qb * 256 + 256)
                    diag = kt >= 2 * qb
                    mm(ST[:, qlo:256], KZ[h][:, kt * 128:(kt + 1) * 128], QZ[h][:, qs], True, not diag,
                       [("KZ", h, kt // 4), ("KZz", h), ("QZ", h, qb // 2), ("QZz", h, qb // 4)], [skey])
                    if diag:
                        dq0 = 0 if kt == 2 * qb else 128
                        mm(ST[:, dq0:dq0 + 128], identb, mneg, False, True, ["identb", "mneg"], [skey])
                    act(PT[slot][:, qlo:256], ST[:, qlo:256], AF.Exp, [skey], [("PT", slot)], scale=0.125)

                def emit_pv(i):
                    h, qb, kt = tiles[i]
                    slot = i % 8
                    pt = PT[slot]
                    ob = 6 + (h * 8 + qb) % 2
                    O = bankv(ob, 130).rearrange("p (q c) -> p q c", c=65)
                    nkt = 2 * qb + 2
                    qlo = 128 if kt == 2 * qb + 1 else 0
                    for qi in range(2):
                        if qi * 128 < qlo:
                            continue
                        mm(O[:, qi, :], pt[:, qi * 128:(qi + 1) * 128], VA[:, kt, h, :], (kt == 0 and qi == 0),
                           (kt == 2 * qb + qi), [("PT", slot), ("VA", kt // 4), "VAone"], bk(ob), sgc=True)
                    if kt == nkt - 1:
                        rc = rec2[ob % 2]
                        rk = ("rec", ob % 2)
                        P.op("dve", lambda e, O=O, rc=rc: e.reciprocal(out=rc.rearrange("p (q c) -> p q c", c=1), in_=O[:, :, 64:65]),
                             bk(ob), [rk])
                        for qi in range(2):
                            t = qb * 2 + qi
                            ts("dve", otok[:, t, h * 64:(h + 1) * 64], O[:, qi, 0:64], rc[:, qi:qi + 1], None,
                               ALU.mult, None, bk(ob) + [rk], [("otok", t, h)])
                        if h == 1:
                            emit_tail(qb)

                LAG = 4
                for i in range(len(tiles) + LAG):
                    if i == len(early):
                        emit_bias_T()
                    if i < len(tiles):
                        emit_st(i)
                    if i >= LAG:
                        emit_pv(i - LAG)

            def ret_unit(r):
                u = 4 + r
                sl = u % 2
                W = WIN[sl]
                wk = ("WIN", sl)
                o = U
                raS = [carve(256, at=o + i * 256) for i in range(2)]; o += 512
                rbS = [carve(256, at=o + i * 256) for i in range(2)]; o += 512
                rrS = [carve(256, at=o + i * 256) for i in range(2)]; o += 512
                qd = carve(256, BF16, at=o).rearrange("p (i c) -> p i c", c=128); o += 256
                ki = carve(256, BF16, at=o).rearrange("p (i c) -> p i c", c=128); o += 256
                kd = carve(1024, BF16, at=o).rearrange("p (t c) -> p t c", c=128); o += 1024
                vr = carve(1024, BF16, at=o).rearrange("p (t c) -> p t c", c=128); o += 1024
                sg = carve(1024, BF16, at=o).rearrange("p (t c) -> p t c", c=128); o += 1024
                qdT = carve(1024, BF16, at=o); o += 1024
                kiT = carve(1024, BF16, at=o); o += 1024
                PTr = [carve(64, BF16, at=o + i * 64) for i in range(2)]; o += 128
                stf = [carve(128, at=o + i * 128) for i in range(2)]; o += 256
                stb = [carve(64, BF16, at=o + i * 64) for i in range(2)]; o += 128
                Of = carve(2048, at=o).rearrange("p (t c) -> p t c", c=128); o += 2048
                on = [carve(64, BF16, at=o + i * 64) for i in range(2)]; o += 128
                sqr = carve(64, BF16, at=o); o += 64
                ssr = carve(NT, at=o); o += NT
                rsr = carve(NT, at=o); o += NT
                assert o <= TOT
                dq = dec[:, r * 3 + 0:r * 3 + 1]
                dki = dec[:, r * 3 + 1:r * 3 + 2]
                dkd = dec[:, r * 3 + 2:r * 3 + 3]
                gamma_c = float((1.0 - 2.0 ** (-5.0 - r)) ** 128)
                def emit_inproj_tile(t):
                    tq, i = t // 4, t % 4
                    b = t % 2
                    pp = bankv(b, 512)
                    for kc in range(KC):
                        mm(pp, HT[:, kc, t * 128:(t + 1) * 128], W[:, kc, :], kc == 0, kc == KC - 1,
                           [wk, ("HT", kc, tq)], bk(b))
                    bkk = bk(b)
                    ra, rb, rr = raS[t % 2], rbS[t % 2], rrS[t % 2]
                    p2 = t % 2
                    QK = pp[:, 0:256].rearrange("p (a h c) -> p a h c", a=2, h=2)
                    ra4 = ra.rearrange("p (a h c) -> p a h c", a=2, h=2)
                    rb3 = rb.rearrange("p (a h c) -> p a h c", a=2, h=2)
                    rr4 = rr.rearrange("p (a h c) -> p a h c", a=2, h=2)
                    cb4 = cosT[:, t, :].unsqueeze(1).unsqueeze(1).broadcast_to([128, 2, 2, 64])
                    sb3 = sinT[:, t, :].unsqueeze(1).broadcast_to([128, 2, 64])
                    tt("dve", ra4, QK, cb4, ALU.mult, bkk + ["cos"], [("ra", p2)])
                    tt("dve", rb3[:, :, 0, :], QK[:, :, 1, :], sb3, ALU.mult, bkk + ["sin"], [("rb0", p2)])
                    tt("dve", rb3[:, :, 1, :], QK[:, :, 0, :], sb3, ALU.mult, bkk + ["sin"], [("rb1", p2)])
                    tt("dve", rr4[:, :, 0, :], ra4[:, :, 0, :], rb3[:, :, 0, :], ALU.subtract, [("ra", p2), ("rb0", p2)], [("rr0", p2)])
                    tt("dve", rr4[:, :, 1, :], ra4[:, :, 1, :], rb3[:, :, 1, :], ALU.add, [("ra", p2), ("rb1", p2)], [("rr1", p2)])
                    rk = [("rr0", p2), ("rr1", p2)]
                    ts("pool", qd[:, i, :], rr[:, 0:128], dq, 0.0, ALU.mult, ALU.add, rk + ["dec"], [("qd", i)])
                    ts("pool", ki[:, i, :], rr[:, 128:256], dki, 0.0, ALU.mult, ALU.add, rk + ["dec"], [("ki", i)])
                    ts("pool", kd[:, t, :], rr[:, 128:256], dkd, 0.0, ALU.mult, ALU.add, rk + ["dec"], [("kd", t)])
                    cp("act", vr[:, t, :], pp[:, 256:384], bkk, [("vr", t)])
                    act(sg[:, t, :], pp[:, 384:512], AF.Silu, bkk, [("sg", t)])
                    if i == 3:
                        for src, dst, nm in ((qd, qdT, "qdT"), (ki, kiT, "kiT")):
                            bb = 0 if nm == "qdT" else 1
                            pT = bankv(bb, 256, BF16)
                            for ii in range(4):
                                tr(pT[:, ii * 128:(ii + 1) * 128], src[:, ii, :], [(nm[:2], ii)], bk(bb))
                            cp("act" if nm == "qdT" else "dve", dst[:, tq * 512:(tq + 1) * 512], pT, bk(bb), [(nm, tq)])

                for t in range(4):
                    emit_inproj_tile(t)
                def emit_sc(n):
                    cs_ = slice(n * 128, (n + 1) * 128)
                    sl2 = n % 2
                    SC = bankv(2 + sl2, 128)
                    mm(SC, kiT[:, cs_], qdT[:, cs_], True, True, [("kiT", n // 4), ("qdT", n // 4)], [(2 + sl2, 0)])
                    tt("dve", PTr[sl2], SC, m01, ALU.mult, [(2 + sl2, 0), "m01"], [("PTr", sl2)])
                    if n < NT - 1:
                        KV = bankv(6 + sl2, 128)
                        mm(KV, kd[:, n, :], vr[:, n, :], True, True, [("kd", n), ("vr", n)], [(6 + sl2, 0)])

                emit_sc(0)
                for n in range(NT):
                    cs_ = slice(n * 128, (n + 1) * 128)
                    sl2 = n % 2
                    if n + 4 < NT:
                        emit_inproj_tile(n + 4)
                    if n + 1 < NT:
                        emit_sc(n + 1)
                    OB = bankv(4 + sl2, 128)
                    mm(OB, PTr[sl2], vr[:, n, :], True, n == 0, [("PTr", sl2), ("vr", n)], [(4 + sl2, 0)])
                    if n > 0:
                        mm(OB, qdT[:, cs_], stb[(n - 1) % 2], False, True, [("qdT", n // 4), ("stb", (n - 1) % 2)],
                           [(4 + sl2, 0)])
                    if n < NT - 1:
                        KV = bankv(6 + sl2, 128)
                        if n == 0:
                            cp("dve", stf[0], KV, [(6 + sl2, 0)], [("stf", 0)])
                        else:
                            stt(stf[n % 2], stf[(n - 1) % 2], gamma_c, KV, ALU.mult, ALU.add,
                                [("stf", (n - 1) % 2), (6 + sl2, 0)], [("stf", n % 2)])
                        cp("pool", stb[n % 2], stf[n % 2], [("stf", n % 2)], [("stb", n % 2)])
                    cp("act", Of[:, n, :], OB, [(4 + sl2, 0)], [("Of", n)])
                    act(sqr, OB, AF.Square, [(4 + sl2, 0)], ["sqr", "ssr"], accum_out=ssr[:, n:n + 1])
                ts("dve", rsr, ssr, 1.0 / 128, EPS, ALU.mult, ALU.add, ["ssr"], ["rsr"])
                act(rsr, rsr, AF.Sqrt, ["rsr"], ["rsr"])
                P.op("dve", lambda e: e.reciprocal(out=rsr, in_=rsr), ["rsr"], ["rsr"])
                for tq in range(4):
                    b = 6 + tq % 2
                    pT = bankv(b, 256, BF16)
                    for i in range(4):
                        t = tq * 4 + i
                        stt(on[t % 2], Of[:, t, :], rsr[:, t:t + 1], sg[:, t, :], ALU.mult, ALU.mult,
                            [("Of", t), "rsr", ("sg", t)], [("on", t % 2)])
                        tr(pT[:, i * 128:(i + 1) * 128], on[t % 2], [("on", t % 2)], bk(b))
                    ts("dve", CT[:, 4 + r, tq * 512:(tq + 1) * 512], pT, rog[:, l * 4 + r:l * 4 + r + 1], None, ALU.mult, None,
                       bk(b) + ["rog"], [("CT", 4 + r, tq)])

            load_unit(0)
            for u in range(8):
                if u + 1 < 8:
                    load_unit(u + 1)
                if u < 4:
                    att_unit(u)
                else:
                    ret_unit(u - 4)
                tap(f"CT{u}_{l}", CT[:, u, :], [("CT", u, tq) for tq in range(4)])
                ck(f"u{u}_{l}")
            castload(6, WO[:, 0:4, :], wout_d[l, :, 0:4, :], (), [("WIN", 0)])
            castload(7, WO[:, 4:8, :], wout_d[l, :, 4:8, :], (), [("WIN", 1)])
            for h in range(2):
                tt("pool", WO[:, 4 * h:4 * h + 4, :], WO[:, 4 * h:4 * h + 4, :], GBC.unsqueeze(1).broadcast_to([128, 4, D]), ALU.mult,
                   [("WIN", h), "GBC"], [("WIN", h)])
            P.op("dve", lambda e: e.tensor_reduce(out=rstda, in_=ssa.rearrange("p (u t) -> p t u", t=NT), axis=AX.X, op=ALU.add),
                 [("ssa", j) for j in range(4)], ["rstda"])
            ts("dve", rstda, rstda, 1.0 / 512, EPS, ALU.mult, ALU.add, ["rstda"], ["rstda"])
            act(rstda, rstda, AF.Sqrt, ["rstda"], ["rstda"])
            P.op("dve", lambda e: e.reciprocal(out=rstda, in_=rstda), ["rstda"], ["rstda"])
            bi = 0
            for t in range(NT):
                for hf in range(2):
                    xs = X[:, t, hf * 512:(hf + 1) * 512]
                    b = bi % 4; bi += 1
                    for kc in range(4):
                        mm(bankv(b, 512), CT[:, kc, t * 128:(t + 1) * 128], WO[:, kc, hf * 512:(hf + 1) * 512],
                           kc == 0, kc == 3, [("CT", kc, t // 4), ("WIN", 0)], bk(b))
                    stt(xs, bankv(b, 512), rstda[:, t:t + 1], xs, ALU.mult, ALU.add, bk(b) + ["rstda", ("X", t)], [("X", t)])
                    b = bi % 4; bi += 1
                    for kc in range(4, 8):
                        mm(bankv(b, 512), CT[:, kc, t * 128:(t + 1) * 128], WO[:, kc, hf * 512:(hf + 1) * 512],
                           kc == 4, kc == 7, [("CT", kc, t // 4), ("WIN", 1)], bk(b))
                    tt("dve", xs, bankv(b, 512), xs, ALU.add, bk(b) + [("X", t)], [("X", t)])

        GROUPS = [(0, 4), (4, 4), (8, 4), (12, 4), (16, 4), (20, 2)]

        def ffn(experts, stage):
            o = PH
            AT = carve(4096, BF16, at=o).rearrange("p (j s) -> p j s", s=S); o += 4096
            WG = [carve(2048, BF16, at=o + i * 6144).rearrange("p (k c) -> p k c", c=512) for i in range(2)]
            WU = [carve(2048, BF16, at=o + 2048 + i * 6144).rearrange("p (k c) -> p k c", c=512) for i in range(2)]
            WD = [carve(2048, BF16, at=o + 4096 + i * 6144).rearrange("p (j c) -> p j c", c=D) for i in range(2)]
            o += 12288
            SG = [carve(512, at=o + i * 512) for i in range(2)]; o += 1024
            assert o <= TOT
            work = [(e, g) for e in range(len(experts)) for g in range(len(GROUPS))]

            def load(idx):
                e, g = work[idx]
                gd, ud, dd, _ = experts[e]
                j0, nj = GROUPS[g]
                sl = idx % 2
                castload(8 + sl, WG[sl][:, :, 0:nj * 128], gd[:, :, j0 * 128:(j0 + nj) * 128], (), [("WG", sl)])
                castload(10 + sl, WU[sl][:, :, 0:nj * 128], ud[:, :, j0 * 128:(j0 + nj) * 128], (), [("WU", sl)])
                castload(12 + sl, WD[sl][:, 0:nj, :], dd[:, j0:j0 + nj, :], (), [("WD", sl)])
                tt("pool", WD[sl][:, 0:nj, :], WD[sl][:, 0:nj, :], GBC.unsqueeze(1).broadcast_to([128, nj, D]), ALU.mult,
                   [("WD", sl), "GBC"], [("WD", sl)])

            if stage == "pre":
                load(0)
                return
            gi = 0
            di = 0
            for idx, (e, g) in enumerate(work):
                if idx + 1 < len(work):
                    load(idx + 1)
                sl = idx % 2
                j0, nj = GROUPS[g]
                wcol = experts[e][3]
                for tc in range(4):
                    for j in range(nj):
                        bg = gi % 2
                        bu = 2 + gi % 2
                        gi += 1
                        for kc in range(KC):
                            mm(bankv(bg, 512), WG[sl][:, kc, j * 128:(j + 1) * 128], HT[:, kc, tc * 512:(tc + 1) * 512],
                               kc == 0, kc == KC - 1, [("WG", sl), ("HT", kc, tc)], bk(bg))
                        for kc in range(KC):
                            mm(bankv(bu, 512), WU[sl][:, kc, j * 128:(j + 1) * 128], HT[:, kc, tc * 512:(tc + 1) * 512],
                               kc == 0, kc == KC - 1, [("WU", sl), ("HT", kc, tc)], bk(bu))
                        sgt = SG[bg]
                        act(sgt, bankv(bg, 512), AF.Silu, bk(bg), [("SG", bg)])
                        tt("dve", AT[:, j, tc * 512:(tc + 1) * 512], sgt, bankv(bu, 512), ALU.mult,
                           [("SG", bg)] + bk(bu), [("AT", j, tc)])
                for t in range(NT):
                    for hf in range(2):
                        b = 4 + di % 4
                        di += 1
                        for j in range(nj):
                            mm(bankv(b, 512), AT[:, j, t * 128:(t + 1) * 128], WD[sl][:, j, hf * 512:(hf + 1) * 512],
                               j == 0, j == nj - 1, [("AT", j, t // 4), ("WD", sl)], bk(b))
                        xs = X[:, t, hf * 512:(hf + 1) * 512]
                        if wcol is None:
                            tt("dve", xs, bankv(b, 512), xs, ALU.add, bk(b) + [("X", t)], [("X", t)])
                        else:
                            stt(xs, bankv(b, 512), wgt[:, t * NE + wcol:t * NE + wcol + 1], xs, ALU.mult, ALU.add,
                                bk(b) + [("X", t), "wgt"], [("X", t)])

        def router():
            lp = bankv(7, 128)
            for t in range(NT):
                for kc in range(KC):
                    mm(lp[:, t * 8:(t + 1) * 8], HT[:, kc, t * 128:(t + 1) * 128], rwb[:, kc, :], kc == 0, kc == KC - 1,
                       [("HT", kc, t // 4), "rwb"], bk(7))
            cp("dve", lg, lp, bk(7), ["lg"])
            l3 = lg.rearrange("p (t e) -> p t e", e=NE)
            a3 = r1.rearrange("p (t e) -> p t e", e=NE)
            b3 = r2.rearrange("p (t e) -> p t e", e=NE)
            w3 = wgt.rearrange("p (t e) -> p t e", e=NE)
            m1b = r3.unsqueeze(2).broadcast_to([128, NT, NE])
            m2b = r4.unsqueeze(2).broadcast_to([128, NT, NE])
            P.op("dve", lambda e: e.tensor_reduce(out=r3, in_=l3, axis=AX.X, op=ALU.max), ["lg"], ["r3"])
            tt("dve", a3, l3, m1b, ALU.is_equal, ["lg", "r3"], ["r1"])
            stt(b3, a3, -1e30, l3, ALU.mult, ALU.add, ["r1", "lg"], ["r2"])
            P.op("dve", lambda e: e.tensor_reduce(out=r4, in_=b3, axis=AX.X, op=ALU.max), ["r2"], ["r4"])
            tt("dve", a3, l3, m2b, ALU.is_ge, ["lg", "r4", "r1"], ["r1"])
            tt("dve", b3, l3, m1b, ALU.subtract, ["lg", "r3", "r2"], ["r2"])
            act(r2, r2, AF.Exp, ["r2"], ["r2"])
            tt("dve", b3, b3, a3, ALU.mult, ["r2", "r1"], ["r2"])
            P.op("dve", lambda e: e.tensor_reduce(out=r3, in_=b3, axis=AX.X, op=ALU.add), ["r2", "r3"], ["r3"])
            P.op("dve", lambda e: e.reciprocal(out=r3, in_=r3), ["r3"], ["r3"])
            tt("dve", w3, b3, m1b, ALU.mult, ["r2", "r3"], ["wgt"])

        def ck(name):
            if stop == name:
                raise StopBuild()

        def tap(name, ap, keys):
            if name not in tap_d:
                return
            dt_ = tap_d[name]
            P.dma("sp", 3, lambda e: e.dma_start(out=dt_, in_=ap), list(keys), [("dbg", "tap", name)])

        try:
            ck("consts")
            for l in range(DEPTH):
                compute_mod(l)
                tap(f"modT{l}", modT, ["modT"])
                ck(f"mod{l}")
                make_gbc(modT[:, 16:24], "modT")
                tap(f"gbc{l}", GBC, ["GBC"])
                ck(f"gbc{l}")
                make_hT(s1, modT[:, 0:8], ["s1", "modT"])
                tap(f"hT{l}", HT.rearrange("p k s -> p (k s)"), HT_ALL)
                ck(f"hT{l}")
                barrier()
                mixer(l)
                dump(f"mix{l}")
                ck(f"mix{l}")
                barrier()
                make_gbc(modT[:, 40:48], "modT")
                if l % 2 == 0:
                    experts = [(fg_d, fu_d, fd_d, None)]
                else:
                    experts = [(mg_d[e], mu_d[e], md_d[e], e) for e in range(NE)]
                ffn(experts, "pre")
                make_hT(s2, modT[:, 24:32], ["s2", "modT"])
                barrier()
                if l % 2 == 1:
                    router()
                ffn(experts, "main")
                dump(f"ffn{l}")
                ck(f"ffn{l}")
                barrier()
        except StopBuild:
            barrier()
        make_gbc(fng, "fng")
        yt = [carve(D, at=PH + 8000 + i * D) for i in range(2)]
        junk = carve(512, BF16, at=PH)
        for t in range(NT):
            act(junk, X[:, t, :], AF.Square, [("X", t)], ["junk", "ssf"], accum_out=ss[:, t:t + 1])
        ts("dve", rstd, ss, 1.0 / D, EPS, ALU.mult, ALU.add, ["ssf"], ["rstdf"])
        act(rstd, rstd, AF.Sqrt, ["rstdf"], ["rstdf"])
        P.op("dve", lambda e: e.reciprocal(out=rstd, in_=rstd), ["rstdf"], ["rstdf"])
        for t in range(NT):
            stt(yt[t % 2], X[:, t, :], rstd[:, t:t + 1], GBC, ALU.mult, ALU.mult, [("X", t), "rstdf", "GBC"], [("yt", t % 2)])
            P.dma("sp", 14 + t % 2, lambda e, t=t: e.dma_start(out=y_d[t * 128:(t + 1) * 128, :], in_=yt[t % 2]),
                  [("yt", t % 2)], [("y", t)])
        P.final_wait("sp", [("y", t) for t in range(NT)] + [k for k in P.lastw if isinstance(k, tuple) and k[0] == "dbg"])

        with nc.Block() as block:
            @block.sync
            def _(e):
                P.replay("sp", e)

            @block.tensor
            def _(e):
                P.replay("pe", e)

            @block.vector
            def _(e):
                P.replay("dve", e)

            @block.scalar
            def _(e):
                P.replay("act", e)

            @block.gpsimd
            def _(e):
                P.replay("pool", e)
    return nc


def host_consts():
    f = np.float32
    c = {}
    c["idn"] = np.eye(128, dtype=f)
    half = 64
    inv_freq = (10000.0 ** (-np.arange(half, dtype=np.float32) / half)).astype(f)
    pos = np.arange(S, dtype=np.float32)
    ang = (pos[:, None] * inv_freq[None, :]).astype(f)
    cos = np.cos(ang).astype(f).reshape(NT, 128, half).transpose(1, 0, 2).reshape(128, NT * half)
    sin = np.sin(ang).astype(f).reshape(NT, 128, half).transpose(1, 0, 2).reshape(128, NT * half)
    c["cos"] = np.ascontiguousarray(cos)
    c["sin"] = np.ascontiguousarray(sin)
    dec = np.zeros((128, 12), f)
    idx = np.arange(128, dtype=np.float64)
    for r in range(4):
        lgm = np.log(1.0 - 2.0 ** (-5.0 - r))
        dec[:, r * 3 + 0] = np.exp((idx + 1.0) * lgm)
        dec[:, r * 3 + 1] = np.exp(-(idx + 1.0) * lgm) * (128.0 ** -0.5)
        dec[:, r * 3 + 2] = np.exp((127.0 - idx) * lgm) * (128.0 ** -0.5)
    c["dec"] = dec
    kk = np.arange(128)
    c["m01"] = (kk[None, :] >= kk[:, None]).astype(f)
    c["mneg"] = np.where(kk[:, None] > kk[None, :], NEG, 0.0).astype(f)
    en = np.zeros((128, 8, 128), f)
    for n in range(8):
        en[n, n, :] = 1.0
        en[64 + n, n, :] = 1.0
    c["en"] = en.reshape(128, 8 * 128)
    past = np.zeros((2, NT, 8), f)
    nb = np.zeros((2, NT, 8), f)
    for t in range(NT):
        qb = t // 2
        for n in range(8):
            past[:, t, n] = 0.0 if n < qb else -1e30
            nb[:, t, n] = 0.0 if n == qb else NEG
    c["past"] = np.ascontiguousarray(np.broadcast_to(past.reshape(1, 256), (128, 256)))
    c["nb"] = np.ascontiguousarray(np.broadcast_to(nb.reshape(1, 256), (128, 256)))
    return c


def fm(v, n):
    return np.ascontiguousarray(np.asarray(v, np.float32).reshape(n, 128).T)


def prep_shared(inp):
    f = np.float32
    sh = dict(host_consts())
    sh["nmg"] = np.concatenate([fm(inp["norm_mix_g"][l], KC) for l in range(DEPTH)], axis=1)
    sh["nfg"] = np.concatenate([fm(inp["norm_ffn_g"][l], KC) for l in range(DEPTH)], axis=1)
    sh["aog"] = np.concatenate([fm(inp["att_out_g"][l], 4) for l in range(DEPTH)], axis=1)
    sh["rog"] = np.concatenate([fm(inp["ret_out_g"][l], 4) for l in range(DEPTH)], axis=1)
    sh["fng"] = fm(inp["final_norm_g"], KC)
    sh["adaw"] = np.ascontiguousarray(np.asarray(inp["ada_w"], f).reshape(DEPTH, KC, 128, 6 * D))
    sh["adabr"] = np.ascontiguousarray(np.asarray(inp["ada_b"], f))
    sh["adab"] = np.concatenate([fm(inp["ada_b"][l], 48) for l in range(DEPTH)], axis=1)
    cols = []
    for j in range(4):
        for w in range(3):
            cols += list(range(w * 512 + j * 128, w * 512 + (j + 1) * 128))
    for r in range(4):
        for w in range(4):
            cols += list(range(1536 + w * 512 + r * 128, 1536 + w * 512 + (r + 1) * 128))
    cols = np.array(cols)

    def pk(w):
        w = np.asarray(w, f)
        k = w.shape[0] // 128
        return np.ascontiguousarray(w.reshape(k, 128, w.shape[1]).transpose(1, 0, 2))
    sh["win"] = np.stack([pk(np.asarray(inp["w_in"][l], f)[:, cols]) for l in range(DEPTH)])
    sh["wout"] = np.stack([pk(inp["w_out"][l]) for l in range(DEPTH)])
    sh["fg"] = pk(inp["ffn_w_gate"][0])
    sh["fu"] = pk(inp["ffn_w_up"][0])
    sh["fd"] = pk(inp["ffn_w_down"][0])
    sh["rw"] = pk(inp["router_w"][0]).reshape(128, KC * NE)
    sh["mg"] = np.stack([pk(inp["moe_w_gate"][0][e]) for e in range(NE)])
    sh["mu"] = np.stack([pk(inp["moe_w_up"][0][e]) for e in range(NE)])
    sh["md"] = np.stack([pk(inp["moe_w_down"][0][e]) for e in range(NE)])
    return sh


def kernel(**inputs):
    inp = {k: np.asarray(v) for k, v in inputs.items()}
    sh = prep_shared(inp)
    nc = build_nc()
    in_maps = []
    for b in range(8):
        m = dict(sh)
        m["x"] = np.ascontiguousarray(inp["x"][b], dtype=np.float32)
        m["cT"] = fm(inp["c"][b], KC)
        in_maps.append(m)
    res = run_bass_kernel_spmd(nc, in_maps, core_ids=list(range(8)))
    return np.stack([np.asarray(r["y"], dtype=np.float32) for r in res.results], axis=0)
```

```python
import numpy as np
from contextlib import ExitStack
import concourse.bass as bass
import concourse.mybir as mybir
from concourse.bass_utils import run_bass_kernel_spmd
from concourse.alu_op_type import AluOpType as ALU

F32 = mybir.dt.float32
BF16 = mybir.dt.bfloat16
AF = mybir.ActivationFunctionType
AX = mybir.AxisListType

DEPTH = 2
S = 2048
D = 1024
NT = 16
KC = 8
DFF = 2816
NFC = 22
NE = 8
EPS = 1e-6
NEG = -240000.0
SAME_ENGINE_SYNC = True


class Prog:
    ENG = ["pe", "act", "dve", "pool", "sp"]

    def __init__(self, nc, sems, dma_sems):
        self.nc = nc
        self.items = {e: [] for e in self.ENG}
        self.sem = {e: sems[i] for i, e in enumerate(self.ENG)}
        self.cnt = {e: 0 for e in self.ENG}
        self.waited = {e: {} for e in self.ENG}
        self.lastw = {}
        self.readers = {}
        self.dma_sems = dma_sems
        self.dma_cnt = [0] * len(dma_sems)
        self.lock_last = {}

    @staticmethod
    def _locks(reads, writes):
        return {k[0] for k in list(reads) + list(writes)
                if isinstance(k, tuple) and len(k) == 2 and isinstance(k[0], int)}

    def _deps(self, eng, reads, writes):
        toks = {}

        def add(t):
            if t is None:
                return
            s, v = t
            k = id(s)
            if k not in toks or toks[k][1] < v:
                toks[k] = (s, v)
        for r in reads:
            add(self.lastw.get(r))
        for w in writes:
            add(self.lastw.get(w))
            for t in self.readers.get(w, {}).values():
                add(t)
        for b in self._locks(reads, writes):
            for e2, t in self.lock_last.get(b, {}).items():
                if e2 != eng:
                    add(t)
        out = []
        for k, (s, v) in toks.items():
            if s is self.sem[eng] and (eng == "pe" or not SAME_ENGINE_SYNC):
                continue
            if self.waited[eng].get(k, 0) < v:
                self.waited[eng][k] = v
                out.append((s, v))
        return out

    def _commit(self, tok, reads, writes):
        k = id(tok[0])
        for r in reads:
            d = self.readers.setdefault(r, {})
            if k not in d or d[k][1] < tok[1]:
                d[k] = tok
        for w in writes:
            self.lastw[w] = tok
            self.readers[w] = {}

    def op(self, eng, fn, reads=(), writes=()):
        for s, v in self._deps(eng, reads, writes):
            self.items[eng].append(("wait", s, v))
        self.cnt[eng] += 1
        self.items[eng].append(("op", fn, self.sem[eng], 1))
        tok = (self.sem[eng], self.cnt[eng])
        self._commit(tok, reads, writes)
        for b in self._locks(reads, writes):
            self.lock_last.setdefault(b, {})[eng] = tok

    def dma(self, eng, chan, fn, reads=(), writes=()):
        s = self.dma_sems[chan]
        deps = self._deps(eng, reads, writes)
        prev = self.dma_cnt[chan] * 16
        if prev and self.waited[eng].get(id(s), 0) < prev:
            self.waited[eng][id(s)] = prev
            deps.append((s, prev))
        for ss, v in deps:
            self.items[eng].append(("wait", ss, v))
        self.dma_cnt[chan] += 1
        self.items[eng].append(("op", fn, s, 16))
        self._commit((s, self.dma_cnt[chan] * 16), reads, writes)

    def final_wait(self, eng, keys):
        for s, v in self._deps(eng, keys, ()):
            self.items[eng].append(("wait", s, v))

    def replay(self, eng, e):
        for it in self.items[eng]:
            if it[0] == "wait":
                e.wait_ge(it[1], it[2])
            else:
                it[1](e).then_inc(it[2], it[3])


class StopBuild(Exception):
    pass


def build_nc(debug=(), stop=None, taps=()):
    nc = bass.Bass("TRN2", target_bir_lowering=False)

    def din(name, shape):
        return nc.dram_tensor(name, list(shape), F32, kind="ExternalInput").ap()
    x_d = din("x", [S, D])
    cT_d = din("cT", [128, KC])
    nmg_d = din("nmg", [128, DEPTH * KC])
    nfg_d = din("nfg", [128, DEPTH * KC])
    aog_d = din("aog", [128, DEPTH * 4])
    rog_d = din("rog", [128, DEPTH * 4])
    fng_d = din("fng", [128, KC])
    adaw_d = din("adaw", [DEPTH, KC, 128, 6 * D])
    adab_d = din("adab", [128, DEPTH * 48])
    adabr_d = din("adabr", [DEPTH, 6 * D])
    win_d = din("win", [DEPTH, 128, KC, 3584])
    wout_d = din("wout", [DEPTH, 128, KC, D])
    fg_d = din("fg", [128, KC, DFF])
    fu_d = din("fu", [128, KC, DFF])
    fd_d = din("fd", [128, NFC, D])
    rw_d = din("rw", [128, KC * NE])
    mg_d = din("mg", [NE, 128, KC, DFF])
    mu_d = din("mu", [NE, 128, KC, DFF])
    md_d = din("md", [NE, 128, NFC, D])
    idn_d = din("idn", [128, 128])
    cos_d = din("cos", [128, NT * 64])
    sin_d = din("sin", [128, NT * 64])
    dec_d = din("dec", [128, 12])
    m01_d = din("m01", [128, 128])
    mneg_d = din("mneg", [128, 128])
    en_d = din("en", [128, 8 * 128])
    past_d = din("past", [128, 256])
    nb_d = din("nb", [128, 256])
    y_d = nc.dram_tensor("y", [S, D], F32, kind="ExternalOutput").ap()
    tap_d = {}
    for name, (shp, dt_) in dict(taps).items():
        tap_d[name] = nc.dram_tensor("tap_" + name, list(shp), dt_, kind="ExternalOutput").ap()
    dbg_d = {}
    for name in debug:
        dbg_d[name] = nc.dram_tensor("dbg_" + name, [S, D], F32, kind="ExternalOutput").ap()

    with ExitStack() as st:
        TOT = 52800
        A = st.enter_context(nc.sbuf_tensor("arena", [128, TOT], F32))
        banks = [st.enter_context(nc.psum_tensor(f"bank{i}", [128, 512], F32)) for i in range(8)]
        sems = [st.enter_context(nc.semaphore(f"es{i}")) for i in range(5)]
        NCH = 24
        dsems = [st.enter_context(nc.semaphore(f"ds{i}")) for i in range(NCH)]
        P = Prog(nc, sems, dsems)

        cur = [0]

        def carve(n, dt=F32, at=None):
            if at is None:
                off = cur[0]
                cur[0] += n
            else:
                off = at
            assert off + n <= TOT, (off, n)
            ap = A[:, off:off + n]
            if dt != F32:
                ap = ap.bitcast(dt)
            return ap

        X = carve(NT * D).rearrange("p (t d) -> p t d", d=D)
        HT = carve(KC * S // 2, BF16).rearrange("p (k s) -> p k s", s=S)
        identb = carve(64, BF16)
        identf = carve(128)
        onesf = carve(128)
        cosT = carve(NT * 64).rearrange("p (t c) -> p t c", c=64)
        sinT = carve(NT * 64).rearrange("p (t c) -> p t c", c=64)
        dec = carve(12)
        m01 = carve(64, BF16)
        mneg = carve(64, BF16)
        EN = carve(512, BF16).rearrange("p (n k) -> p n k", k=128)
        pastm = carve(256)
        NB = carve(256)
        modT = carve(48)
        adab = carve(DEPTH * 48)
        nmg = carve(DEPTH * KC)
        nfg = carve(DEPTH * KC)
        aog = carve(DEPTH * 4)
        rog = carve(DEPTH * 4)
        fng = carve(KC)
        s1 = carve(KC)
        s2 = carve(KC)
        GBC = carve(D)
        cTt = carve(KC)
        csb = carve(KC // 2, BF16)
        csf = carve(KC)
        ss = carve(NT)
        rstd = carve(NT)
        tmp16 = carve(NT)
        ssa = carve(4 * NT)
        rstda = carve(NT)
        rwb = carve(KC * NE // 2, BF16).rearrange("p (k e) -> p k e", e=NE)
        lg = carve(NT * NE)
        wgt = carve(NT * NE)
        r1 = carve(NT * NE)
        r2 = carve(NT * NE)
        r3 = carve(NT)
        r4 = carve(NT)
        PH = cur[0]
        PHN = TOT - PH

        def bankv(b, n, dt=F32, off=0):
            ap = banks[b][:, off:off + n]
            if dt != F32:
                ap = ap.bitcast(dt)
            return ap

        def bk(b):
            return [(b, 0), (b, 1)]

        def mm(out, lhsT, rhs, start, stop, reads, writes, sgc=False):
            P.op("pe", lambda e: e.matmul(out, lhsT=lhsT, rhs=rhs, start=start, stop=stop, skip_group_check=sgc),
                 reads, writes)

        def tr(out, in_, reads, writes):
            P.op("pe", lambda e: e.transpose(out, in_, identb), list(reads) + ["identb"], writes)

        def act(out, in_, func, reads, writes, **kw):
            P.op("act", lambda e: e.activation(out=out, in_=in_, func=func, **kw), reads, writes)

        def tt(eng, out, in0, in1, op, reads, writes):
            P.op(eng, lambda e: e.tensor_tensor(out=out, in0=in0, in1=in1, op=op), reads, writes)

        def ts(eng, out, in0, s1_, s2_, op0, op1, reads, writes):
            if op1 is None:
                P.op(eng, lambda e: e.tensor_scalar(out=out, in0=in0, scalar1=s1_, scalar2=None, op0=op0), reads, writes)
            else:
                P.op(eng, lambda e: e.tensor_scalar(out=out, in0=in0, scalar1=s1_, scalar2=s2_, op0=op0, op1=op1), reads, writes)

        def stt(out, in0, scalar, in1, op0, op1, reads, writes):
            P.op("dve", lambda e: e.scalar_tensor_tensor(out=out, in0=in0, scalar=scalar, in1=in1, op0=op0, op1=op1),
                 reads, writes)

        def cp(eng, out, in_, reads, writes):
            if eng == "act":
                act(out, in_, AF.Copy, reads, writes)
            else:
                P.op(eng, lambda e: e.tensor_copy(out=out, in_=in_), reads, writes)

        cch = [0]

        def cload(out, in_, key, eng="sp"):
            ch = 16 + (cch[0] % 4)
            cch[0] += 1
            P.dma(eng, ch, lambda e: e.dma_start(out=out, in_=in_), (), [key])

        def castload(chan, out, in_, reads, writes):
            P.dma("pool", chan, lambda e: e.dma_start(out=out, in_=in_, max_dma_last_dim=4096), reads, writes)

        cload(identf, idn_d[:, :], "identf")
        cload(cosT, cos_d[:, :].rearrange("p (t c) -> p t c", c=64), "cos")
        cload(sinT, sin_d[:, :].rearrange("p (t c) -> p t c", c=64), "sin")
        cload(dec, dec_d[:, :], "dec")
        cload(pastm, past_d[:, :], "past")
        cload(NB, nb_d[:, :], "NB")
        cload(adab, adab_d[:, :], "adab")
        cload(nmg, nmg_d[:, :], "nmg")
        cload(nfg, nfg_d[:, :], "nfg")
        cload(aog, aog_d[:, :], "aog")
        cload(rog, rog_d[:, :], "rog")
        cload(fng, fng_d[:, :], "fng")
        cload(cTt, cT_d[:, :], "cT")
        castload(20, identb, idn_d[:, :], (), ["identb"])
        castload(21, m01, m01_d[:, :], (), ["m01"])
        castload(22, mneg, mneg_d[:, :], (), ["mneg"])
        castload(23, EN.rearrange("p n k -> p (n k)"), en_d[:, :], (), ["EN"])
        castload(20, rwb.rearrange("p k e -> p (k e)"), rw_d[:, :], (), ["rwb"])
        P.op("dve", lambda e: e.memset(onesf, 1.0), (), ["onesf"])
        for t4 in range(4):
            P.dma("sp", t4, lambda e, t4=t4: e.dma_start(
                out=X[:, 4 * t4:4 * t4 + 4, :],
                in_=x_d[512 * t4:512 * (t4 + 1), :].rearrange("(t p) d -> p t d", p=128)),
                (), [("X", 4 * t4 + i) for i in range(4)])
        act(csb, cTt, AF.Silu, ["cT"], ["csb"])
        act(csf, cTt, AF.Silu, ["cT"], ["csf"])

        def barrier():
            alle = ["pe", "act", "dve", "pool", "sp"]
            for e in alle:
                for o in alle:
                    if o == e or P.cnt[o] == 0:
                        continue
                    k = id(P.sem[o])
                    if P.waited[e].get(k, 0) < P.cnt[o]:
                        P.waited[e][k] = P.cnt[o]
                        P.items[e].append(("wait", P.sem[o], P.cnt[o]))
                for ch in range(NCH):
                    v = P.dma_cnt[ch] * 16
                    k = id(P.dma_sems[ch])
                    if v and P.waited[e].get(k, 0) < v:
                        P.waited[e][k] = v
                        P.items[e].append(("wait", P.dma_sems[ch], v))

        def dump(name):
            if name in dbg_d:
                for t4 in range(4):
                    P.dma("sp", 2 + (t4 % 2), lambda e, t4=t4: e.dma_start(
                        out=dbg_d[name][512 * t4:512 * (t4 + 1), :].rearrange("(t p) d -> p t d", p=128),
                        in_=X[:, 4 * t4:4 * t4 + 4, :]),
                        [("X", 4 * t4 + i) for i in range(4)], [("dbg", name, t4)])

        def compute_mod(l):
            ADA = [carve(3072, at=PH + 3000 + i * 3072) for i in range(2)]
            dtmp = carve(2048, at=PH + 9200)
            csrep = carve(KC * 128, at=PH + 11300).rearrange("p (k m) -> p k m", m=128)
            for kc in range(KC):
                ts("dve", csrep[:, kc, :], onesf, csf[:, kc:kc + 1], None, ALU.mult, None, ["onesf", "csf"], ["csrep"])
            it = 0
            for hf in range(2):
                for kc in range(KC):
                    sl = it % 2
                    it += 1
                    P.dma("sp", 4 + sl, lambda e, sl=sl, kc=kc, hf=hf: e.dma_start(
                        out=ADA[sl], in_=adaw_d[l, kc, :, hf * 3072:(hf + 1) * 3072]), (), [("ADA", sl)])
                    for j in range(6):
                        mm(bankv(j, 512), csrep[:, kc, :], ADA[sl][:, j * 512:(j + 1) * 512], kc == 0, kc == KC - 1,
                           [("ADA", sl), "csrep"], bk(j))
                for j in range(6):
                    d3 = dtmp[:, (j % 4) * 512:(j % 4 + 1) * 512].rearrange("p (a b) -> p a b", b=128)
                    tt("dve", d3, bankv(j, 512).rearrange("p (a b) -> p a b", b=128),
                       identf.unsqueeze(1).broadcast_to([128, 4, 128]), ALU.mult, bk(j) + ["identf"], [("dtmp", j % 4)])
                    c0 = hf * 24 + j * 4
                    P.op("dve", lambda e, d3=d3, c0=c0: e.tensor_reduce(out=modT[:, c0:c0 + 4], in_=d3, axis=AX.X, op=ALU.add),
                         [("dtmp", j % 4)], ["modT"])
            tt("dve", modT, modT, adab[:, l * 48:(l + 1) * 48], ALU.add, ["modT", "adab"], ["modT"])
            stt(s1, modT[:, 8:16], 1.0, nmg[:, l * KC:(l + 1) * KC], ALU.add, ALU.mult, ["modT", "nmg"], ["s1"])
            stt(s2, modT[:, 32:40], 1.0, nfg[:, l * KC:(l + 1) * KC], ALU.add, ALU.mult, ["modT", "nfg"], ["s2"])

        def make_gbc(gT, gkey):
            diag = carve(2 * 128, at=PH + 2600).rearrange("p (a b) -> p a b", b=128)
            for kc in range(KC):
                sl = kc % 2
                ts("dve", diag[:, sl, :], identf, gT[:, kc:kc + 1], None, ALU.mult, None,
                   ["identf", gkey], [("diag", sl)])
                b = 5 + kc // 4
                mm(bankv(b, 128, off=(kc % 4) * 128), onesf, diag[:, sl, :], True, True,
                   ["onesf", ("diag", sl)], bk(b))
            for h in range(2):
                cp("dve", GBC[:, h * 512:(h + 1) * 512], bankv(5 + h, 512), bk(5 + h), ["GBC"])

        def make_hT(sT, shT, skeys):
            junk = carve(512, BF16, at=PH)
            xn = carve(2048, BF16, at=PH + 512).rearrange("p (i d) -> p i d", d=D)
            for tq in range(4):
                for i in range(4):
                    t = tq * 4 + i
                    act(junk, X[:, t, :], AF.Square, [("X", t)], ["junk", ("ss", tq)], accum_out=ss[:, t:t + 1])
                q = slice(tq * 4, tq * 4 + 4)
                ts("dve", tmp16[:, q], ss[:, q], 1.0 / D, EPS, ALU.mult, ALU.add, [("ss", tq)], [("tmp16", tq)])
                act(tmp16[:, q], tmp16[:, q], AF.Sqrt, [("tmp16", tq)], [("tmp16", tq)])
                P.op("dve", lambda e, q=q: e.reciprocal(out=rstd[:, q], in_=tmp16[:, q]), [("tmp16", tq)], [("rstd", tq)])
                for i in range(4):
                    t = tq * 4 + i
                    if i % 2 == 0:
                        act(xn[:, i, :], X[:, t, :], AF.Identity, [("X", t), ("rstd", tq)], [("xn", i)], scale=rstd[:, t:t + 1])
                    else:
                        ts("pool", xn[:, i, :], X[:, t, :], rstd[:, t:t + 1], 0.0, ALU.mult, ALU.add,
                           [("X", t), ("rstd", tq)], [("xn", i)])
                for kc in range(KC):
                    b = kc % 4
                    pT = bankv(b, 256, BF16)
                    for i in range(4):
                        tr(pT[:, i * 128:(i + 1) * 128], xn[:, i, kc * 128:(kc + 1) * 128], [("xn", i)], bk(b))
                    o = HT[:, kc, tq * 512:(tq + 1) * 512]
                    if kc % 2 == 0:
                        act(o, pT, AF.Identity, bk(b) + skeys, [("HT", kc, tq)],
                            scale=sT[:, kc:kc + 1], bias=shT[:, kc:kc + 1])
                    else:
                        ts("dve", o, pT, sT[:, kc:kc + 1], shT[:, kc:kc + 1], ALU.mult, ALU.add,
                           bk(b) + skeys, [("HT", kc, tq)])

        HT_ALL = [("HT", kc, tq) for kc in range(KC) for tq in range(4)]

        def HTk(tq):
            return [("HT", kc, tq) for kc in range(KC)]

        def mixer(l):
            o = PH
            CT = carve(8192, BF16, at=o).rearrange("p (k s) -> p k s", s=S); o += 8192
            WIN = [carve(2048, BF16, at=o + i * 2048).rearrange("p (k c) -> p k c", c=512) for i in range(2)]
            WO = carve(4096, BF16, at=o).rearrange("p (k c) -> p k c", c=D); o += 4096
            U = o

            def load_unit(u):
                sl = u % 2
                if u < 4:
                    castload(6 + sl, WIN[sl][:, :, 0:384], win_d[l, :, :, u * 384:(u + 1) * 384], (), [("WIN", sl)])
                else:
                    r = u - 4
                    castload(6 + sl, WIN[sl][:, :, :], win_d[l, :, :, 1536 + r * 512:1536 + (r + 1) * 512], (), [("WIN", sl)])

            def att_unit(j):
                sl = j % 2
                W = WIN[sl]
                o = U
                QZ = [carve(1024, BF16, at=o + h * 1024) for h in range(2)]; o += 2048
                KZ = [carve(1024, BF16, at=o + h * 1024) for h in range(2)]; o += 2048
                VA = carve(1040, BF16, at=o).rearrange("p (t h c) -> p t h c", h=2, c=65); o += 1040
                bias2 = carve(1024, BF16, at=o).rearrange("p (t c) -> p t c", c=128); o += 1024
                PT = [carve(128, BF16, at=o + i * 128) for i in range(8)]; o += 1024
                otok = carve(1024, BF16, at=o).rearrange("p (t c) -> p t c", c=128); o += 1024
                gm = carve(256, at=o); o += 256
                cmp = carve(1024, at=o); o += 1024
                cnt = carve(256, at=o); o += 256
                kms = carve(8, at=o); o += 8
                kmbz = [carve(4, BF16, at=o + h * 4) for h in range(2)]; o += 8
                rec = carve(2, at=o); o += 2
                recb = carve(2, at=o); o += 2
                sq = carve(64, BF16, at=o); o += 64
                assert o <= TOT, o
                wk = ("WIN", sl)
                own = [slice(0, 64), slice(64, 128)]
                oth = [slice(64, 128), slice(0, 64)]
                aug = [slice(64, 72), slice(0, 8)]
                for h in range(2):
                    P.op("pool", lambda e, h=h: e.memset(QZ[h][oth[h], :], 0.0), (), [("QZz", h, 0), ("QZz", h, 1)])
                    P.op("pool", lambda e, h=h: e.memset(KZ[h][oth[h], :], 0.0), (), [("KZz", h)])
                    P.op("pool", lambda e, h=h: e.tensor_copy(
                        out=KZ[h][aug[h], :].rearrange("p (n r k) -> p n r k", r=2, k=128),
                        in_=EN[aug[h], :, :].unsqueeze(2).broadcast_to([8, 8, 2, 128])), ["EN", ("KZz", h)], [("KZz", h)])
                    P.op("pool", lambda e, h=h: e.memset(kmbz[h], 0.0), (), [("kmbz", h)])
                for which, dst, nm in ((0, QZ, "QZ"), (1, KZ, "KZ")):
                    for tc in range(4):
                        b = tc % 2
                        for kc in range(KC):
                            mm(bankv(b, 512), W[:, kc, which * 128:(which + 1) * 128], HT[:, kc, tc * 512:(tc + 1) * 512],
                               kc == 0, kc == KC - 1, [wk, ("HT", kc, tc)], bk(b))
                        for h in range(2):
                            cp("dve", dst[h][own[h], tc * 512:(tc + 1) * 512], bankv(b, 512)[own[h], :],
                               bk(b), [(nm, h, tc)])
                P.op("pool", lambda e: e.memset(VA[:, :, :, 64:65], 1.0), (), ["VAone"])
                for tq in range(4):
                    b = 2 + tq % 2
                    for i in range(4):
                        t = tq * 4 + i
                        for kc in range(KC):
                            mm(bankv(b, 128, off=i * 128), HT[:, kc, t * 128:(t + 1) * 128], W[:, kc, 256:384],
                               kc == 0, kc == KC - 1, [wk, ("HT", kc, tq)], bk(b))
                    cp("act" if tq % 2 == 0 else "dve", VA[:, tq * 4:tq * 4 + 4, :, 0:64],
                       bankv(b, 512).rearrange("p (t h c) -> p t h c", h=2, c=64), bk(b), [("VA", tq)])
                P.op("pool", lambda e: e.memset(bias2, 0.0), (), ["bias2"])
                for h in range(2):
                    P.op("dve", lambda e, h=h: e.tensor_reduce(out=kms[own[h], :], in_=KZ[h][own[h], :].rearrange("p (n k) -> p n k", k=256),
                                                              axis=AX.X, op=ALU.add),
                         [("KZ", h, tc) for tc in range(4)], [("kms", h)])
                    cp("dve", kmbz[h][own[h], :], kms[own[h], :], [("kms", h), ("kmbz", h)], [("kmbz", h)])
                    gp = bankv(h, 128)
                    for t in range(NT):
                        mm(gp[:, t * 8:t * 8 + 8], QZ[h][:, t * 128:(t + 1) * 128], kmbz[h],
                           True, True, [("QZ", h, t // 4), ("QZz", h, t // 8), ("kmbz", h)], bk(h))
                    tt("dve", gm[:, h * 128:(h + 1) * 128], gp, pastm[:, h * 128:(h + 1) * 128], ALU.add,
                       bk(h) + ["past"], [("gm", h)])
                    g3 = gm[:, h * 128:(h + 1) * 128].rearrange("p (g n) -> p g n", n=8)
                    cmp4 = cmp.rearrange("p (g n m) -> p g n m", n=8, m=8)
                    tt("dve", cmp4, g3.unsqueeze(2).broadcast_to([128, 16, 8, 8]), g3.unsqueeze(3).broadcast_to([128, 16, 8, 8]),
                       ALU.is_gt, [("gm", h)], ["cmp"])
                    P.op("dve", lambda e, h=h, cmp4=cmp4: e.tensor_reduce(
                        out=cnt[:, h * 128:(h + 1) * 128].rearrange("p (g n) -> p g n", n=8), in_=cmp4, axis=AX.X, op=ALU.add),
                        ["cmp"], [("cnt", h)])
                    c0 = 64 if h == 0 else 0
                    stt(bias2[:, :, c0:c0 + 8], cnt[:, h * 128:(h + 1) * 128].rearrange("p (t n) -> p t n", n=8), 2.5,
                        NB[:, h * 128:(h + 1) * 128].rearrange("p (t n) -> p t n", n=8), ALU.is_gt, ALU.mult,
                        [("cnt", h), "NB", "bias2"], ["bias2"])

                def emit_bias_T():
                    for tq in range(2, 4):
                        b = tq % 2
                        pT = bankv(b, 256, BF16)
                        for i in range(4):
                            t = tq * 4 + i
                            tr(pT[:, i * 128:(i + 1) * 128], bias2[:, t, :], ["bias2"], bk(b))
                        for h in range(2):
                            cp("dve", QZ[h][aug[h], tq * 512:(tq + 1) * 512], pT[aug[h], :], bk(b), [("QZz", h, 1)])

                def emit_tail(qb):
                    for t in (2 * qb, 2 * qb + 1):
                        act(sq, otok[:, t, :], AF.Square, [("otok", t, 0), ("otok", t, 1)], ["sq", ("ssa", j)],
                            accum_out=ssa[:, j * NT + t:j * NT + t + 1])
                    if qb % 2 == 1:
                        tq = qb // 2
                        b = tq % 2
                        pT = bankv(b, 256, BF16)
                        for i in range(4):
                            t = tq * 4 + i
                            tr(pT[:, i * 128:(i + 1) * 128], otok[:, t, :], [("otok", t, 0), ("otok", t, 1)], bk(b))
                        ts("dve", CT[:, j, tq * 512:(tq + 1) * 512], pT, aog[:, l * 4 + j:l * 4 + j + 1], None, ALU.mult, None,
                           bk(b) + ["aog"], [("CT", j, tq)])

                early = [(h, qb, kt) for h in range(2) for qb in range(4) for kt in range(2 * qb + 2)]
                late = [(h, qb, kt) for h in range(2) for qb in range(4, 8) for kt in range(2 * qb + 2)]
                tiles = early + late
                rec2 = [rec, recb]

                def emit_st(i):
                    h, qb, kt = tiles[i]
                    hr = slice(h * 64, (h + 1) * 64)
                    slot = i % 8
                    sb_ = 2 + (i % 4)
                    hb_ = (i // 4) % 2
                    ST = bankv(sb_, 256, off=hb_ * 256)
                    skey = (sb_, hb_)
                    qlo = 128 if kt == 2 * qb + 1 else 0
                    qs = slice(qb * 256 + qlo, qb * 256 + 256)
                    diag = kt >= 2 * qb
                    mm(ST[:, qlo:256], KZ[h][:, kt * 128:(kt + 1) * 128], QZ[h][:, qs], True, not diag,
                       [("KZ", h, kt // 4), ("KZz", h), ("QZ", h, qb // 2), ("QZz", h, qb // 4)], [skey])
                    if diag:
                        dq0 = 0 if kt == 2 * qb else 128
                        mm(ST[:, dq0:dq0 + 128], identb, mneg, False, True, ["identb", "mneg"], [skey])
                    act(PT[slot][:, qlo:256], ST[:, qlo:256], AF.Exp, [skey], [("PT", slot)], scale=0.125)

                def emit_pv(i):
                    h, qb, kt = tiles[i]
                    slot = i % 8
                    pt = PT[slot]
                    ob = 6 + (h * 8 + qb) % 2
                    O = bankv(ob, 130).rearrange("p (q c) -> p q c", c=65)
                    nkt = 2 * qb + 2
                    qlo = 128 if kt == 2 * qb + 1 else 0
                    for qi in range(2):
                        if qi * 128 < qlo:
                            continue
                        mm(O[:, qi, :], pt[:, qi * 128:(qi + 1) * 128], VA[:, kt, h, :], (kt == 0 and qi == 0),
                           (kt == 2 * qb + qi), [("PT", slot), ("VA", kt // 4), "VAone"], bk(ob), sgc=True)
                    if kt == nkt - 1:
                        rc = rec2[ob % 2]
                        rk = ("rec", ob % 2)
                        P.op("dve", lambda e, O=O, rc=rc: e.reciprocal(out=rc.rearrange("p (q c) -> p q c", c=1), in_=O[:, :, 64:65]),
                             bk(ob), [rk])
                        for qi in range(2):
                            t = qb * 2 + qi
                            ts("dve", otok[:, t, h * 64:(h + 1) * 64], O[:, qi, 0:64], rc[:, qi:qi + 1], None,
                               ALU.mult, None, bk(ob) + [rk], [("otok", t, h)])
                        if h == 1:
                            emit_tail(qb)

                LAG = 4
                for i in range(len(tiles) + LAG):
                    if i == len(early):
                        emit_bias_T()
                    if i < len(tiles):
                        emit_st(i)
                    if i >= LAG:
                        emit_pv(i - LAG)

            def ret_unit(r):
                u = 4 + r
                sl = u % 2
                W = WIN[sl]
                wk = ("WIN", sl)
                o = U
                raS = [carve(256, at=o + i * 256) for i in range(2)]; o += 512
                rbS = [carve(256, at=o + i * 256) for i in range(2)]; o += 512
                rrS = [carve(256, at=o + i * 256) for i in range(2)]; o += 512
                qd = carve(256, BF16, at=o).rearrange("p (i c) -> p i c", c=128); o += 256
                ki = carve(256, BF16, at=o).rearrange("p (i c) -> p i c", c=128); o += 256
                kd = carve(1024, BF16, at=o).rearrange("p (t c) -> p t c", c=128); o += 1024
                vr = carve(1024, BF16, at=o).rearrange("p (t c) -> p t c", c=128); o += 1024
                sg = carve(1024, BF16, at=o).rearrange("p (t c) -> p t c", c=128); o += 1024
                qdT = carve(1024, BF16, at=o); o += 1024
                kiT = carve(1024, BF16, at=o); o += 1024
                PTr = [carve(64, BF16, at=o + i * 64) for i in range(2)]; o += 128
                stf = [carve(128, at=o + i * 128) for i in range(2)]; o += 256
                stb = [carve(64, BF16, at=o + i * 64) for i in range(2)]; o += 128
                Of = carve(2048, at=o).rearrange("p (t c) -> p t c", c=128); o += 2048
                on = [carve(64, BF16, at=o + i * 64) for i in range(2)]; o += 128
                sqr = carve(64, BF16, at=o); o += 64
                ssr = carve(NT, at=o); o += NT
                rsr = carve(NT, at=o); o += NT
                assert o <= TOT
                dq = dec[:, r * 3 + 0:r * 3 + 1]
                dki = dec[:, r * 3 + 1:r * 3 + 2]
                dkd = dec[:, r * 3 + 2:r * 3 + 3]
                gamma_c = float((1.0 - 2.0 ** (-5.0 - r)) ** 128)
                def emit_inproj_tile(t):
                    tq, i = t // 4, t % 4
                    b = t % 2
                    pp = bankv(b, 512)
                    for kc in range(KC):
                        mm(pp, HT[:, kc, t * 128:(t + 1) * 128], W[:, kc, :], kc == 0, kc == KC - 1,
                           [wk, ("HT", kc, tq)], bk(b))
                    bkk = bk(b)
                    ra, rb, rr = raS[t % 2], rbS[t % 2], rrS[t % 2]
                    p2 = t % 2
                    QK = pp[:, 0:256].rearrange("p (a h c) -> p a h c", a=2, h=2)
                    ra4 = ra.rearrange("p (a h c) -> p a h c", a=2, h=2)
                    rb3 = rb.rearrange("p (a h c) -> p a h c", a=2, h=2)
                    rr4 = rr.rearrange("p (a h c) -> p a h c", a=2, h=2)
                    cb4 = cosT[:, t, :].unsqueeze(1).unsqueeze(1).broadcast_to([128, 2, 2, 64])
                    sb3 = sinT[:, t, :].unsqueeze(1).broadcast_to([128, 2, 64])
                    tt("dve", ra4, QK, cb4, ALU.mult, bkk + ["cos"], [("ra", p2)])
                    tt("dve", rb3[:, :, 0, :], QK[:, :, 1, :], sb3, ALU.mult, bkk + ["sin"], [("rb0", p2)])
                    tt("dve", rb3[:, :, 1, :], QK[:, :, 0, :], sb3, ALU.mult, bkk + ["sin"], [("rb1", p2)])
                    tt("dve", rr4[:, :, 0, :], ra4[:, :, 0, :], rb3[:, :, 0, :], ALU.subtract, [("ra", p2), ("rb0", p2)], [("rr0", p2)])
                    tt("dve", rr4[:, :, 1, :], ra4[:, :, 1, :], rb3[:, :, 1, :], ALU.add, [("ra", p2), ("rb1", p2)], [("rr1", p2)])
                    rk = [("rr0", p2), ("rr1", p2)]
                    ts("pool", qd[:, i, :], rr[:, 0:128], dq, 0.0, ALU.mult, ALU.add, rk + ["dec"], [("qd", i)])
                    ts("pool", ki[:, i, :], rr[:, 128:256], dki, 0.0, ALU.mult, ALU.add, rk + ["dec"], [("ki", i)])
                    ts("pool", kd[:, t, :], rr[:, 128:256], dkd, 0.0, ALU.mult, ALU.add, rk + ["dec"], [("kd", t)])
                    cp("act", vr[:, t, :], pp[:, 256:384], bkk, [("vr", t)])
                    act(sg[:, t, :], pp[:, 384:512], AF.Silu, bkk, [("sg", t)])
                    if i == 3:
                        for src, dst, nm in ((qd, qdT, "qdT"), (ki, kiT, "kiT")):
                            bb = 0 if nm == "qdT" else 1
                            pT = bankv(bb, 256, BF16)
                            for ii in range(4):
                                tr(pT[:, ii * 128:(ii + 1) * 128], src[:, ii, :], [(nm[:2], ii)], bk(bb))
                            cp("act" if nm == "qdT" else "dve", dst[:, tq * 512:(tq + 1) * 512], pT, bk(bb), [(nm, tq)])

                for t in range(4):
                    emit_inproj_tile(t)
                def emit_sc(n):
                    cs_ = slice(n * 128, (n + 1) * 128)
                    sl2 = n % 2
                    SC = bankv(2 + sl2, 128)
                    mm(SC, kiT[:, cs_], qdT[:, cs_], True, True, [("kiT", n // 4), ("qdT", n // 4)], [(2 + sl2, 0)])
                    tt("dve", PTr[sl2], SC, m01, ALU.mult, [(2 + sl2, 0), "m01"], [("PTr", sl2)])
                    if n < NT - 1:
                        KV = bankv(6 + sl2, 128)
                        mm(KV, kd[:, n, :], vr[:, n, :], True, True, [("kd", n), ("vr", n)], [(6 + sl2, 0)])

                emit_sc(0)
                for n in range(NT):
                    cs_ = slice(n * 128, (n + 1) * 128)
                    sl2 = n % 2
                    if n < NT - 1:
                        KV = bankv(6 + sl2, 128)
                        if n == 0:
                            cp("dve", stf[0], KV, [(6 + sl2, 0)], [("stf", 0)])
                        else:
                            stt(stf[n % 2], stf[(n - 1) % 2], gamma_c, KV, ALU.mult, ALU.add,
                                [("stf", (n - 1) % 2), (6 + sl2, 0)], [("stf", n % 2)])
                        cp("pool", stb[n % 2], stf[n % 2], [("stf", n % 2)], [("stb", n % 2)])
                    if n + 4 < NT:
                        emit_inproj_tile(n + 4)
                    if n + 1 < NT:
                        emit_sc(n + 1)
                    OB = bankv(4 + sl2, 128)
                    mm(OB, PTr[sl2], vr[:, n, :], True, n == 0, [("PTr", sl2), ("vr", n)], [(4 + sl2, 0)])
                    if n > 0:
                        mm(OB, qdT[:, cs_], stb[(n - 1) % 2], False, True, [("qdT", n // 4), ("stb", (n - 1) % 2)],
                           [(4 + sl2, 0)])
                    cp("act", Of[:, n, :], OB, [(4 + sl2, 0)], [("Of", n)])
                    act(sqr, OB, AF.Square, [(4 + sl2, 0)], ["sqr", "ssr"], accum_out=ssr[:, n:n + 1])
                ts("dve", rsr, ssr, 1.0 / 128, EPS, ALU.mult, ALU.add, ["ssr"], ["rsr"])
                act(rsr, rsr, AF.Sqrt, ["rsr"], ["rsr"])
                P.op("dve", lambda e: e.reciprocal(out=rsr, in_=rsr), ["rsr"], ["rsr"])
                for tq in range(4):
                    b = 6 + tq % 2
                    pT = bankv(b, 256, BF16)
                    for i in range(4):
                        t = tq * 4 + i
                        stt(on[t % 2], Of[:, t, :], rsr[:, t:t + 1], sg[:, t, :], ALU.mult, ALU.mult,
                            [("Of", t), "rsr", ("sg", t)], [("on", t % 2)])
                        tr(pT[:, i * 128:(i + 1) * 128], on[t % 2], [("on", t % 2)], bk(b))
                    ts("dve", CT[:, 4 + r, tq * 512:(tq + 1) * 512], pT, rog[:, l * 4 + r:l * 4 + r + 1], None, ALU.mult, None,
                       bk(b) + ["rog"], [("CT", 4 + r, tq)])

            load_unit(0)
            for u in range(8):
                if u + 1 < 8:
                    load_unit(u + 1)
                if u < 4:
                    att_unit(u)
                else:
                    ret_unit(u - 4)
                tap(f"CT{u}_{l}", CT[:, u, :], [("CT", u, tq) for tq in range(4)])
                ck(f"u{u}_{l}")
            castload(6, WO[:, 0:4, :], wout_d[l, :, 0:4, :], (), [("WIN", 0)])
            castload(7, WO[:, 4:8, :], wout_d[l, :, 4:8, :], (), [("WIN", 1)])
            for h in range(2):
                tt("pool", WO[:, 4 * h:4 * h + 4, :], WO[:, 4 * h:4 * h + 4, :], GBC.unsqueeze(1).broadcast_to([128, 4, D]), ALU.mult,
                   [("WIN", h), "GBC"], [("WIN", h)])
            P.op("dve", lambda e: e.tensor_reduce(out=rstda, in_=ssa.rearrange("p (u t) -> p t u", t=NT), axis=AX.X, op=ALU.add),
                 [("ssa", j) for j in range(4)], ["rstda"])
            ts("dve", rstda, rstda, 1.0 / 512, EPS, ALU.mult, ALU.add, ["rstda"], ["rstda"])
            act(rstda, rstda, AF.Sqrt, ["rstda"], ["rstda"])
            P.op("dve", lambda e: e.reciprocal(out=rstda, in_=rstda), ["rstda"], ["rstda"])
            bi = 0
            for t in range(NT):
                for hf in range(2):
                    xs = X[:, t, hf * 512:(hf + 1) * 512]
                    b = bi % 4; bi += 1
                    for kc in range(4):
                        mm(bankv(b, 512), CT[:, kc, t * 128:(t + 1) * 128], WO[:, kc, hf * 512:(hf + 1) * 512],
                           kc == 0, kc == 3, [("CT", kc, t // 4), ("WIN", 0)], bk(b))
                    stt(xs, bankv(b, 512), rstda[:, t:t + 1], xs, ALU.mult, ALU.add, bk(b) + ["rstda", ("X", t)], [("X", t)])
                    b = bi % 4; bi += 1
                    for kc in range(4, 8):
                        mm(bankv(b, 512), CT[:, kc, t * 128:(t + 1) * 128], WO[:, kc, hf * 512:(hf + 1) * 512],
                           kc == 4, kc == 7, [("CT", kc, t // 4), ("WIN", 1)], bk(b))
                    tt("dve", xs, bankv(b, 512), xs, ALU.add, bk(b) + [("X", t)], [("X", t)])

        GROUPS = [(0, 4), (4, 4), (8, 4), (12, 4), (16, 4), (20, 2)]

        def ffn(experts, stage):
            o = PH
            AT = carve(4096, BF16, at=o).rearrange("p (j s) -> p j s", s=S); o += 4096
            WG = [carve(2048, BF16, at=o + i * 6144).rearrange("p (k c) -> p k c", c=512) for i in range(2)]
            WU = [carve(2048, BF16, at=o + 2048 + i * 6144).rearrange("p (k c) -> p k c", c=512) for i in range(2)]
            WD = [carve(2048, BF16, at=o + 4096 + i * 6144).rearrange("p (j c) -> p j c", c=D) for i in range(2)]
            o += 12288
            SG = [carve(512, at=o + i * 512) for i in range(2)]; o += 1024
            assert o <= TOT
            work = [(e, g) for e in range(len(experts)) for g in range(len(GROUPS))]

            def load(idx):
                e, g = work[idx]
                gd, ud, dd, _ = experts[e]
                j0, nj = GROUPS[g]
                sl = idx % 2
                castload(8 + sl, WG[sl][:, :, 0:nj * 128], gd[:, :, j0 * 128:(j0 + nj) * 128], (), [("WG", sl)])
                castload(10 + sl, WU[sl][:, :, 0:nj * 128], ud[:, :, j0 * 128:(j0 + nj) * 128], (), [("WU", sl)])
                castload(12 + sl, WD[sl][:, 0:nj, :], dd[:, j0:j0 + nj, :], (), [("WD", sl)])
                tt("pool", WD[sl][:, 0:nj, :], WD[sl][:, 0:nj, :], GBC.unsqueeze(1).broadcast_to([128, nj, D]), ALU.mult,
                   [("WD", sl), "GBC"], [("WD", sl)])

            if stage == "pre":
                load(0)
                return
            gi = 0
            di = 0
            for idx, (e, g) in enumerate(work):
                if idx + 1 < len(work):
                    load(idx + 1)
                sl = idx % 2
                j0, nj = GROUPS[g]
                wcol = experts[e][3]
                for tc in range(4):
                    for j in range(nj):
                        bg = gi % 2
                        bu = 2 + gi % 2
                        gi += 1
                        for kc in range(KC):
                            mm(bankv(bg, 512), WG[sl][:, kc, j * 128:(j + 1) * 128], HT[:, kc, tc * 512:(tc + 1) * 512],
                               kc == 0, kc == KC - 1, [("WG", sl), ("HT", kc, tc)], bk(bg))
                        for kc in range(KC):
                            mm(bankv(bu, 512), WU[sl][:, kc, j * 128:(j + 1) * 128], HT[:, kc, tc * 512:(tc + 1) * 512],
                               kc == 0, kc == KC - 1, [("WU", sl), ("HT", kc, tc)], bk(bu))
                        sgt = SG[bg]
                        act(sgt, bankv(bg, 512), AF.Silu, bk(bg), [("SG", bg)])
                        tt("dve", AT[:, j, tc * 512:(tc + 1) * 512], sgt, bankv(bu, 512), ALU.mult,
                           [("SG", bg)] + bk(bu), [("AT", j, tc)])
                for t in range(NT):
                    for hf in range(2):
                        b = 4 + di % 4
                        di += 1
                        for j in range(nj):
                            mm(bankv(b, 512), AT[:, j, t * 128:(t + 1) * 128], WD[sl][:, j, hf * 512:(hf + 1) * 512],
                               j == 0, j == nj - 1, [("AT", j, t // 4), ("WD", sl)], bk(b))
                        xs = X[:, t, hf * 512:(hf + 1) * 512]
                        if wcol is None:
                            tt("dve", xs, bankv(b, 512), xs, ALU.add, bk(b) + [("X", t)], [("X", t)])
                        else:
                            stt(xs, bankv(b, 512), wgt[:, t * NE + wcol:t * NE + wcol + 1], xs, ALU.mult, ALU.add,
                                bk(b) + [("X", t), "wgt"], [("X", t)])

        def router():
            lp = bankv(7, 128)
            for t in range(NT):
                for kc in range(KC):
                    mm(lp[:, t * 8:(t + 1) * 8], HT[:, kc, t * 128:(t + 1) * 128], rwb[:, kc, :], kc == 0, kc == KC - 1,
                       [("HT", kc, t // 4), "rwb"], bk(7))
            cp("dve", lg, lp, bk(7), ["lg"])
            l3 = lg.rearrange("p (t e) -> p t e", e=NE)
            a3 = r1.rearrange("p (t e) -> p t e", e=NE)
            b3 = r2.rearrange("p (t e) -> p t e", e=NE)
            w3 = wgt.rearrange("p (t e) -> p t e", e=NE)
            m1b = r3.unsqueeze(2).broadcast_to([128, NT, NE])
            m2b = r4.unsqueeze(2).broadcast_to([128, NT, NE])
            P.op("dve", lambda e: e.tensor_reduce(out=r3, in_=l3, axis=AX.X, op=ALU.max), ["lg"], ["r3"])
            tt("dve", a3, l3, m1b, ALU.is_equal, ["lg", "r3"], ["r1"])
            stt(b3, a3, -1e30, l3, ALU.mult, ALU.add, ["r1", "lg"], ["r2"])
            P.op("dve", lambda e: e.tensor_reduce(out=r4, in_=b3, axis=AX.X, op=ALU.max), ["r2"], ["r4"])
            tt("dve", a3, l3, m2b, ALU.is_ge, ["lg", "r4", "r1"], ["r1"])
            tt("dve", b3, l3, m1b, ALU.subtract, ["lg", "r3", "r2"], ["r2"])
            act(r2, r2, AF.Exp, ["r2"], ["r2"])
            tt("dve", b3, b3, a3, ALU.mult, ["r2", "r1"], ["r2"])
            P.op("dve", lambda e: e.tensor_reduce(out=r3, in_=b3, axis=AX.X, op=ALU.add), ["r2", "r3"], ["r3"])
            P.op("dve", lambda e: e.reciprocal(out=r3, in_=r3), ["r3"], ["r3"])
            tt("dve", w3, b3, m1b, ALU.mult, ["r2", "r3"], ["wgt"])

        def ck(name):
            if stop == name:
                raise StopBuild()

        def tap(name, ap, keys):
            if name not in tap_d:
                return
            dt_ = tap_d[name]
            P.dma("sp", 3, lambda e: e.dma_start(out=dt_, in_=ap), list(keys), [("dbg", "tap", name)])

        try:
            ck("consts")
            for l in range(DEPTH):
                compute_mod(l)
                tap(f"modT{l}", modT, ["modT"])
                ck(f"mod{l}")
                make_gbc(modT[:, 16:24], "modT")
                tap(f"gbc{l}", GBC, ["GBC"])
                ck(f"gbc{l}")
                make_hT(s1, modT[:, 0:8], ["s1", "modT"])
                tap(f"hT{l}", HT.rearrange("p k s -> p (k s)"), HT_ALL)
                ck(f"hT{l}")
                barrier()
                mixer(l)
                dump(f"mix{l}")
                ck(f"mix{l}")
                barrier()
                make_gbc(modT[:, 40:48], "modT")
                if l % 2 == 0:
                    experts = [(fg_d, fu_d, fd_d, None)]
                else:
                    experts = [(mg_d[e], mu_d[e], md_d[e], e) for e in range(NE)]
                ffn(experts, "pre")
                make_hT(s2, modT[:, 24:32], ["s2", "modT"])
                barrier()
                if l % 2 == 1:
                    router()
                ffn(experts, "main")
                dump(f"ffn{l}")
                ck(f"ffn{l}")
                barrier()
        except StopBuild:
            barrier()
        make_gbc(fng, "fng")
        yt = [carve(D, at=PH + 8000 + i * D) for i in range(2)]
        junk = carve(512, BF16, at=PH)
        for t in range(NT):
            act(junk, X[:, t, :], AF.Square, [("X", t)], ["junk", "ssf"], accum_out=ss[:, t:t + 1])
        ts("dve", rstd, ss, 1.0 / D, EPS, ALU.mult, ALU.add, ["ssf"], ["rstdf"])
        act(rstd, rstd, AF.Sqrt, ["rstdf"], ["rstdf"])
        P.op("dve", lambda e: e.reciprocal(out=rstd, in_=rstd), ["rstdf"], ["rstdf"])
        for t in range(NT):
            stt(yt[t % 2], X[:, t, :], rstd[:, t:t + 1], GBC, ALU.mult, ALU.mult, [("X", t), "rstdf", "GBC"], [("yt", t % 2)])
            P.dma("sp", 14 + t % 2, lambda e, t=t: e.dma_start(out=y_d[t * 128:(t + 1) * 128, :], in_=yt[t % 2]),
                  [("yt", t % 2)], [("y", t)])
        P.final_wait("sp", [("y", t) for t in range(NT)] + [k for k in P.lastw if isinstance(k, tuple) and k[0] == "dbg"])

        with nc.Block() as block:
            @block.sync
            def _(e):
                P.replay("sp", e)

            @block.tensor
            def _(e):
                P.replay("pe", e)

            @block.vector
            def _(e):
                P.replay("dve", e)

            @block.scalar
            def _(e):
                P.replay("act", e)

            @block.gpsimd
            def _(e):
                P.replay("pool", e)
    return nc


def host_consts():
    f = np.float32
    c = {}
    c["idn"] = np.eye(128, dtype=f)
    half = 64
    inv_freq = (10000.0 ** (-np.arange(half, dtype=np.float32) / half)).astype(f)
    pos = np.arange(S, dtype=np.float32)
    ang = (pos[:, None] * inv_freq[None, :]).astype(f)
    cos = np.cos(ang).astype(f).reshape(NT, 128, half).transpose(1, 0, 2).reshape(128, NT * half)
    sin = np.sin(ang).astype(f).reshape(NT, 128, half).transpose(1, 0, 2).reshape(128, NT * half)
    c["cos"] = np.ascontiguousarray(cos)
    c["sin"] = np.ascontiguousarray(sin)
    dec = np.zeros((128, 12), f)
    idx = np.arange(128, dtype=np.float64)
    for r in range(4):
        lgm = np.log(1.0 - 2.0 ** (-5.0 - r))
        dec[:, r * 3 + 0] = np.exp((idx + 1.0) * lgm)
        dec[:, r * 3 + 1] = np.exp(-(idx + 1.0) * lgm) * (128.0 ** -0.5)
        dec[:, r * 3 + 2] = np.exp((127.0 - idx) * lgm) * (128.0 ** -0.5)
    c["dec"] = dec
    kk = np.arange(128)
    c["m01"] = (kk[None, :] >= kk[:, None]).astype(f)
    c["mneg"] = np.where(kk[:, None] > kk[None, :], NEG, 0.0).astype(f)
    en = np.zeros((128, 8, 128), f)
    for n in range(8):
        en[n, n, :] = 1.0
        en[64 + n, n, :] = 1.0
    c["en"] = en.reshape(128, 8 * 128)
    past = np.zeros((2, NT, 8), f)
    nb = np.zeros((2, NT, 8), f)
    for t in range(NT):
        qb = t // 2
        for n in range(8):
            past[:, t, n] = 0.0 if n < qb else -1e30
            nb[:, t, n] = 0.0 if n == qb else NEG
    c["past"] = np.ascontiguousarray(np.broadcast_to(past.reshape(1, 256), (128, 256)))
    c["nb"] = np.ascontiguousarray(np.broadcast_to(nb.reshape(1, 256), (128, 256)))
    return c


def fm(v, n):
    return np.ascontiguousarray(np.asarray(v, np.float32).reshape(n, 128).T)


def prep_shared(inp):
    f = np.float32
    sh = dict(host_consts())
    sh["nmg"] = np.concatenate([fm(inp["norm_mix_g"][l], KC) for l in range(DEPTH)], axis=1)
    sh["nfg"] = np.concatenate([fm(inp["norm_ffn_g"][l], KC) for l in range(DEPTH)], axis=1)
    sh["aog"] = np.concatenate([fm(inp["att_out_g"][l], 4) for l in range(DEPTH)], axis=1)
    sh["rog"] = np.concatenate([fm(inp["ret_out_g"][l], 4) for l in range(DEPTH)], axis=1)
    sh["fng"] = fm(inp["final_norm_g"], KC)
    sh["adaw"] = np.ascontiguousarray(np.asarray(inp["ada_w"], f).reshape(DEPTH, KC, 128, 6 * D))
    sh["adabr"] = np.ascontiguousarray(np.asarray(inp["ada_b"], f))
    sh["adab"] = np.concatenate([fm(inp["ada_b"][l], 48) for l in range(DEPTH)], axis=1)
    cols = []
    for j in range(4):
        for w in range(3):
            cols += list(range(w * 512 + j * 128, w * 512 + (j + 1) * 128))
    for r in range(4):
        for w in range(4):
            cols += list(range(1536 + w * 512 + r * 128, 1536 + w * 512 + (r + 1) * 128))
    cols = np.array(cols)

    def pk(w):
        w = np.asarray(w, f)
        k = w.shape[0] // 128
        return np.ascontiguousarray(w.reshape(k, 128, w.shape[1]).transpose(1, 0, 2))
    sh["win"] = np.stack([pk(np.asarray(inp["w_in"][l], f)[:, cols]) for l in range(DEPTH)])
    sh["wout"] = np.stack([pk(inp["w_out"][l]) for l in range(DEPTH)])
    sh["fg"] = pk(inp["ffn_w_gate"][0])
    sh["fu"] = pk(inp["ffn_w_up"][0])
    sh["fd"] = pk(inp["ffn_w_down"][0])
    sh["rw"] = pk(inp["router_w"][0]).reshape(128, KC * NE)
    sh["mg"] = np.stack([pk(inp["moe_w_gate"][0][e]) for e in range(NE)])
    sh["mu"] = np.stack([pk(inp["moe_w_up"][0][e]) for e in range(NE)])
    sh["md"] = np.stack([pk(inp["moe_w_down"][0][e]) for e in range(NE)])
    return sh


def kernel(**inputs):
    inp = {k: np.asarray(v) for k, v in inputs.items()}
    sh = prep_shared(inp)
    nc = build_nc()
    in_maps = []
    for b in range(8):
        m = dict(sh)
        m["x"] = np.ascontiguousarray(inp["x"][b], dtype=np.float32)
        m["cT"] = fm(inp["c"][b], KC)
        in_maps.append(m)
    res = run_bass_kernel_spmd(nc, in_maps, core_ids=list(range(8)))
    return np.stack([np.asarray(r["y"], dtype=np.float32) for r in res.results], axis=0)
```
